# Optimizing a Trainium2 kernel written in Bass

```python
import math
import jax, jax.numpy as jnp
from jax import lax
import numpy as np

D_MODEL = 2048
BATCH = 4
SEQ = 4096
DEPTH = 2

GRID_W = 64
CTX_LEN = 256
N_GROUPS = 4
D_GROUP = D_MODEL // N_GROUPS
D_IN = 13 * D_GROUP
N_MOD = 6
RMS_EPS = 1e-6

HY_ORDER = 2
HY_EMB = 33
HY_FFN = 64
HY_TARGET = 1e-2
HY_STRONG_DECAY_PCT = 0.3
HY_WEAK_DECAY_PCT = 1.5
HY_MIN_DECAY = math.log(HY_TARGET) / HY_WEAK_DECAY_PCT
HY_MAX_DECAY = math.log(HY_TARGET) / HY_STRONG_DECAY_PCT

RG_HEADS = 8
RG_CONV = 4
RG_C = 8.0

RW_HEAD = 64
RW_HEADS = D_GROUP // RW_HEAD
RW_DECAY_LORA = 64
RW_A_LORA = 64
RW_V_LORA = 32
RW_G_LORA = 128
RW_GN_EPS = 64e-5

RT_HEADS = 4
RT_DK = D_GROUP // RT_HEADS
RT_DV = D_GROUP // RT_HEADS
RT_CHUNK = 128
RT_GN_EPS = 1e-6
ROPE_BASE = 10000.0

N_EXPERTS = 32
TOP_K = 4
D_EXPERT = D_MODEL // 2
SWIGLU_LIMIT = 7.0
SWIGLU_ALPHA = 1.702

kernel_name = "hybrid_prefix_dit_trunk"


def rms_norm(x, w):
    xf = x.astype(jnp.float32)
    y = xf * lax.rsqrt(jnp.mean(xf * xf, axis=-1, keepdims=True) + RMS_EPS)
    return (y * w.astype(jnp.float32)).astype(x.dtype)


def modulate(x, w, shift, scale):
    return rms_norm(x, w) * (1 + scale) + shift


def shift_prev(x, n):
    if n == 0:
        return x
    return jnp.pad(x, ((0, 0), (n, 0), (0, 0)))[:, : x.shape[1]]


def shift_next(x, n):
    return jnp.pad(x, ((0, 0), (0, n), (0, 0)))[:, n:]


def linrec_combine(lhs, rhs):
    a1, b1 = lhs
    a2, b2 = rhs
    return a1 * a2, a2 * b1 + b2


def rope_2d(x, rows, cols):
    half = x.shape[-1] // 2
    quarter = half // 2
    inv = jnp.power(ROPE_BASE, -jnp.arange(quarter, dtype=jnp.float32) / quarter)

    def rot(xp, pos):
        ang = pos[:, None] * inv[None, :]
        cos = jnp.cos(ang)[None, :, None, :]
        sin = jnp.sin(ang)[None, :, None, :]
        x1, x2 = xp[..., :quarter], xp[..., quarter:]
        return jnp.concatenate([x1 * cos - x2 * sin, x1 * sin + x2 * cos], axis=-1)

    return jnp.concatenate([rot(x[..., :half], rows), rot(x[..., half:], cols)], axis=-1)


def bidirectional(fn, ctx_in, lat_in, state0, dir_params):
    out_c, out_l = 0.0, 0.0
    for d in range(2):
        p = tuple(a[d] for a in dir_params)
        flip = (lambda t: jnp.flip(t, axis=1)) if d == 1 else (lambda t: t)
        oc, s_ctx = fn(tuple(flip(t) for t in ctx_in), state0, p, d)
        ol, _ = fn(tuple(flip(t) for t in lat_in), s_ctx, p, d)
        out_c = out_c + flip(oc)
        out_l = out_l + flip(ol)
    return out_c, out_l


def hyena_filter_spectrum(L, w1, b1, w2, b2, w3, b3, w4, freq):
    f32 = jnp.float32
    t = jnp.linspace(0.0, 1.0, L, dtype=f32)[:, None]
    bands = (HY_EMB - 1) // 2
    fr = jnp.linspace(1e-4, bands - 1, bands, dtype=f32)
    ang = (2.0 * math.pi / L) * jnp.arange(L, dtype=f32)[:, None] * fr[None, :]
    z = jnp.concatenate([t, jnp.cos(ang), -jnp.sin(ang)], axis=-1)
    h = jnp.sin(freq * (z @ w1 + b1))
    h = jnp.sin(freq * (h @ w2 + b2))
    h = jnp.sin(freq * (h @ w3 + b3))
    h = (h @ w4).reshape(L, 2, HY_ORDER, D_GROUP)
    deltas = jnp.abs(jnp.linspace(HY_MIN_DECAY, HY_MAX_DECAY, D_GROUP, dtype=f32))
    h = h * jnp.exp(-t * deltas)[:, None, None, :]
    taps = jnp.concatenate(
        [h[:, 0], jnp.zeros((1, HY_ORDER, D_GROUP), f32), h[:0:-1, 1]], axis=0)
    taps = taps / jnp.sum(jnp.abs(taps), axis=0, keepdims=True)
    return jnp.fft.rfft(taps, axis=0)


def hyena_mixer(u, short_w, short_b, fw1, fb1, fw2, fb2, fw3, fb3, fw4, ffreq, bias):
    L = u.shape[1]
    u = short_w[0] * shift_prev(u, 1) + short_w[1] * u + short_w[2] * shift_next(u, 1) + short_b
    v, x1, x2 = jnp.split(u, 3, axis=-1)
    spec = hyena_filter_spectrum(L, fw1, fb1, fw2, fb2, fw3, fb3, fw4, ffreq)
    z = v
    for o, gate in enumerate((x1, x2)):
        zf = jnp.fft.rfft(z, n=2 * L, axis=1)
        z = jnp.fft.irfft(zf * spec[None, :, o], n=2 * L, axis=1)[:, :L] + bias[o] * z
        z = gate * z
    return z


def rglru_direction(inp, h0, p, d):
    (xb,) = inp
    conv_w, conv_b, wa, ba, wx, bx, lam = p
    xc = conv_b + sum(conv_w[j] * shift_prev(xb, RG_CONV - 1 - j) for j in range(RG_CONV))
    B_, L = xb.shape[0], xb.shape[1]
    xh = xc.reshape(B_, L, RG_HEADS, -1)
    r = jax.nn.sigmoid(jnp.einsum('blhi,hij->blhj', xh, wa).reshape(B_, L, -1) + ba)
    i = jax.nn.sigmoid(jnp.einsum('blhi,hij->blhj', xh, wx).reshape(B_, L, -1) + bx)
    log_a = -RG_C * r * jax.nn.softplus(-lam)
    a = jnp.exp(log_a)
    b = jnp.sqrt(-jnp.expm1(2.0 * log_a)) * (i * xc)
    a_cum, h = lax.associative_scan(linrec_combine, (a, b), axis=1)
    h = h + a_cum * h0[:, None, :]
    return h, h[:, -1]


def rglru_mixer(s_ctx, s_lat, conv_w, conv_b, wa, ba, wx, bx, lam):
    xc_, gc_ = jnp.split(s_ctx, 2, axis=-1)
    xl_, gl_ = jnp.split(s_lat, 2, axis=-1)
    h0 = jnp.zeros((s_ctx.shape[0], D_GROUP), jnp.float32)
    oc, ol = bidirectional(rglru_direction, (xc_,), (xl_,), h0,
                           (conv_w, conv_b, wa, ba, wx, bx, lam))
    return oc * jax.nn.gelu(gc_), ol * jax.nn.gelu(gl_)


def rwkv_mixer(s_ctx, s_lat, vf_ctx, vf_lat, vmix, mu, w0, w1, w2, a0, a1, a2,
               g1, g2, k_k, k_a, r_k, ln_w, ln_b):
    def prep(s, vfirst):
        r, k, v, z = jnp.split(s, 4, axis=-1)
        if vmix is None:
            vfirst = v
        else:
            v0, v1, v2 = vmix
            v = v + (vfirst - v) * jax.nn.sigmoid(v0 + (z @ v1) @ v2)
        g = jax.nn.sigmoid(z @ g1) @ g2
        return (r, k, v, z), g, vfirst

    in_c, g_c, vf_c = prep(s_ctx, vf_ctx)
    in_l, g_l, vf_l = prep(s_lat, vf_lat)

    def direction(inp, S0, p, d):
        mu_d, w0_d, w1_d, w2_d, a0_d, a1_d, a2_d = p
        r, k, v, z = [s + (shift_prev(s, 1) - s) * mu_d[j] for j, s in enumerate(inp)]
        w_log = -jax.nn.softplus(-(w0_d + jnp.tanh(z @ w1_d) @ w2_d)) - 0.5
        decay = jnp.exp(-jnp.exp(w_log))
        a = jax.nn.sigmoid(a0_d + (z @ a1_d) @ a2_d)
        B_, L = r.shape[0], r.shape[1]
        heads = lambda t: t.reshape(B_, L, RW_HEADS, RW_HEAD)
        kk = heads(k * k_k)
        kk = kk / jnp.maximum(jnp.sqrt(jnp.sum(kk * kk, axis=-1, keepdims=True)), 1e-12)
        k = k * (1 + (a - 1) * k_a)
        rh, wh, kh, vh, ah = (heads(t) for t in (r, decay, k, v, a))
        xs = tuple(jnp.moveaxis(t, 1, 0) for t in (rh, wh, kh, vh, kk, kk * ah))

        def step(S, xt):
            r_t, w_t, k_t, v_t, kk_t, kka_t = xt
            S = (S * w_t[..., None, :]
                 - jnp.einsum('bhij,bhj->bhi', S, kk_t)[..., None] * kka_t[..., None, :]
                 + v_t[..., None] * k_t[..., None, :])
            return S, jnp.einsum('bhij,bhj->bhi', S, r_t)

        S_fin, y = lax.scan(step, S0, xs)
        y = jnp.moveaxis(y, 0, 1)
        m = jnp.mean(y, axis=-1, keepdims=True)
        var = jnp.mean(jnp.square(y - m), axis=-1, keepdims=True)
        y = ((y - m) * lax.rsqrt(var + RW_GN_EPS)).reshape(B_, L, -1) * ln_w + ln_b
        bonus = (jnp.sum(rh * kh * r_k, axis=-1, keepdims=True) * vh).reshape(B_, L, -1)
        return y + bonus, S_fin

    S0 = jnp.zeros((s_ctx.shape[0], RW_HEADS, RW_HEAD, RW_HEAD), jnp.float32)
    oc, ol = bidirectional(direction, in_c, in_l, S0, (mu, w0, w1, w2, a0, a1, a2))
    return oc * g_c, ol * g_l, vf_c, vf_l


def retention_direction(inp, S0, p, d):
    q, k, v = inp
    (decay_raw,) = p
    B_, L, H, dk = q.shape
    dv = v.shape[-1]
    n = L // RT_CHUNK
    qc = q.reshape(B_, n, RT_CHUNK, H, dk)
    kc = k.reshape(B_, n, RT_CHUNK, H, dk)
    vc = v.reshape(B_, n, RT_CHUNK, H, dv)
    lg = -jax.nn.softplus(decay_raw.astype(jnp.float32))
    idx = jnp.arange(RT_CHUNK, dtype=jnp.float32)
    diff = idx[:, None] - idx[None, :]
    keep = diff > 0 if d == 1 else diff >= 0
    dmask = jnp.where(keep, jnp.exp(jnp.where(keep, diff, 0.0)[None] * lg[:, None, None]), 0.0)
    inner = jnp.einsum('bnihd,bnjhd->bnhij', qc, kc) * dmask[None, None]
    inner = jnp.einsum('bnhij,bnjhe->bnihe', inner, vc)
    k_dec = jnp.exp((RT_CHUNK - 1 - idx)[:, None] * lg[None, :])
    kv = jnp.einsum('bnjhd,bnjhe->nbhde', kc * k_dec[:, :, None], vc)
    chunk_dec = jnp.exp(RT_CHUNK * lg)[None, :, None, None]

    def step(S, kv_n):
        return chunk_dec * S + kv_n, S

    S_fin, S_prev = lax.scan(step, S0, kv)
    q_dec = jnp.exp((idx + 1.0)[:, None] * lg[None, :])
    cross = jnp.einsum('bnihd,nbhde->bnihe', qc * q_dec[:, :, None], S_prev)
    return (inner + cross).reshape(B_, L, H, dv), S_fin


def retention_mixer(s_ctx, s_lat, rows, cols, decay_raw, gn_w):
    def prep(s, rotate):
        q, k, v, g = jnp.split(s, 4, axis=-1)
        B_, L = s.shape[0], s.shape[1]
        q = q.reshape(B_, L, RT_HEADS, RT_DK)
        k = k.reshape(B_, L, RT_HEADS, RT_DK)
        v = v.reshape(B_, L, RT_HEADS, RT_DV)
        if rotate:
            q, k = rope_2d(q, rows, cols), rope_2d(k, rows, cols)
        return (q, k * RT_DK ** -0.5, v), g

    in_c, g_c = prep(s_ctx, False)
    in_l, g_l = prep(s_lat, True)
    S0 = jnp.zeros((s_ctx.shape[0], RT_HEADS, RT_DK, RT_DV), jnp.float32)
    oc, ol = bidirectional(retention_direction, in_c, in_l, S0, (decay_raw,))

    def finish(o, g):
        m = jnp.mean(o, axis=-1, keepdims=True)
        var = jnp.mean(jnp.square(o - m), axis=-1, keepdims=True)
        o = ((o - m) * lax.rsqrt(var + RT_GN_EPS)).reshape(o.shape[0], o.shape[1], -1)
        return o * gn_w * jax.nn.silu(g)

    return finish(oc, g_c), finish(ol, g_l)


def moe(h, router_w, router_b, w_gu, b_gu, w_dn, b_dn):
    f32 = jnp.float32
    t = h.reshape(-1, h.shape[-1])
    logits = t.astype(f32) @ router_w.astype(f32) + router_b.astype(f32)
    vals, idx = lax.top_k(logits, TOP_K)
    probs = jax.nn.softmax(vals, axis=-1)
    gate = jnp.sum(jax.nn.one_hot(idx, N_EXPERTS, dtype=f32) * probs[..., None], axis=1)
    out = jnp.zeros(t.shape, f32)
    for e in range(N_EXPERTS):
        gu = (t @ w_gu[e]).astype(f32) + b_gu[e]
        glu = jnp.minimum(gu[:, :D_EXPERT], SWIGLU_LIMIT)
        lin = jnp.clip(gu[:, D_EXPERT:], -SWIGLU_LIMIT, SWIGLU_LIMIT)
        act = glu * jax.nn.sigmoid(SWIGLU_ALPHA * glu) * (lin + 1.0)
        y = (act.astype(t.dtype) @ w_dn[e]).astype(f32) + b_dn[e]
        out = out + gate[:, e:e + 1] * y
    return out.reshape(h.shape).astype(h.dtype)


def setup_inputs(seed: int = 0) -> dict:
    key = jax.random.key(seed)
    keys = iter(jax.random.split(key, 64))
    f32 = jnp.float32
    G, D, E, F, NL = D_GROUP, D_MODEL, N_EXPERTS, D_EXPERT, DEPTH

    def normal(shape, scale):
        return jax.random.normal(next(keys), shape, f32) * scale

    def uniform(shape, lo, hi):
        return jax.random.uniform(next(keys), shape, f32, lo, hi)

    def near(shape, centre, noise=0.02):
        return centre + normal(shape, noise)

    lam_s = uniform((NL, 2, G), 0.9, 0.999) ** (1.0 / RG_C)
    rg_lambda = jnp.log(lam_s) - jnp.log1p(-lam_s)
    neg_log_gamma = -np.log1p(-np.power(2.0, -5.0 - np.arange(RT_HEADS)))
    rt_base = jnp.asarray(np.log(np.expm1(neg_log_gamma)), f32)

    return {
        'x': normal((BATCH, SEQ, D), 1.0),
        'c': normal((BATCH, D), 1.0),
        'ctx': normal((BATCH, CTX_LEN, D), 1.0),
        'c_ctx': normal((D,), 1.0),
        'ada_w': normal((NL, D, N_MOD * D), 0.5 * D ** -0.5),
        'ada_b': normal((NL, N_MOD * D), 0.02),
        'norm1_w': near((NL, D), 1.0),
        'norm2_w': near((NL, D), 1.0),
        'w_in': normal((NL, D, D_IN), D ** -0.5),
        'w_out': normal((NL, N_GROUPS * G, D), (N_GROUPS * G) ** -0.5),
        'hy_short_w': normal((NL, 3, 3 * G), 3 ** -0.5),
        'hy_short_b': normal((NL, 3 * G), 0.02),
        'hy_f_w1': normal((NL, HY_EMB, HY_FFN), HY_EMB ** -0.5),
        'hy_f_b1': normal((NL, HY_FFN), 0.1),
        'hy_f_w2': normal((NL, HY_FFN, HY_FFN), HY_FFN ** -0.5),
        'hy_f_b2': normal((NL, HY_FFN), 0.1),
        'hy_f_w3': normal((NL, HY_FFN, HY_FFN), HY_FFN ** -0.5),
        'hy_f_b3': normal((NL, HY_FFN), 0.1),
        'hy_f_w4': normal((NL, HY_FFN, 2 * HY_ORDER * G), HY_FFN ** -0.5),
        'hy_f_freq': near((NL, HY_FFN), 1.0, 0.05),
        'hy_bias': normal((NL, HY_ORDER, G), 1.0),
        'rg_conv_w': normal((NL, 2, RG_CONV, G), RG_CONV ** -0.5),
        'rg_conv_b': normal((NL, 2, G), 0.02),
        'rg_wa': normal((NL, 2, RG_HEADS, G // RG_HEADS, G // RG_HEADS), (G // RG_HEADS) ** -0.5),
        'rg_ba': normal((NL, 2, G), 0.02),
        'rg_wx': normal((NL, 2, RG_HEADS, G // RG_HEADS, G // RG_HEADS), (G // RG_HEADS) ** -0.5),
        'rg_bx': normal((NL, 2, G), 0.02),
        'rg_lambda': rg_lambda,
        'rw_mu': uniform((NL, 2, 4, G), 0.0, 1.0),
        'rw_w0': uniform((NL, 2, G), -6.5, -1.5),
        'rw_w1': normal((NL, 2, G, RW_DECAY_LORA), G ** -0.5),
        'rw_w2': normal((NL, 2, RW_DECAY_LORA, G), 0.1 * RW_DECAY_LORA ** -0.5),
        'rw_a0': normal((NL, 2, G), 0.1),
        'rw_a1': normal((NL, 2, G, RW_A_LORA), G ** -0.5),
        'rw_a2': normal((NL, 2, RW_A_LORA, G), 0.1 * RW_A_LORA ** -0.5),
        'rw_g1': normal((NL, G, RW_G_LORA), G ** -0.5),
        'rw_g2': normal((NL, RW_G_LORA, G), RW_G_LORA ** -0.5),
        'rw_k_k': near((NL, G), 0.85, 0.05),
        'rw_k_a': near((NL, G), 1.0, 0.05),
        'rw_r_k': normal((NL, RW_HEADS, RW_HEAD), 0.1),
        'rw_ln_w': near((NL, G), 1.0),
        'rw_ln_b': normal((NL, G), 0.02),
        'rw_v0': near((NL - 1, G), 1.0, 0.05),
        'rw_v1': normal((NL - 1, G, RW_V_LORA), G ** -0.5),
        'rw_v2': normal((NL - 1, RW_V_LORA, G), 0.1 * RW_V_LORA ** -0.5),
        'rt_decay': rt_base + normal((NL, 2, RT_HEADS), 0.05),
        'rt_gn_w': near((NL, G), 1.0),
        'moe_router_w': normal((NL, D, E), D ** -0.5),
        'moe_router_b': normal((NL, E), 0.01),
        'moe_w_gu': normal((NL, E, D, 2 * F), D ** -0.5),
        'moe_b_gu': normal((NL, E, 2 * F), 0.02),
        'moe_w_dn': normal((NL, E, F, D), F ** -0.5),
        'moe_b_dn': normal((NL, E, D), 0.02),
        'final_norm_w': near((D,), 1.0),
    }


def reference(x, c, ctx, c_ctx, ada_w, ada_b, norm1_w, norm2_w, w_in, w_out,
              hy_short_w, hy_short_b, hy_f_w1, hy_f_b1, hy_f_w2, hy_f_b2, hy_f_w3, hy_f_b3,
              hy_f_w4, hy_f_freq, hy_bias,
              rg_conv_w, rg_conv_b, rg_wa, rg_ba, rg_wx, rg_bx, rg_lambda,
              rw_mu, rw_w0, rw_w1, rw_w2, rw_a0, rw_a1, rw_a2, rw_g1, rw_g2, rw_k_k, rw_k_a,
              rw_r_k, rw_ln_w, rw_ln_b, rw_v0, rw_v1, rw_v2,
              rt_decay, rt_gn_w,
              moe_router_w, moe_router_b, moe_w_gu, moe_b_gu, moe_w_dn, moe_b_dn,
              final_norm_w):
    f32 = jnp.float32
    dt = x.dtype
    G = D_GROUP
    L = x.shape[1]
    n_ctx = ctx.shape[1]
    ROWS = L // GRID_W
    rows = jnp.repeat(jnp.arange(ROWS, dtype=f32), GRID_W)
    cols = jnp.tile(jnp.arange(GRID_W, dtype=f32), ROWS)
    cond_lat = jax.nn.silu(c.astype(f32))[:, None, :]
    cond_ctx = jax.nn.silu(c_ctx.astype(f32))[None, None, :]
    xl, xc = x, ctx.astype(dt)
    vf_c = vf_l = None

    for l in range(DEPTH):
        last = l == DEPTH - 1
        aw = ada_w[l].astype(f32)
        mod_l = jnp.split((cond_lat @ aw + ada_b[l]).astype(dt), N_MOD, axis=-1)
        mod_c = jnp.split((cond_ctx @ aw + ada_b[l]).astype(dt), N_MOD, axis=-1)

        hl = modulate(xl, norm1_w[l], mod_l[0], mod_l[1])
        hc = modulate(xc, norm1_w[l], mod_c[0], mod_c[1])
        ul = (hl @ w_in[l]).astype(f32)
        uc = (hc @ w_in[l]).astype(f32)
        hy_p = (hy_short_w[l], hy_short_b[l], hy_f_w1[l], hy_f_b1[l], hy_f_w2[l], hy_f_b2[l],
                hy_f_w3[l], hy_f_b3[l], hy_f_w4[l], hy_f_freq[l], hy_bias[l])
        hy_l = hyena_mixer(ul[..., : 3 * G], *hy_p)
        rg_c, rg_l = rglru_mixer(uc[..., 3 * G: 5 * G], ul[..., 3 * G: 5 * G],
                                 rg_conv_w[l], rg_conv_b[l], rg_wa[l], rg_ba[l],
                                 rg_wx[l], rg_bx[l], rg_lambda[l])
        vmix = None if l == 0 else (rw_v0[l - 1], rw_v1[l - 1], rw_v2[l - 1])
        rw_c, rw_l, vf_c, vf_l = rwkv_mixer(uc[..., 5 * G: 9 * G], ul[..., 5 * G: 9 * G],
                                            vf_c, vf_l, vmix, rw_mu[l], rw_w0[l], rw_w1[l],
                                            rw_w2[l], rw_a0[l], rw_a1[l], rw_a2[l], rw_g1[l],
                                            rw_g2[l], rw_k_k[l], rw_k_a[l], rw_r_k[l],
                                            rw_ln_w[l], rw_ln_b[l])
        rt_c, rt_l = retention_mixer(uc[..., 9 * G:], ul[..., 9 * G:], rows, cols,
                                     rt_decay[l], rt_gn_w[l])
        yl = jnp.concatenate([hy_l, rg_l, rw_l, rt_l], axis=-1).astype(dt) @ w_out[l]
        xl = xl + mod_l[2] * yl
        if not last:
            hy_c = hyena_mixer(uc[..., : 3 * G], *hy_p)
            yc = jnp.concatenate([hy_c, rg_c, rw_c, rt_c], axis=-1).astype(dt) @ w_out[l]
            xc = xc + mod_c[2] * yc

        moe_p = (moe_router_w[l], moe_router_b[l], moe_w_gu[l], moe_b_gu[l],
                 moe_w_dn[l], moe_b_dn[l])
        hl = modulate(xl, norm2_w[l], mod_l[3], mod_l[4])
        if last:
            xl = xl + mod_l[5] * moe(hl, *moe_p)
        else:
            hc = modulate(xc, norm2_w[l], mod_c[3], mod_c[4])
            f = moe(jnp.concatenate([hc, hl], axis=1), *moe_p)
            xc = xc + mod_c[5] * f[:, :n_ctx]
            xl = xl + mod_l[5] * f[:, n_ctx:]

    return rms_norm(xl, final_norm_w)
```

```python
import numpy as np
import concourse.bass as bass
import concourse.mybir as mybir
from contextlib import ExitStack

F32 = mybir.dt.float32
BF16 = mybir.dt.bfloat16
I32 = mybir.dt.int32
U32 = mybir.dt.uint32
AF = mybir.ActivationFunctionType
ALU = mybir.AluOpType
AX = mybir.AxisListType

ENGS = ("pe", "act", "dve", "pool", "sp")
N_DMA_SLOTS = 8


class Prog:
    def __init__(self):
        self.nc = bass.Bass("TRN2", target_bir_lowering=False)
        self.stack = ExitStack()
        self.ops = {e: [] for e in ENGS}
        self.count = {e: 0 for e in ENGS}
        self.last_write = {}
        self.readers = {}
        self.waited = {e: {} for e in ENGS}
        self.dma_slot_uses = {}
        self.dma_rr = {e: 0 for e in ENGS}
        self.sem_names = set()
        self.psum_keys = set()

    def dram(self, name, shape, dtype=F32, kind="Internal"):
        return self.nc.dram_tensor(name, list(shape), dtype, kind=kind).ap()

    def inp(self, name, shape, dtype=F32):
        return self.dram(name, shape, dtype, "ExternalInput")

    def out(self, name, shape, dtype=F32):
        return self.dram(name, shape, dtype, "ExternalOutput")

    def sb(self, name, shape, dtype=F32):
        return self.stack.enter_context(self.nc.sbuf_tensor(name, list(shape), dtype))

    def ps(self, name, shape, dtype=F32):
        self.psum_keys.add(name)
        return self.stack.enter_context(self.nc.psum_tensor(name, list(shape), dtype))

    def _deps(self, eng, reads, writes):
        toks = set()
        for k in reads:
            t = self.last_write.get(k)
            if t is not None:
                toks.add(t)
            if k in self.psum_keys:
                for t in self.readers.get(k, ()):
                    if t[2] != eng:
                        toks.add(t)
        for k in writes:
            t = self.last_write.get(k)
            if t is not None:
                toks.add(t)
            for t in self.readers.get(k, ()):
                toks.add(t)
        waits = []
        for (sem, val, src_eng) in toks:
            if src_eng == "pe" and eng == "pe":
                continue
            if self.waited[eng].get(sem, 0) >= val:
                continue
            waits.append((sem, val))
        best = {}
        for sem, val in waits:
            best[sem] = max(best.get(sem, 0), val)
        for sem, val in best.items():
            self.waited[eng][sem] = val
        return list(best.items())

    def _commit(self, tok, reads, writes):
        for k in writes:
            self.last_write[k] = tok
            self.readers[k] = []
        for k in reads:
            self.readers.setdefault(k, []).append(tok)

    def op(self, eng, fn, reads=(), writes=()):
        reads = list(reads); writes = list(writes)
        waits = self._deps(eng, reads, writes)
        self.count[eng] += 1
        sem = "c_" + eng
        self.sem_names.add(sem)
        tok = (sem, self.count[eng], eng)
        self.ops[eng].append((fn, waits, (sem, 1)))
        self._commit(tok, reads, writes)

    def dma(self, eng, out, in_, reads=(), writes=(), **kw):
        reads = list(reads); writes = list(writes)
        slot = self.dma_rr[eng] % N_DMA_SLOTS
        self.dma_rr[eng] += 1
        sem = "d_%s_%d" % (eng, slot)
        self.sem_names.add(sem)
        uses = self.dma_slot_uses.get((eng, slot), 0)
        waits = dict(self._deps(eng, reads, writes))
        if uses > 0 and self.waited[eng].get(sem, 0) < 16 * uses:
            waits[sem] = 16 * uses
            self.waited[eng][sem] = 16 * uses
        uses += 1
        self.dma_slot_uses[(eng, slot)] = uses
        tok = (sem, 16 * uses, "dma")

        def fn(e, out=out, in_=in_, kw=kw):
            return e.dma_start(out=out, in_=in_, **kw)
        self.ops[eng].append((fn, list(waits.items()), (sem, 16)))
        self._commit(tok, reads, writes)

    def finish(self, final_keys):
        waits = self._deps("sp", list(final_keys), [])
        nc = self.nc
        sems = {}
        for name in sorted(self.sem_names):
            sems[name] = self.stack.enter_context(nc.semaphore(name))
        engmap = {"pe": "tensor", "act": "scalar", "dve": "vector", "pool": "gpsimd", "sp": "sync"}
        with nc.Block() as block:
            for eng in ENGS:
                oplist = self.ops[eng]
                extra = waits if eng == "sp" else []
                if not oplist and not extra:
                    continue

                def body(e, oplist=oplist, extra=extra):
                    for fn, ws, inc in oplist:
                        for sem, val in ws:
                            e.wait_ge(sems[sem], val)
                        ins = fn(e)
                        ins.then_inc(sems[inc[0]], inc[1])
                    for sem, val in extra:
                        e.wait_ge(sems[sem], val)
                getattr(block, engmap[eng])(body)
        self.stack.close()
        return nc

    def n_instr(self):
        return {e: len(self.ops[e]) for e in ENGS}

from concourse.bass_utils import run_bass_kernel_spmd

D = 2048
NCORES = 8
EPS = 1e-6


def run(nc, in_maps):
    res = run_bass_kernel_spmd(nc, in_maps, core_ids=list(range(len(in_maps))))
    return res.results


def build_mod():
    P = Prog()
    condT = P.inp("condT", [128, 16, 5])
    aw = P.inp("aw", [2, D, 1536])
    ab = P.inp("ab", [2, 1, 1536])
    mod = P.out("mod", [2, 5, 1536])
    ct = P.sb("ct", [128, 16, 5]); sg = P.sb("sg", [128, 16, 5]); awt = P.sb("awt", [128, 16, 1536])
    abt = P.sb("abt", [1, 2, 1536]); ones = P.sb("ones", [1, 8]); ot = P.sb("ot", [5, 2, 1536])
    ps = [P.ps("ps%d" % i, [5, 512]) for i in range(2)]
    P.dma("sp", ct[:], condT[:, :, :], writes=["ct"])
    P.dma("sp", abt[:], ab.rearrange("l o n -> o l n"), writes=["abt"])
    P.op("dve", lambda e: e.memset(ones[:], 1.0), writes=["ones"])
    P.op("act", lambda e: e.activation(out=sg[:], in_=ct[:], func=AF.Sigmoid), reads=["ct"], writes=["sg"])
    P.op("dve", lambda e: e.tensor_tensor(out=ct[:], in0=ct[:], in1=sg[:], op=ALU.mult), reads=["ct", "sg"], writes=["ct"])
    k = 0
    for l in range(2):
        P.dma("sp", awt[:], aw[l].rearrange("(kc p) n -> p kc n", p=128), writes=["awt"])
        for n in range(3):
            pp = ps[k % 2]; key = "ps%d" % (k % 2); k += 1
            for kc in range(16):
                P.op("pe", lambda e, pp=pp, kc=kc, n=n: e.matmul(pp[:], ct[:, kc, :], awt[:, kc, n * 512:(n + 1) * 512], start=(kc == 0), stop=False),
                     reads=["ct", "awt"], writes=[key])
            P.op("pe", lambda e, pp=pp, l=l, n=n: e.matmul(pp[:], ones[:, 0:5], abt[:, l, n * 512:(n + 1) * 512], start=False, stop=True),
                 reads=["ones", "abt"], writes=[key])
            P.op("dve", lambda e, pp=pp, l=l, n=n: e.tensor_copy(out=ot[:, l, n * 512:(n + 1) * 512], in_=pp[:]), reads=[key], writes=["ot"])
    P.dma("sp", mod.rearrange("l r n -> r l n"), ot[:], reads=["ot"], writes=["mod"])
    return P.finish(["mod"])


def launch_mod(c, c_ctx, ada_w, ada_b):
    cond = np.concatenate([c, c_ctx[None]], 0)
    condT = np.ascontiguousarray(cond.T.reshape(16, 128, 5).transpose(1, 0, 2))
    nc = build_mod()
    maps = []
    for i in range(NCORES):
        maps.append({"condT": condT,
                     "aw": np.ascontiguousarray(ada_w[:, :, i * 1536:(i + 1) * 1536]),
                     "ab": np.ascontiguousarray(ada_b[:, None, i * 1536:(i + 1) * 1536])})
    res = run(nc, maps)
    return np.concatenate([r["mod"] for r in res], axis=2)


NT = 17
DIN = 6656


def emit_norm_mod(P, xt_ap, xkey, A, S, which, hb, hkey, tmp, sq, ss, rs, tmpk="tmp"):
    P.op("act", lambda e: e.activation(out=sq[:], in_=xt_ap, func=AF.Square, accum_out=ss[:]), reads=[xkey], writes=["sq", "ss"])
    P.op("dve", lambda e: e.tensor_scalar(out=rs[:], in0=ss[:], scalar1=1.0 / D, scalar2=EPS, op0=ALU.mult, op1=ALU.add), reads=["ss"], writes=["rs"])
    P.op("act", lambda e: e.activation(out=rs[:], in_=rs[:], func=AF.Sqrt), reads=["rs"], writes=["rs"])
    P.op("dve", lambda e: e.reciprocal(out=rs[:], in_=rs[:]), reads=["rs"], writes=["rs"])
    P.op("dve", lambda e: e.scalar_tensor_tensor(out=tmp[:], in0=xt_ap, scalar=rs[:, 0:1], in1=A[:, which, :], op0=ALU.mult, op1=ALU.mult),
         reads=[xkey, "rs", "A"], writes=[tmpk])
    P.op("pool", lambda e: e.tensor_tensor(out=hb, in0=tmp[:], in1=S[:, which, :], op=ALU.add), reads=[tmpk, "S"], writes=[hkey])


def emit_AS(P, msh, nw, A, S, nwb, nwbk="nwb"):
    P.dma("sp", nwb[:], nw[0:1, :].partition_broadcast(128), writes=[nwbk])
    for wch in range(2):
        P.dma("sp", S[:, wch, :], msh[wch, 0:1, :].partition_broadcast(128), writes=["S"])
        P.dma("sp", A[:, wch, :], msh[wch, 1:2, :].partition_broadcast(128), writes=["A"])
    P.op("dve", lambda e: e.scalar_tensor_tensor(out=A[:, 0, :], in0=A[:, 0, :], scalar=1.0, in1=nwb[:], op0=ALU.add, op1=ALU.mult), reads=["A", nwbk], writes=["A"])
    P.op("dve", lambda e: e.scalar_tensor_tensor(out=A[:, 1, :], in0=A[:, 1, :], scalar=1.0, in1=nwb[:], op0=ALU.add, op1=ALU.mult), reads=["A", nwbk], writes=["A"])


def emit_transpose_tile(P, hb, hkey, hT, t, ident, pst, pstkeys, cnt):
    for g in range(4):
        i = cnt[0] % len(pst); cnt[0] += 1
        pt = pst[i]; pk = pstkeys[i]
        for j in range(4):
            kc = g * 4 + j
            P.op("pe", lambda e, pt=pt, j=j, kc=kc: e.transpose(pt[:, j, :], hb[:, kc * 128:(kc + 1) * 128], ident[:]), reads=[hkey, "ident"], writes=[pk])
        eng = "act" if g % 2 == 0 else "dve"
        if eng == "act":
            P.op("act", lambda e, pt=pt, g=g: e.activation(out=hT[:, g * 4:(g + 1) * 4, t * 128:(t + 1) * 128], in_=pt[:], func=AF.Copy), reads=[pk], writes=[("hT", t)])
        else:
            P.op("dve", lambda e, pt=pt, g=g: e.tensor_copy(out=hT[:, g * 4:(g + 1) * 4, t * 128:(t + 1) * 128], in_=pt[:]), reads=[pk], writes=[("hT", t)])


def emit_ident(P, ident, dtype_tmp=None):
    P.op("pool", lambda e: e.memset(ident[:], 1.0), writes=["ident"])
    P.op("pool", lambda e: e.affine_select(out=ident[:], in_=ident[:], pattern=[[-1, 128]], compare_op=ALU.is_equal, fill=0.0, base=0, channel_multiplier=1),
         reads=["ident"], writes=["ident"])


def emit_combine(P, x_, xk, t, parts, g5b, which, pt_tiles, cnt):
    acc = pt_tiles[-1]
    nl = len(pt_tiles) - 1
    for k, pa in enumerate(parts):
        if k == 0:
            P.dma("act", acc[:], pa[t], writes=["pacc"])
        else:
            pt = pt_tiles[cnt[0] % nl]; pk = "ptl%d" % (cnt[0] % nl); cnt[0] += 1
            P.dma("act" if k % 2 else "sp", pt[:], pa[t], writes=[pk])
            P.op("pool", lambda e, pt=pt: e.tensor_tensor(out=acc[:], in0=acc[:], in1=pt[:], op=ALU.add), reads=["pacc", pk], writes=["pacc"])
    P.op("dve", lambda e: e.tensor_tensor(out=acc[:], in0=acc[:], in1=g5b[:, which, :], op=ALU.mult), reads=["pacc", "g5b"], writes=["pacc"])
    P.op("dve", lambda e: e.tensor_tensor(out=x_[:], in0=x_[:], in1=acc[:], op=ALU.add), reads=["pacc", xk], writes=[xk])


def build_win(npart=0):
    P = Prog()
    xt = P.inp("xt", [NT, 128, D]); msh = P.inp("msh", [2, 2, D]); nw = P.inp("nw", [1, D]); w = P.inp("w", [D, DIN])
    u = P.out("u", [NT, 128, DIN])
    if npart:
        parts = [P.inp("part%d" % k, [NT, 128, D]) for k in range(npart)]
        g5 = P.inp("g5", [2, D]); xo = P.out("xo", [NT, 128, D])
        g5b = P.sb("g5b", [128, 2, D]); pt_tiles = [P.sb("ptl%d" % i, [128, D]) for i in range(1)] + [P.sb("pacc", [128, D])]
        for wch in range(2):
            P.dma("sp", g5b[:, wch, :], g5[wch:wch + 1, :].partition_broadcast(128), writes=["g5b"])
        ccnt = [0]
    A = P.sb("A", [128, 2, D]); S = P.sb("S", [128, 2, D])
    xs = [P.sb("xs%d" % i, [128, D]) for i in range(2)]
    sq = P.sb("sq", [128, D]); ss = P.sb("ss", [128, 1]); rs = P.sb("rs", [128, 1])
    tmp = sq; nwb = sq
    hb = [P.sb("hb%d" % i, [128, D], BF16) for i in range(2)]
    hT = P.sb("hT", [128, 16, NT * 128], BF16)
    ident = P.sb("ident", [128, 128], BF16)
    wb = [P.sb("wb%d" % i, [128, 16, 512], BF16) for i in range(2)]
    ob = [P.sb("ob%d" % i, [128, 512]) for i in range(3)]
    pst = [P.ps("pst%d" % i, [128, 4, 128], BF16) for i in range(2)]
    psm = [P.ps("psm%d" % i, [128, 512]) for i in range(4)]
    emit_ident(P, ident)
    emit_AS(P, msh, nw, A, S, nwb, "sq")
    cnt = [0]
    for t in range(NT):
        x_ = xs[t % 2]; xk = "xs%d" % (t % 2); h_ = hb[t % 2]; hk = "hb%d" % (t % 2)
        P.dma("sp", x_[:], xt[t], writes=[xk])
        if npart:
            emit_combine(P, x_, xk, t, parts, g5b, 0 if t == 0 else 1, pt_tiles, ccnt)
            P.dma("sp", xo[t], x_[:], reads=[xk], writes=["xo"])
        emit_norm_mod(P, x_[:], xk, A, S, 0 if t == 0 else 1, h_[:], hk, tmp, sq, ss, rs, "sq")
        emit_transpose_tile(P, h_, hk, hT, t, ident, pst, ["pst0", "pst1"], cnt)
    k = 0
    for n in range(13):
        wt = wb[n % 2]; wk = "wb%d" % (n % 2)
        P.dma("pool", wt[:], w[:, n * 512:(n + 1) * 512].rearrange("(kc p) n -> p kc n", p=128), writes=[wk])
        for t in range(NT):
            pp = psm[k % 4]; pk = "psm%d" % (k % 4); o_ = ob[k % 3]; ok = "ob%d" % (k % 3)
            for kc in range(16):
                P.op("pe", lambda e, pp=pp, kc=kc, t=t, wt=wt: e.matmul(pp[:], hT[:, kc, t * 128:(t + 1) * 128], wt[:, kc, :], start=(kc == 0), stop=(kc == 15)),
                     reads=[("hT", t), wk], writes=[pk])
            if k % 2 == 0:
                P.op("act", lambda e, pp=pp, o_=o_: e.activation(out=o_[:], in_=pp[:], func=AF.Copy), reads=[pk], writes=[ok])
            else:
                P.op("dve", lambda e, pp=pp, o_=o_: e.tensor_copy(out=o_[:], in_=pp[:]), reads=[pk], writes=[ok])
            P.dma("sp", u[t, :, n * 512:(n + 1) * 512], o_[:], reads=[ok], writes=["u"])
            k += 1
    return P.finish(["u"] + (["xo"] if npart else []))


def tile_split(lat, ctxv):
    outs = []
    for c in range(NCORES):
        b, h = c // 2, c % 2
        ct = ctxv[b, h * 128:(h + 1) * 128][None]
        lt = lat[b, h * 2048:(h + 1) * 2048].reshape(16, 128, -1)
        outs.append(np.ascontiguousarray(np.concatenate([ct, lt], 0)))
    return outs


def tile_merge(parts):
    X = parts[0].shape[-1]
    lat = np.empty((4, 4096, X), parts[0].dtype); ctxv = np.empty((4, 256, X), parts[0].dtype)
    for c in range(NCORES):
        b, h = c // 2, c % 2
        ctxv[b, h * 128:(h + 1) * 128] = parts[c][0]
        lat[b, h * 2048:(h + 1) * 2048] = parts[c][1:].reshape(2048, X)
    return lat, ctxv


def launch_win(xts, mod_l, w_in_l, nw_l, parts=None, mod_prev=None):
    npart = 0 if parts is None else len(parts[0])
    nc = build_win(npart)
    m6 = mod_l.reshape(5, 6, D)
    maps = []
    for c in range(NCORES):
        b = c // 2
        msh = np.ascontiguousarray(np.stack([m6[4, 0:2], m6[b, 0:2]], 0))
        m = {"xt": xts[c], "msh": msh, "nw": np.ascontiguousarray(nw_l[None]), "w": w_in_l}
        if npart:
            p6 = mod_prev.reshape(5, 6, D)
            m["g5"] = np.ascontiguousarray(np.stack([p6[4, 5], p6[b, 5]], 0))
            for k in range(npart):
                m["part%d" % k] = parts[c][k]
        maps.append(m)
    res = run(nc, maps)
    ul, uc = tile_merge([r["u"] for r in res])
    xo = [r["xo"] for r in res] if npart else xts
    return ul, uc, xo


def rev(ap):
    a = [list(x) for x in ap.ap]
    assert len(a) == 2
    st, n = a[1]
    return bass.AP(ap.tensor, ap.offset + st * (n - 1), [a[0], [-st, n]])


def dview(ap, d):
    return ap if d == 0 else rev(ap)


TS = 4352
SEGS = ((0, 256), (256, 4352))


def build_rglru():
    P = Prog()
    xT = P.inp("xT", [4, 64, TS]); gT = P.inp("gT", [4, 64, TS])
    cw = P.inp("cw", [64, 2, 4]); vec = P.inp("vec", [64, 2, 4])
    wa = P.inp("wa", [2, 64, 64]); wx = P.inp("wx", [2, 64, 64])
    oT = P.out("oT", [4, 64, TS])
    cwt = P.sb("cwt", [64, 2, 4]); vt = P.sb("vt", [64, 2, 4]); wat = P.sb("wat", [64, 2, 64]); wxt = P.sb("wxt", [64, 2, 64])
    cl = P.sb("cl", [64, 2]); tl = P.sb("tl", [64, 2])
    x = P.sb("x", [64, TS]); g = P.sb("g", [64, TS]); xc = P.sb("xc", [64, TS]); r = P.sb("r", [64, TS]); ii = P.sb("ii", [64, TS])
    a = P.sb("a", [64, TS]); h0 = P.sb("h0", [64, TS]); h1 = P.sb("h1", [64, TS])
    ps = [P.ps("ps%d" % i, [64, 512]) for i in range(4)]
    P.dma("sp", cwt[:], cw[:, :, :], writes=["cwt"]); P.dma("sp", vt[:], vec[:, :, :], writes=["vt"])
    P.dma("sp", wat[:], wa.rearrange("d i j -> i d j"), writes=["wat"]); P.dma("sp", wxt[:], wx.rearrange("d i j -> i d j"), writes=["wxt"])
    P.op("act", lambda e: e.activation(out=tl[:], in_=vt[:, :, 3], func=AF.Exp, scale=-1.0), reads=["vt"], writes=["tl"])
    P.op("act", lambda e: e.activation(out=tl[:], in_=tl[:], func=AF.Ln, bias=1.0), reads=["tl"], writes=["tl"])
    P.op("dve", lambda e: e.tensor_scalar(out=cl[:], in0=tl[:], scalar1=-8.0, scalar2=None, op0=ALU.mult), reads=["tl"], writes=["cl"])
    k = 0
    for b in range(4):
        P.dma("sp", x[:], xT[b], writes=["x"]); P.dma("sp", g[:], gT[b], writes=["g"])
        for d in range(2):
            h = h0 if d == 0 else h1; hk = "h%d" % d
            P.op("dve", lambda e, d=d: e.tensor_scalar(out=xc[:], in0=x[:], scalar1=cwt[:, d, 3:4], scalar2=vt[:, d, 0:1], op0=ALU.mult, op1=ALU.add),
                 reads=["x", "cwt", "vt"], writes=["xc"])
            for (s0, s1) in SEGS:
                n = s1 - s0
                for j in range(3):
                    sh = 3 - j
                    xo = dview(xc[:, s0:s1], d); xi = dview(x[:, s0:s1], d)
                    P.op("dve", lambda e, xo=xo, xi=xi, sh=sh, n=n, d=d, j=j: e.scalar_tensor_tensor(out=xo[:, sh:n], in0=xi[:, 0:n - sh], scalar=cwt[:, d, j:j + 1], in1=xo[:, sh:n], op0=ALU.mult, op1=ALU.add),
                         reads=["x", "xc", "cwt"], writes=["xc"])
            for ci in range(0, TS, 512):
                ce = min(TS, ci + 512); w_ = ce - ci
                pa = ps[k % 4]; pak = "ps%d" % (k % 4); k += 1
                px = ps[k % 4]; pxk = "ps%d" % (k % 4); k += 1
                P.op("pe", lambda e, pa=pa, ci=ci, ce=ce, w_=w_, d=d: e.matmul(pa[:, 0:w_], wat[:, d, :], xc[:, ci:ce], start=True, stop=True), reads=["wat", "xc"], writes=[pak])
                P.op("pe", lambda e, px=px, ci=ci, ce=ce, w_=w_, d=d: e.matmul(px[:, 0:w_], wxt[:, d, :], xc[:, ci:ce], start=True, stop=True), reads=["wxt", "xc"], writes=[pxk])
                P.op("act", lambda e, pa=pa, ci=ci, ce=ce, w_=w_, d=d: e.activation(out=r[:, ci:ce], in_=pa[:, 0:w_], func=AF.Sigmoid, bias=vt[:, d, 1:2]), reads=[pak, "vt"], writes=["r"])
                P.op("act", lambda e, px=px, ci=ci, ce=ce, w_=w_, d=d: e.activation(out=ii[:, ci:ce], in_=px[:, 0:w_], func=AF.Sigmoid, bias=vt[:, d, 2:3]), reads=[pxk, "vt"], writes=["ii"])
            P.op("act", lambda e, d=d: e.activation(out=a[:], in_=r[:], func=AF.Exp, scale=cl[:, d:d + 1]), reads=["r", "cl"], writes=["a"])
            P.op("dve", lambda e: e.tensor_tensor(out=r[:], in0=a[:], in1=a[:], op=ALU.mult), reads=["a"], writes=["r"])
            P.op("act", lambda e: e.activation(out=r[:], in_=r[:], func=AF.Sqrt, scale=-1.0, bias=1.0), reads=["r"], writes=["r"])
            P.op("dve", lambda e: e.tensor_tensor(out=ii[:], in0=ii[:], in1=xc[:], op=ALU.mult), reads=["ii", "xc"], writes=["ii"])
            P.op("dve", lambda e: e.tensor_tensor(out=r[:], in0=r[:], in1=ii[:], op=ALU.mult), reads=["r", "ii"], writes=["r"])
            hv = dview(h[:, 0:256], d); av = dview(a[:, 0:256], d); bv = dview(r[:, 0:256], d)
            P.op("dve", lambda e, hv=hv, av=av, bv=bv: e.tensor_tensor_scan(out=hv, data0=av, data1=bv, initial=0.0, op0=ALU.mult, op1=ALU.add), reads=["a", "r"], writes=[hk])
            init = h[:, 255:256] if d == 0 else h[:, 0:1]
            hv = dview(h[:, 256:TS], d); av = dview(a[:, 256:TS], d); bv = dview(r[:, 256:TS], d)
            P.op("dve", lambda e, hv=hv, av=av, bv=bv, init=init: e.tensor_tensor_scan(out=hv, data0=av, data1=bv, initial=init, op0=ALU.mult, op1=ALU.add), reads=["a", "r", hk], writes=[hk])
        P.op("act", lambda e: e.activation(out=g[:], in_=g[:], func=AF.Gelu), reads=["g"], writes=["g"])
        P.op("dve", lambda e: e.tensor_tensor(out=h0[:], in0=h0[:], in1=h1[:], op=ALU.add), reads=["h0", "h1"], writes=["h0"])
        P.op("dve", lambda e: e.tensor_tensor(out=h0[:], in0=h0[:], in1=g[:], op=ALU.mult), reads=["h0", "g"], writes=["h0"])
        P.dma("sp", oT[b], h0[:], reads=["h0"], writes=["oT"])
    return P.finish(["oT"])


def scanT(ul, uc, c0, c1):
    return np.ascontiguousarray(np.concatenate([uc[:, :, c0:c1], ul[:, :, c0:c1]], axis=1).transpose(0, 2, 1))


def unscanT(oT):
    o = oT.transpose(0, 2, 1)
    return np.ascontiguousarray(o[:, 256:]), np.ascontiguousarray(o[:, :256])


def launch_rglru(ul, uc, conv_w, conv_b, wa, ba, wx, bx, lam, cores=range(NCORES)):
    G = 512
    nc = build_rglru()
    maps = []
    for c in cores:
        sl = slice(c * 64, (c + 1) * 64)
        maps.append({
            "xT": scanT(ul, uc, 3 * G + c * 64, 3 * G + (c + 1) * 64),
            "gT": scanT(ul, uc, 4 * G + c * 64, 4 * G + (c + 1) * 64),
            "cw": np.ascontiguousarray(conv_w[:, :, sl].transpose(2, 0, 1)),
            "vec": np.ascontiguousarray(np.stack([conv_b[:, sl], ba[:, sl], bx[:, sl], lam[:, sl]], -1).transpose(1, 0, 2)),
            "wa": np.ascontiguousarray(wa[:, c]), "wx": np.ascontiguousarray(wx[:, c])})
    res = run(nc, maps)
    oT = np.concatenate([r["oT"] for r in res], axis=1)
    return unscanT(oT)


def seg_chunks(W):
    out = []
    for si, (s0, s1) in enumerate(SEGS):
        c = s0
        while c < s1:
            out.append((si, c, min(c + W, s1)))
            c += W
    return out


def tau_of(col):
    return 255 - col if col < 256 else 4607 - col


def build_rwkv(debug=False):
    P = Prog()
    NS = TS
    rT = P.inp("rT", [4, 64, NS]); kT = P.inp("kT", [4, 64, NS]); vT = P.inp("vT", [4, 64, NS]); vfT = P.inp("vfT", [4, 64, NS])
    zT = P.inp("zT", [4, 512, NS])
    mu_h = P.inp("mu_h", [64, 2, 3]); mu_z = P.inp("mu_z", [128, 4, 2])
    w1 = P.inp("w1", [2, 512, 64]); w2 = P.inp("w2", [2, 64, 64]); a1 = P.inp("a1", [2, 512, 64]); a2 = P.inp("a2", [2, 64, 64])
    g1 = P.inp("g1", [512, 128]); g2 = P.inp("g2", [128, 64]); v1 = P.inp("v1", [512, 32]); v2 = P.inp("v2", [32, 64])
    hv = P.inp("hv", [64, 10])
    oT = P.out("oT", [4, 64, NS])
    kind = "ExternalOutput" if debug else "Internal"
    X = P.dram("X", [2, NS, 4, 5, 64], F32, kind); Vd = P.dram("Vd", [2, 64, 4, NS], F32, kind)
    Bd = P.dram("Bd", [2, 64, 4, NS], F32, kind); Gd = P.dram("Gd", [64, 4, NS], F32, kind); Yd = P.dram("Yd", [2, 64, 4, NS], F32, kind)

    mh = P.sb("mh", [64, 2, 3]); mh1 = P.sb("mh1", [64, 2, 3]); mz = P.sb("mz", [128, 4, 2]); mz1 = P.sb("mz1", [128, 4, 2])
    w1t = P.sb("w1t", [128, 2, 4, 64]); a1t = P.sb("a1t", [128, 2, 4, 64]); w2t = P.sb("w2t", [64, 2, 64]); a2t = P.sb("a2t", [64, 2, 64])
    g1t = P.sb("g1t", [128, 4, 128]); g2t = P.sb("g2t", [128, 64]); v1t = P.sb("v1t", [128, 4, 32]); v2t = P.sb("v2t", [32, 64])
    hvt = P.sb("hvt", [128, 10]); ones64 = P.sb("ones64", [64, 64]); ident = P.sb("identf", [128, 128])
    blk = P.sb("blk", [128, 128]); F0 = P.sb("F0", [128, 64]); F1 = P.sb("F1", [128, 64])
    P.dma("sp", mh[:], mu_h[:, :, :], writes=["mh"]); P.dma("sp", mz[:], mu_z[:, :, :], writes=["mz"])
    for d in range(2):
        P.dma("sp", w1t[:, d], w1[d].rearrange("(pt p) n -> p pt n", p=128), writes=["w1t"])
        P.dma("sp", a1t[:, d], a1[d].rearrange("(pt p) n -> p pt n", p=128), writes=["a1t"])
        P.dma("sp", w2t[:, d, :], w2[d], writes=["w2t"]); P.dma("sp", a2t[:, d, :], a2[d], writes=["a2t"])
    P.dma("sp", g1t[:], g1.rearrange("(pt p) n -> p pt n", p=128), writes=["g1t"]); P.dma("sp", g2t[:], g2[:, :], writes=["g2t"])
    P.dma("sp", v1t[:], v1.rearrange("(pt p) n -> p pt n", p=128), writes=["v1t"]); P.dma("sp", v2t[:], v2[:, :], writes=["v2t"])
    P.dma("sp", hvt[0:64, :], hv[:, :], writes=["hvt"]); P.dma("sp", hvt[64:128, :], hv[:, :], writes=["hvt"])
    P.op("dve", lambda e: e.tensor_scalar(out=mh1[:], in0=mh[:], scalar1=-1.0, scalar2=1.0, op0=ALU.mult, op1=ALU.add), reads=["mh"], writes=["mh1"])
    P.op("dve", lambda e: e.tensor_scalar(out=mz1[:], in0=mz[:], scalar1=-1.0, scalar2=1.0, op0=ALU.mult, op1=ALU.add), reads=["mz"], writes=["mz1"])
    P.op("dve", lambda e: e.memset(ones64[:], 1.0), writes=["ones64"])
    P.op("pool", lambda e: e.memset(ident[:], 1.0), writes=["identf"])
    P.op("pool", lambda e: e.affine_select(out=ident[:], in_=ident[:], pattern=[[-1, 128]], compare_op=ALU.is_equal, fill=0.0, base=0, channel_multiplier=1), reads=["identf"], writes=["identf"])
    P.op("dve", lambda e: e.memset(blk[:], 0.0), writes=["blk"])
    P.op("dve", lambda e: e.memset(blk[0:64, 0:64], 1.0 / 64), reads=["blk"], writes=["blk"])
    P.op("dve", lambda e: e.memset(blk[64:128, 64:128], 1.0 / 64), reads=["blk"], writes=["blk"])
    P.op("dve", lambda e: e.memset(F0[:], 0.0), writes=["F0"]); P.op("dve", lambda e: e.memset(F1[:], 0.0), writes=["F1"])
    P.op("dve", lambda e: e.tensor_copy(out=F0[0:64, :], in_=ident[0:64, 0:64]), reads=["identf", "F0"], writes=["F0"])
    P.op("dve", lambda e: e.tensor_copy(out=F1[64:128, :], in_=ident[64:128, 64:128]), reads=["identf", "F1"], writes=["F1"])
    W0 = lambda d: hvt[0:64, d:d + 1]
    A0 = lambda d: hvt[0:64, 2 + d:3 + d]
    V0 = hvt[0:64, 4:5]; KK_ = hvt[0:64, 5:6]; KA_ = hvt[0:64, 6:7]; RK_ = hvt[0:64, 7:8]

    zc = P.sb("zc", [128, 4, 512]); rc = P.sb("rc", [64, 512]); kc = P.sb("kc", [64, 512]); vc = P.sb("vc", [64, 512]); vfc = P.sb("vfc", [64, 512])
    hid = P.sb("hid", [128, 512]); h32 = P.sb("h32", [32, 512]); sgv = P.sb("sgv", [64, 512]); gq = P.sb("gq", [64, 512])
    xz = P.sb("xz", [128, 4, 512]); xr = P.sb("xr", [64, 512]); xk = P.sb("xk", [64, 512]); xv = P.sb("xv", [64, 512])
    hw = P.sb("hw", [64, 512]); dec = P.sb("dec", [64, 512]); ha = P.sb("ha", [64, 512]); aa = P.sb("aa", [64, 512])
    kk = P.sb("kk", [64, 512]); t1 = P.sb("t1", [64, 512]); kp = P.sb("kp", [64, 512]); kka = P.sb("kka", [64, 512]); bon = P.sb("bon", [64, 512])
    stg = P.sb("stg", [128, 5, 64]); rvb = P.sb("rvb", [64, 512]); rv5 = P.sb("rv5", [64, 5, 512])
    pA = P.ps("pA", [128, 512]); pB = P.ps("pB", [128, 512]); pT = P.ps("pT", [128, 5, 64])

    def mm_z(ps, keyp, wt, wkey, d, nout, src, skey, W):
        for pt in range(4):
            lhs = wt[:, d, pt, :] if d is not None else wt[:, pt, :]
            P.op("pe", lambda e, lhs=lhs, pt=pt: e.matmul(ps[0:nout, 0:W], lhs, src[:, pt, 0:W], start=(pt == 0), stop=(pt == 3)), reads=[wkey, skey], writes=[keyp])

    def prep_chunk(b, si, c0, c1):
        if True:
            s0, s1 = SEGS[si]; W = c1 - c0; lo = max(c0 - 1, s0); hi = min(c1 + 1, s1); q0 = lo - (c0 - 1); q1 = hi - (c0 - 1)
            WW = W + 2
            for tl, key in ((zc, "zc"), (rc, "rc"), (kc, "kc"), (vc, "vc"), (vfc, "vfc")):
                P.op("pool", lambda e, tl=tl: e.memset(tl[:], 0.0), writes=[key])
            P.dma("sp", zc[:, :, q0:q1], zT[b, :, lo:hi].rearrange("(pt p) n -> p pt n", p=128), reads=[], writes=["zc"])
            P.dma("act", rc[:, q0:q1], rT[b, :, lo:hi], writes=["rc"]); P.dma("act", kc[:, q0:q1], kT[b, :, lo:hi], writes=["kc"])
            P.dma("sp", vc[:, q0:q1], vT[b, :, lo:hi], writes=["vc"]); P.dma("act", vfc[:, q0:q1], vfT[b, :, lo:hi], writes=["vfc"])
            mm_z(pA, "pA", g1t, "g1t", None, 128, zc, "zc", WW)
            P.op("act", lambda e, WW=WW: e.activation(out=hid[:, 0:WW], in_=pA[:, 0:WW], func=AF.Sigmoid), reads=["pA"], writes=["hid"])
            P.op("pe", lambda e, WW=WW: e.matmul(pB[0:64, 0:WW], g2t[:], hid[:, 0:WW], start=True, stop=True), reads=["g2t", "hid"], writes=["pB"])
            P.op("act", lambda e, WW=WW: e.activation(out=gq[:, 0:WW], in_=pB[0:64, 0:WW], func=AF.Copy), reads=["pB"], writes=["gq"])
            P.dma("sp", Gd[:, b, c0:c1], gq[:, 1:W + 1], reads=["gq"], writes=["Gd"])
            mm_z(pA, "pA", v1t, "v1t", None, 32, zc, "zc", WW)
            P.op("act", lambda e, WW=WW: e.activation(out=h32[:, 0:WW], in_=pA[0:32, 0:WW], func=AF.Copy), reads=["pA"], writes=["h32"])
            P.op("pe", lambda e, WW=WW: e.matmul(pB[0:64, 0:WW], v2t[:], h32[:, 0:WW], start=True, stop=True), reads=["v2t", "h32"], writes=["pB"])
            P.op("act", lambda e, WW=WW: e.activation(out=sgv[:, 0:WW], in_=pB[0:64, 0:WW], func=AF.Sigmoid, bias=V0), reads=["pB", "hvt"], writes=["sgv"])
            P.op("dve", lambda e: e.tensor_tensor(out=vfc[:], in0=vfc[:], in1=vc[:], op=ALU.subtract), reads=["vfc", "vc"], writes=["vfc"])
            P.op("dve", lambda e: e.tensor_tensor(out=vfc[:], in0=vfc[:], in1=sgv[:], op=ALU.mult), reads=["vfc", "sgv"], writes=["vfc"])
            P.op("dve", lambda e: e.tensor_tensor(out=vc[:], in0=vc[:], in1=vfc[:], op=ALU.add), reads=["vfc", "vc"], writes=["vc"])
            for d in range(2):
                nb = 0 if d == 0 else 2
                for j, (src, skey, dst, dkey) in enumerate(((rc, "rc", xr, "xr"), (kc, "kc", xk, "xk"), (vc, "vc", xv, "xv"))):
                    P.op("dve", lambda e, src=src, dst=dst, d=d, j=j: e.tensor_scalar(out=dst[:, 0:W], in0=src[:, 1:W + 1], scalar1=mh1[:, d, j:j + 1], scalar2=None, op0=ALU.mult),
                         reads=[skey, "mh1"], writes=[dkey])
                    P.op("dve", lambda e, src=src, dst=dst, d=d, j=j, nb=nb: e.scalar_tensor_tensor(out=dst[:, 0:W], in0=src[:, nb:nb + W], scalar=mh[:, d, j:j + 1], in1=dst[:, 0:W], op0=ALU.mult, op1=ALU.add),
                         reads=[skey, "mh", dkey], writes=[dkey])
                for pt in range(4):
                    P.op("pool", lambda e, pt=pt, d=d: e.tensor_scalar(out=xz[:, pt, 0:W], in0=zc[:, pt, 1:W + 1], scalar1=mz1[:, pt, d:d + 1], scalar2=None, op0=ALU.mult),
                         reads=["zc", "mz1"], writes=["xz"])
                    P.op("dve", lambda e, pt=pt, d=d, nb=nb: e.scalar_tensor_tensor(out=xz[:, pt, 0:W], in0=zc[:, pt, nb:nb + W], scalar=mz[:, pt, d:d + 1], in1=xz[:, pt, 0:W], op0=ALU.mult, op1=ALU.add),
                         reads=["zc", "mz", "xz"], writes=["xz"])
                mm_z(pA, "pA", w1t, "w1t", d, 64, xz, "xz", W)
                P.op("act", lambda e: e.activation(out=hw[:, 0:W], in_=pA[0:64, 0:W], func=AF.Tanh), reads=["pA"], writes=["hw"])
                P.op("pe", lambda e, d=d: e.matmul(pB[0:64, 0:W], w2t[:, d, :], hw[:, 0:W], start=True, stop=True), reads=["w2t", "hw"], writes=["pB"])
                P.op("act", lambda e, d=d: e.activation(out=dec[:, 0:W], in_=pB[0:64, 0:W], func=AF.Sigmoid, bias=W0(d)), reads=["pB", "hvt"], writes=["dec"])
                P.op("act", lambda e: e.activation(out=dec[:, 0:W], in_=dec[:, 0:W], func=AF.Exp, scale=-float(np.exp(-0.5))), reads=["dec"], writes=["dec"])
                mm_z(pA, "pA", a1t, "a1t", d, 64, xz, "xz", W)
                P.op("act", lambda e: e.activation(out=ha[:, 0:W], in_=pA[0:64, 0:W], func=AF.Copy), reads=["pA"], writes=["ha"])
                P.op("pe", lambda e, d=d: e.matmul(pB[0:64, 0:W], a2t[:, d, :], ha[:, 0:W], start=True, stop=True), reads=["a2t", "ha"], writes=["pB"])
                P.op("act", lambda e, d=d: e.activation(out=aa[:, 0:W], in_=pB[0:64, 0:W], func=AF.Sigmoid, bias=A0(d)), reads=["pB", "hvt"], writes=["aa"])
                P.op("dve", lambda e: e.tensor_scalar(out=kk[:, 0:W], in0=xk[:, 0:W], scalar1=KK_, scalar2=None, op0=ALU.mult), reads=["xk", "hvt"], writes=["kk"])
                P.op("dve", lambda e: e.tensor_tensor(out=t1[:, 0:W], in0=kk[:, 0:W], in1=kk[:, 0:W], op=ALU.mult), reads=["kk"], writes=["t1"])
                P.op("pe", lambda e: e.matmul(pA[0:64, 0:W], ones64[:], t1[:, 0:W], start=True, stop=True), reads=["ones64", "t1"], writes=["pA"])
                P.op("act", lambda e: e.activation(out=t1[:, 0:W], in_=pA[0:64, 0:W], func=AF.Sqrt), reads=["pA"], writes=["t1"])
                P.op("dve", lambda e: e.tensor_scalar(out=t1[:, 0:W], in0=t1[:, 0:W], scalar1=1e-12, scalar2=None, op0=ALU.max), reads=["t1"], writes=["t1"])
                P.op("dve", lambda e: e.reciprocal(out=t1[:, 0:W], in_=t1[:, 0:W]), reads=["t1"], writes=["t1"])
                P.op("dve", lambda e: e.tensor_tensor(out=kk[:, 0:W], in0=kk[:, 0:W], in1=t1[:, 0:W], op=ALU.mult), reads=["kk", "t1"], writes=["kk"])
                P.op("dve", lambda e: e.tensor_scalar(out=kp[:, 0:W], in0=aa[:, 0:W], scalar1=-1.0, scalar2=KA_, op0=ALU.add, op1=ALU.mult), reads=["aa", "hvt"], writes=["kp"])
                P.op("dve", lambda e: e.scalar_tensor_tensor(out=kp[:, 0:W], in0=kp[:, 0:W], scalar=1.0, in1=xk[:, 0:W], op0=ALU.add, op1=ALU.mult), reads=["kp", "xk"], writes=["kp"])
                P.op("dve", lambda e: e.tensor_tensor(out=kka[:, 0:W], in0=kk[:, 0:W], in1=aa[:, 0:W], op=ALU.mult), reads=["kk", "aa"], writes=["kka"])
                P.op("dve", lambda e: e.scalar_tensor_tensor(out=t1[:, 0:W], in0=xr[:, 0:W], scalar=RK_, in1=kp[:, 0:W], op0=ALU.mult, op1=ALU.mult), reads=["xr", "kp", "hvt", "t1"], writes=["t1"])
                P.op("pe", lambda e: e.matmul(pB[0:64, 0:W], ones64[:], t1[:, 0:W], start=True, stop=True), reads=["ones64", "t1"], writes=["pB"])
                P.op("dve", lambda e: e.tensor_tensor(out=bon[:, 0:W], in0=pB[0:64, 0:W], in1=xv[:, 0:W], op=ALU.mult), reads=["pB", "xv"], writes=["bon"])
                if d == 0:
                    t_lo = c0
                    vsrc, bsrc = xv, bon
                    P.dma("sp", Vd[0, :, b, t_lo:t_lo + W], xv[:, 0:W], reads=["xv"], writes=["Vd"])
                    P.dma("act", Bd[0, :, b, t_lo:t_lo + W], bon[:, 0:W], reads=["bon"], writes=["Bd"])
                else:
                    t_lo = tau_of(c1 - 1)
                    P.op("pool", lambda e: e.tensor_copy(out=rvb[:, 0:W], in_=rev(xv[:, 0:W])), reads=["xv"], writes=["rvb"])
                    P.dma("sp", Vd[1, :, b, t_lo:t_lo + W], rvb[:, 0:W], reads=["rvb"], writes=["Vd"])
                    P.op("pool", lambda e: e.tensor_copy(out=rvb[:, 0:W], in_=rev(bon[:, 0:W])), reads=["bon", "rvb"], writes=["rvb"])
                    P.dma("sp", Bd[1, :, b, t_lo:t_lo + W], rvb[:, 0:W], reads=["rvb"], writes=["Bd"])
                vecs = ((kk, "kk"), (dec, "dec"), (kka, "kka"), (kp, "kp"), (xr, "xr"))
                if d == 1:
                    for vi, (vt_, vk_) in enumerate(vecs):
                        P.op("pool", lambda e, vt_=vt_, vi=vi: e.tensor_copy(out=rv5[:, vi, 0:W], in_=rev(vt_[:, 0:W])), reads=[vk_], writes=["rv5"])
                for bi in range(0, W, 128):
                    bw = min(128, W - bi)
                    for vi, (vt_, vk_) in enumerate(vecs):
                        if d == 0:
                            src = vt_[:, bi:bi + bw]
                        else:
                            src = rv5[:, vi, bi:bi + bw]; vk_ = "rv5"
                        P.op("pe", lambda e, src=src, vi=vi, bw=bw: e.transpose(pT[0:bw, vi, :], src, ident[0:64, 0:64]), reads=[vk_, "identf"], writes=["pT"])
                    P.op("act", lambda e, bw=bw: e.activation(out=stg[0:bw], in_=pT[0:bw], func=AF.Copy), reads=["pT"], writes=["stg"])
                    P.dma("act", X[d, t_lo + bi:t_lo + bi + bw, b], stg[0:bw], reads=["stg"], writes=["X"])

    for b in range(4):
        for (si, c0, c1) in seg_chunks(510):
            prep_chunk(b, si, c0, c1)

    TT = 8
    Bt = [P.sb("Bt%d" % i, [128, TT, 4, 5, 64]) for i in range(2)]
    Vt = [P.sb("Vt%d" % i, [128, 4, 128]) for i in range(2)]
    Yt = [P.sb("Yt%d" % i, [128, 4, 128]) for i in range(2)]
    S = P.sb("S", [128, 4, 64]); tmp = P.sb("tmpS", [128, 4, 64]); tmp2 = [P.sb("tmpK%d" % i, [128, 4, 64]) for i in range(2)]
    sa = P.sb("sa", [128, 4])
    P.op("dve", lambda e: e.memset(S[:], 0.0), writes=["S"])
    for ch in range(NS // TT):
        t0 = ch * TT
        bt = Bt[ch % 2]; bk = "Bt%d" % (ch % 2)
        for d in range(2):
            srcap = X[d, t0:t0 + TT].rearrange("t b v j -> (t b v j)").partition_broadcast(64)
            P.dma("sp" if d == 0 else "act", bt[d * 64:(d + 1) * 64].rearrange("p t b v j -> p (t b v j)"), srcap, reads=["X"], writes=[bk])
        if t0 % 128 == 0:
            vi_ = (t0 // 128) % 2
            vt_ = Vt[vi_]; vk_ = "Vt%d" % vi_
            P.dma("sp", vt_[:], Vd[:, :, :, t0:t0 + 128].rearrange("d i b t -> (d i) b t"), reads=["Vd"], writes=[vk_])
        yi_ = (t0 // 128) % 2
        yt_ = Yt[yi_]; yk_ = "Yt%d" % yi_
        for tl in range(TT):
            t = t0 + tl; tq = t % 128
            KKv = bt[:, tl, :, 0, :]; Wv = bt[:, tl, :, 1, :]; KKAv = bt[:, tl, :, 2, :]; Kv = bt[:, tl, :, 3, :]; Rv = bt[:, tl, :, 4, :]
            vv_ = vt_[:, :, tq:tq + 1].to_broadcast([128, 4, 64])
            tk = tmp2[t % 2]; tkk = "tmpK%d" % (t % 2)
            P.op("pool", lambda e, tk=tk, Kv=Kv, vv_=vv_: e.tensor_tensor(out=tk[:], in0=Kv, in1=vv_, op=ALU.mult), reads=[bk, vk_], writes=[tkk])
            P.op("dve", lambda e, KKv=KKv: e.tensor_tensor(out=tmp[:], in0=S[:], in1=KKv, op=ALU.mult), reads=["S", bk], writes=["tmpS"])
            P.op("dve", lambda e: e.tensor_reduce(out=sa[:], in_=tmp[:], axis=AX.X, op=ALU.add), reads=["tmpS"], writes=["sa"])
            P.op("dve", lambda e, Wv=Wv: e.tensor_tensor(out=S[:], in0=S[:], in1=Wv, op=ALU.mult), reads=["S", bk], writes=["S"])
            P.op("dve", lambda e, KKAv=KKAv: e.tensor_tensor(out=tmp[:], in0=KKAv, in1=sa[:, :].unsqueeze(2).to_broadcast([128, 4, 64]), op=ALU.mult), reads=["sa", bk, "tmpS"], writes=["tmpS"])
            P.op("dve", lambda e: e.tensor_tensor(out=S[:], in0=S[:], in1=tmp[:], op=ALU.subtract), reads=["S", "tmpS"], writes=["S"])
            P.op("dve", lambda e, tk=tk: e.tensor_tensor(out=S[:], in0=S[:], in1=tk[:], op=ALU.add), reads=["S", tkk], writes=["S"])
            P.op("dve", lambda e, Rv=Rv: e.tensor_tensor(out=tmp[:], in0=S[:], in1=Rv, op=ALU.mult), reads=["S", bk, "tmpS"], writes=["tmpS"])
            P.op("dve", lambda e, yt_=yt_, tq=tq: e.tensor_reduce(out=yt_[:, :, tq], in_=tmp[:], axis=AX.X, op=ALU.add), reads=["tmpS"], writes=[yk_])
        if (t0 + TT) % 128 == 0:
            tb = t0 + TT - 128
            P.dma("sp", Yd[:, :, :, tb:tb + 128].rearrange("d i b t -> (d i) b t"), yt_[:], reads=[yk_], writes=["Yd"])

    yc = P.sb("yc", [128, 512]); bc = P.sb("bc", [128, 512]); cen = P.sb("cen", [128, 512]); sq2 = P.sb("sq2", [128, 512]); zr = P.sb("zr", [128, 512])
    gc = P.sb("gc", [64, 512]); oc = P.sb("oc", [64, 512])
    LNW = hvt[:, 8:9]; LNB = hvt[:, 9:10]
    def post_chunk(b, si, c0, c1):
        if True:
            W = c1 - c0; tl1 = tau_of(c1 - 1)
            P.dma("sp", yc[0:64, 0:W], Yd[0, :, b, c0:c1], reads=["Yd"], writes=["yc"]); P.dma("act", yc[64:128, 0:W], Yd[1, :, b, tl1:tl1 + W], reads=["Yd"], writes=["yc"])
            P.dma("sp", bc[0:64, 0:W], Bd[0, :, b, c0:c1], reads=["Bd"], writes=["bc"]); P.dma("act", bc[64:128, 0:W], Bd[1, :, b, tl1:tl1 + W], reads=["Bd"], writes=["bc"])
            P.dma("sp", gc[:, 0:W], Gd[:, b, c0:c1], reads=["Gd"], writes=["gc"])
            P.op("pe", lambda e: e.matmul(pA[:, 0:W], blk[:], yc[:, 0:W], start=True, stop=True), reads=["blk", "yc"], writes=["pA"])
            P.op("dve", lambda e: e.tensor_tensor(out=cen[:, 0:W], in0=yc[:, 0:W], in1=pA[:, 0:W], op=ALU.subtract), reads=["yc", "pA"], writes=["cen"])
            P.op("act", lambda e: e.activation(out=sq2[:, 0:W], in_=cen[:, 0:W], func=AF.Square), reads=["cen"], writes=["sq2"])
            P.op("pe", lambda e: e.matmul(pB[:, 0:W], blk[:], sq2[:, 0:W], start=True, stop=True), reads=["blk", "sq2"], writes=["pB"])
            P.op("act", lambda e: e.activation(out=sq2[:, 0:W], in_=pB[:, 0:W], func=AF.Sqrt, bias=64e-5), reads=["pB", "sq2"], writes=["sq2"])
            P.op("dve", lambda e: e.reciprocal(out=sq2[:, 0:W], in_=sq2[:, 0:W]), reads=["sq2"], writes=["sq2"])
            P.op("dve", lambda e: e.tensor_tensor(out=cen[:, 0:W], in0=cen[:, 0:W], in1=sq2[:, 0:W], op=ALU.mult), reads=["cen", "sq2"], writes=["cen"])
            P.op("dve", lambda e: e.tensor_scalar(out=cen[:, 0:W], in0=cen[:, 0:W], scalar1=LNW, scalar2=LNB, op0=ALU.mult, op1=ALU.add), reads=["cen", "hvt"], writes=["cen"])
            P.op("dve", lambda e: e.tensor_tensor(out=cen[:, 0:W], in0=cen[:, 0:W], in1=bc[:, 0:W], op=ALU.add), reads=["cen", "bc"], writes=["cen"])
            P.op("pool", lambda e: e.tensor_copy(out=zr[:, 0:W], in_=rev(cen[:, 0:W])), reads=["cen"], writes=["zr"])
            P.op("pe", lambda e: e.matmul(pA[0:64, 0:W], F0[:], cen[:, 0:W], start=True, stop=False), reads=["F0", "cen"], writes=["pA"])
            P.op("pe", lambda e: e.matmul(pA[0:64, 0:W], F1[:], zr[:, 0:W], start=False, stop=True), reads=["F1", "zr"], writes=["pA"])
            P.op("dve", lambda e: e.tensor_tensor(out=oc[:, 0:W], in0=pA[0:64, 0:W], in1=gc[:, 0:W], op=ALU.mult), reads=["pA", "gc"], writes=["oc"])
            P.dma("sp", oT[b, :, c0:c1], oc[:, 0:W], reads=["oc"], writes=["oT"])
    for b in range(4):
        for (si, c0, c1) in seg_chunks(512):
            post_chunk(b, si, c0, c1)
    finals = ["oT"] + (["X", "Vd", "Bd", "Gd", "Yd"] if debug else [])
    return P.finish(finals)


def launch_rwkv(ul, uc, vfl, vfc_, p, layer, cores=range(NCORES), debug=False):
    G = 512
    nc = build_rwkv(debug)
    zT = scanT(ul, uc, 8 * G, 9 * G)
    maps = []
    for c in cores:
        sl = slice(c * 64, (c + 1) * 64)
        m = {"rT": scanT(ul, uc, 5 * G + c * 64, 5 * G + (c + 1) * 64), "kT": scanT(ul, uc, 6 * G + c * 64, 6 * G + (c + 1) * 64),
             "vT": scanT(ul, uc, 7 * G + c * 64, 7 * G + (c + 1) * 64), "zT": zT,
             "vfT": scanT(vfl, vfc_, c * 64, (c + 1) * 64)}
        mu = p["rw_mu"]
        m["mu_h"] = np.ascontiguousarray(mu[:, 0:3, sl].transpose(2, 0, 1))
        m["mu_z"] = np.ascontiguousarray(mu[:, 3, :].reshape(2, 4, 128).transpose(2, 1, 0))
        m["w1"] = np.ascontiguousarray(p["rw_w1"]); m["w2"] = np.ascontiguousarray(p["rw_w2"][:, :, sl])
        m["a1"] = np.ascontiguousarray(p["rw_a1"]); m["a2"] = np.ascontiguousarray(p["rw_a2"][:, :, sl])
        m["g1"] = np.ascontiguousarray(p["rw_g1"]); m["g2"] = np.ascontiguousarray(p["rw_g2"][:, sl])
        m["v1"] = np.ascontiguousarray(p["rw_v1"]); m["v2"] = np.ascontiguousarray(p["rw_v2"][:, sl])
        m["hv"] = np.ascontiguousarray(np.stack([p["rw_w0"][0, sl], p["rw_w0"][1, sl], p["rw_a0"][0, sl], p["rw_a0"][1, sl], p["rw_v0"][sl],
                                                 p["rw_k_k"][sl], p["rw_k_a"][sl], p["rw_r_k"].reshape(-1)[sl], p["rw_ln_w"][sl], p["rw_ln_b"][sl]], -1))
        maps.append(m)
    res = run(nc, maps)
    oT = np.concatenate([r["oT"] for r in res], axis=1)
    if debug:
        return unscanT(oT), res
    return unscanT(oT)


def rt_consts():
    inv = np.power(10000.0, -np.arange(32, dtype=np.float32) / 32).astype(np.float32)
    t = np.arange(4096)
    rows = (t // 64).astype(np.float32); cols = (t % 64).astype(np.float32)
    ang = np.zeros((128, 4096), np.float32)
    for d in range(128):
        pos = rows if d < 64 else cols
        ang[d] = pos * inv[(d % 64) % 32]
    cosT = np.cos(ang).astype(np.float32); sinT = np.sin(ang).astype(np.float32)
    Pi = np.zeros((128, 128), np.float32)
    for m in range(128):
        if m % 64 < 32:
            Pi[m, m + 32] = -1.0
        else:
            Pi[m, m - 32] = 1.0
    j = np.arange(128, dtype=np.float32)[:, None]; i = np.arange(128, dtype=np.float32)[None, :]
    diff = i - j
    c = np.zeros((128, 6, 128), np.float32)
    c[:, 0] = np.maximum(diff, 0); c[:, 1] = (diff >= 0); c[:, 2] = np.maximum(-diff, 0); c[:, 3] = (diff < 0)
    c[:, 4] = i + 1.0 + 0 * j; c[:, 5] = 128.0 - i + 0 * j
    colv = np.zeros((128, 2), np.float32); colv[:, 0] = 127 - np.arange(128); colv[:, 1] = np.arange(128)
    return cosT, sinT, np.ascontiguousarray(Pi.T), c, colv


def build_ret():
    P = Prog()
    NCH = 34
    QT = P.inp("QT", [2, 128, TS]); KT = P.inp("KT", [2, 128, TS]); Vv = P.inp("V", [2, TS, 128]); Gg = P.inp("G", [2, TS, 128])
    dec = P.inp("dec", [2, 2]); gnw = P.inp("gnw", [2, 128])
    cosT = P.inp("cosT", [128, 4096]); sinT = P.inp("sinT", [128, 4096]); PiT = P.inp("PiT", [128, 128]); cc = P.inp("cc", [128, 6, 128]); colv = P.inp("colv", [128, 2])
    out = P.out("out", [2, TS, 128])
    ct = P.sb("ct", [128, 4096]); st = P.sb("st", [128, 4096]); pit = P.sb("pit", [128, 128]); cct = P.sb("cct", [128, 6, 128]); cvt = P.sb("cvt", [128, 2])
    ident = P.sb("identf", [128, 128])
    qt = P.sb("qt", [128, TS]); kt = P.sb("kt", [128, TS]); vt = P.sb("vt", [128, NCH, 128]); gt = P.sb("gt", [128, NCH, 128]); O = P.sb("O", [128, NCH, 128])
    lg = P.sb("lg", [128, 2]); M = P.sb("M", [128, 2, 128]); QD = P.sb("QD", [128, 2, 128]); kd = P.sb("kd", [128, 2]); cd = P.sb("cd", [128, 2])
    gwb = P.sb("gwb", [128, 128])
    S = [P.sb("S%d" % d, [128, 128]) for d in range(2)]
    A0 = P.sb("A0", [128, 128]); A1 = P.sb("A1", [128, 128]); Q0 = P.sb("Q0", [128, 128]); K0 = P.sb("K0", [128, 128]); tmpr = P.sb("tmpr", [128, 512])
    mv = P.sb("mv", [128, 4]); cen = P.sb("cen", [128, 128]); sq = P.sb("sqr", [128, 128]); sg = P.sb("sgr", [128, 128])
    pA = P.ps("pA", [128, 512]); pB = P.ps("pB", [128, 128]); pC = P.ps("pC", [128, 128]); pD = P.ps("pD", [128, 128])
    P.dma("sp", ct[:], cosT[:, :], writes=["ct"]); P.dma("act", st[:], sinT[:, :], writes=["st"]); P.dma("sp", pit[:], PiT[:, :], writes=["pit"])
    P.dma("sp", cct[:], cc[:, :, :], writes=["cct"]); P.dma("sp", cvt[:], colv[:, :], writes=["cvt"])
    P.op("pool", lambda e: e.memset(ident[:], 1.0), writes=["identf"])
    P.op("pool", lambda e: e.affine_select(out=ident[:], in_=ident[:], pattern=[[-1, 128]], compare_op=ALU.is_equal, fill=0.0, base=0, channel_multiplier=1), reads=["identf"], writes=["identf"])

    def pair(pi):
        P.dma("sp", qt[:], QT[pi], writes=["qt"]); P.dma("act", kt[:], KT[pi], writes=["kt"])
        P.dma("sp", vt[:], Vv[pi].rearrange("(n p) e -> p n e", p=128), writes=["vt"]); P.dma("act", gt[:], Gg[pi].rearrange("(n p) e -> p n e", p=128), writes=["gt"])
        P.dma("sp", lg[:], dec[pi:pi + 1, :].partition_broadcast(128), writes=["lg"])
        P.dma("sp", gwb[:], gnw[pi:pi + 1, :].partition_broadcast(128), writes=["gwb"])
        P.op("act", lambda e: e.activation(out=lg[:], in_=lg[:], func=AF.Exp), reads=["lg"], writes=["lg"])
        P.op("act", lambda e: e.activation(out=lg[:], in_=lg[:], func=AF.Ln, bias=1.0), reads=["lg"], writes=["lg"])
        P.op("dve", lambda e: e.tensor_scalar(out=lg[:], in0=lg[:], scalar1=-1.0, scalar2=None, op0=ALU.mult), reads=["lg"], writes=["lg"])
        for d in range(2):
            P.op("act", lambda e, d=d: e.activation(out=M[:, d, :], in_=cct[:, 2 * d, :], func=AF.Exp, scale=lg[:, d:d + 1]), reads=["cct", "lg"], writes=["M"])
            P.op("dve", lambda e, d=d: e.tensor_tensor(out=M[:, d, :], in0=M[:, d, :], in1=cct[:, 2 * d + 1, :], op=ALU.mult), reads=["M", "cct"], writes=["M"])
            P.op("act", lambda e, d=d: e.activation(out=QD[:, d, :], in_=cct[:, 4 + d, :], func=AF.Exp, scale=lg[:, d:d + 1]), reads=["cct", "lg"], writes=["QD"])
            P.op("act", lambda e, d=d: e.activation(out=kd[:, d:d + 1], in_=cvt[:, d:d + 1], func=AF.Exp, scale=lg[:, d:d + 1]), reads=["cvt", "lg"], writes=["kd"])
            P.op("act", lambda e, d=d: e.activation(out=cd[:, d:d + 1], in_=lg[:, d:d + 1], func=AF.Exp, scale=128.0), reads=["lg"], writes=["cd"])
            P.op("dve", lambda e, d=d: e.memset(S[d][:], 0.0), writes=["S%d" % d])
        P.op("act", lambda e: e.activation(out=kt[:], in_=kt[:], func=AF.Copy, scale=float(128 ** -0.5)), reads=["kt"], writes=["kt"])
        for (x, xk) in ((qt, "qt"), (kt, "kt")):
            for c0 in range(256, TS, 512):
                def rope(x=x, xk=xk, c0=c0):
                    P.op("pe", lambda e: e.matmul(pA[:], pit[:], x[:, c0:c0 + 512], start=True, stop=True), reads=["pit", xk], writes=["pA"])
                    P.op("dve", lambda e: e.tensor_tensor(out=tmpr[:], in0=pA[:], in1=st[:, c0 - 256:c0 + 256], op=ALU.mult), reads=["pA", "st"], writes=["tmpr"])
                    P.op("pool", lambda e: e.tensor_tensor(out=x[:, c0:c0 + 512], in0=x[:, c0:c0 + 512], in1=ct[:, c0 - 256:c0 + 256], op=ALU.mult), reads=[xk, "ct", "pA"], writes=[xk])
                    P.op("dve", lambda e: e.tensor_tensor(out=x[:, c0:c0 + 512], in0=x[:, c0:c0 + 512], in1=tmpr[:], op=ALU.add), reads=[xk, "tmpr"], writes=[xk])
                rope()

        def state_update(n, d):
            cs = slice(n * 128, (n + 1) * 128); Sk = "S%d" % d
            P.op("pe", lambda e: e.transpose(pC[:], kt[:, cs], ident[:]), reads=["kt", "identf"], writes=["pC"])
            P.op("act", lambda e: e.activation(out=K0[:], in_=pC[:], func=AF.Copy, scale=kd[:, d:d + 1]), reads=["pC", "kd"], writes=["K0"])
            P.op("pe", lambda e: e.matmul(pD[:], K0[:], vt[:, n, :], start=True, stop=True), reads=["K0", "vt"], writes=["pD"])
            P.op("dve", lambda e: e.scalar_tensor_tensor(out=S[d][:], in0=S[d][:], scalar=cd[:, d:d + 1], in1=pD[:], op0=ALU.mult, op1=ALU.add), reads=[Sk, "cd", "pD"], writes=[Sk])

        def fwd(n):
            cs = slice(n * 128, (n + 1) * 128)
            P.op("pe", lambda e: e.matmul(pB[:], kt[:, cs], qt[:, cs], start=True, stop=True), reads=["kt", "qt"], writes=["pB"])
            P.op("dve", lambda e: e.tensor_tensor(out=A0[:], in0=pB[:], in1=M[:, 0, :], op=ALU.mult), reads=["pB", "M"], writes=["A0"])
            P.op("dve", lambda e: e.tensor_tensor(out=A1[:], in0=pB[:], in1=M[:, 1, :], op=ALU.mult), reads=["pB", "M"], writes=["A1"])
            P.op("pool", lambda e: e.tensor_tensor(out=Q0[:], in0=qt[:, cs], in1=QD[:, 0, :], op=ALU.mult), reads=["qt", "QD"], writes=["Q0"])
            P.op("pe", lambda e: e.matmul(pA[:, 0:128], A0[:], vt[:, n, :], start=True, stop=False), reads=["A0", "vt"], writes=["pA"])
            P.op("pe", lambda e: e.matmul(pA[:, 0:128], A1[:], vt[:, n, :], start=False, stop=False), reads=["A1", "vt"], writes=["pA"])
            P.op("pe", lambda e: e.matmul(pA[:, 0:128], Q0[:], S[0][:], start=False, stop=True), reads=["Q0", "S0"], writes=["pA"])
            P.op("act", lambda e: e.activation(out=O[:, n, :], in_=pA[:, 0:128], func=AF.Copy), reads=["pA"], writes=[("O", n)])
            state_update(n, 0)

        def bwd(n):
            cs = slice(n * 128, (n + 1) * 128)
            P.op("pool", lambda e: e.tensor_tensor(out=Q0[:], in0=qt[:, cs], in1=QD[:, 1, :], op=ALU.mult), reads=["qt", "QD"], writes=["Q0"])
            P.op("pe", lambda e: e.matmul(pA[:, 0:128], Q0[:], S[1][:], start=True, stop=True), reads=["Q0", "S1"], writes=["pA"])
            P.op("dve", lambda e: e.tensor_tensor(out=O[:, n, :], in0=O[:, n, :], in1=pA[:, 0:128], op=ALU.add), reads=["pA", ("O", n)], writes=[("O", n)])
            state_update(n, 1)
            P.op("dve", lambda e: e.tensor_reduce(out=mv[:, 0:1], in_=O[:, n, :], axis=AX.X, op=ALU.add), reads=[("O", n)], writes=["mv"])
            P.op("dve", lambda e: e.tensor_scalar(out=mv[:, 0:1], in0=mv[:, 0:1], scalar1=-1.0 / 128, scalar2=None, op0=ALU.mult), reads=["mv"], writes=["mv"])
            P.op("dve", lambda e: e.tensor_scalar(out=cen[:], in0=O[:, n, :], scalar1=mv[:, 0:1], scalar2=None, op0=ALU.add), reads=[("O", n), "mv"], writes=["cen"])
            P.op("act", lambda e: e.activation(out=sq[:], in_=cen[:], func=AF.Square, accum_out=mv[:, 1:2]), reads=["cen", "mv"], writes=["sqr", "mv"])
            P.op("dve", lambda e: e.tensor_scalar(out=mv[:, 2:3], in0=mv[:, 1:2], scalar1=1.0 / 128, scalar2=1e-6, op0=ALU.mult, op1=ALU.add), reads=["mv"], writes=["mv"])
            P.op("act", lambda e: e.activation(out=mv[:, 2:3], in_=mv[:, 2:3], func=AF.Sqrt), reads=["mv"], writes=["mv"])
            P.op("dve", lambda e: e.reciprocal(out=mv[:, 3:4], in_=mv[:, 2:3]), reads=["mv"], writes=["mv"])
            P.op("dve", lambda e: e.scalar_tensor_tensor(out=cen[:], in0=cen[:], scalar=mv[:, 3:4], in1=gwb[:], op0=ALU.mult, op1=ALU.mult), reads=["cen", "mv", "gwb"], writes=["cen"])
            P.op("act", lambda e: e.activation(out=sg[:], in_=gt[:, n, :], func=AF.Sigmoid), reads=["gt"], writes=["sgr"])
            P.op("pool", lambda e: e.tensor_tensor(out=sg[:], in0=sg[:], in1=gt[:, n, :], op=ALU.mult), reads=["sgr", "gt"], writes=["sgr"])
            P.op("dve", lambda e: e.tensor_tensor(out=O[:, n, :], in0=cen[:], in1=sg[:], op=ALU.mult), reads=["cen", "sgr"], writes=[("O", n)])

        for n in range(NCH):
            fwd(n)
        for n in [1, 0] + list(range(NCH - 1, 1, -1)):
            bwd(n)
        P.dma("sp", out[pi].rearrange("(n p) e -> p n e", p=128), O[:], reads=[("O", n) for n in range(NCH)], writes=["out"])

    pair(0)
    pair(1)
    return P.finish(["out"])


def launch_ret(ul, uc, rt_decay_l, rt_gn_w_l, cores=range(NCORES)):
    G = 512
    nc = build_ret()
    cosT, sinT, PiT, cc, colv = rt_consts()
    u = np.concatenate([uc, ul], axis=1)
    maps = []
    for c in cores:
        qs, ks, vs, gs, ds, gw = [], [], [], [], [], []
        for pi in (2 * c, 2 * c + 1):
            b, hd = pi // 4, pi % 4
            qs.append(u[b, :, 9 * G + hd * 128:9 * G + (hd + 1) * 128].T); ks.append(u[b, :, 10 * G + hd * 128:10 * G + (hd + 1) * 128].T)
            vs.append(u[b, :, 11 * G + hd * 128:11 * G + (hd + 1) * 128]); gs.append(u[b, :, 12 * G + hd * 128:12 * G + (hd + 1) * 128])
            ds.append(rt_decay_l[:, hd]); gw.append(rt_gn_w_l[hd * 128:(hd + 1) * 128])
        maps.append({"QT": np.ascontiguousarray(np.stack(qs)), "KT": np.ascontiguousarray(np.stack(ks)), "V": np.ascontiguousarray(np.stack(vs)),
                     "G": np.ascontiguousarray(np.stack(gs)), "dec": np.ascontiguousarray(np.stack(ds)), "gnw": np.ascontiguousarray(np.stack(gw)),
                     "cosT": cosT, "sinT": sinT, "PiT": PiT, "cc": cc, "colv": colv})
    res = run(nc, maps)
    o = np.zeros((4, TS, 512), np.float32)
    for ci, c in enumerate(cores):
        for k, pi in enumerate((2 * c, 2 * c + 1)):
            b, hd = pi // 4, pi % 4
            o[b, :, hd * 128:(hd + 1) * 128] = res[ci]["out"][k]
    return np.ascontiguousarray(o[:, 256:]), np.ascontiguousarray(o[:, :256])


HY_MIN_DECAY = np.log(1e-2) / 1.5
HY_MAX_DECAY = np.log(1e-2) / 0.3


def hy_consts(L):
    t = np.linspace(0.0, 1.0, L, dtype=np.float32)
    fr = np.linspace(1e-4, 15, 16, dtype=np.float32)
    ang = (np.float32(2.0 * np.pi / L) * np.arange(L, dtype=np.float32)[:, None] * fr[None, :]).astype(np.float32)
    z = np.concatenate([t[:, None], np.cos(ang), -np.sin(ang)], axis=-1).astype(np.float32)
    return np.ascontiguousarray(z.T), np.ascontiguousarray(t[None, :])


def hy_deltas():
    return np.abs(np.linspace(HY_MIN_DECAY, HY_MAX_DECAY, 512, dtype=np.float32)).astype(np.float32)


HY_DEBUG = False


def build_hyena(with_ctx=True):
    P = Prog()
    Ls = [4096, 256] if with_ctx else [4096]
    sT = {4096: P.inp("sT", [3, 64, 4, 4096])}
    zTs = {4096: P.inp("zT", [33, 4096])}; tls = {4096: P.inp("tl", [1, 4096])}
    oq = {4096: P.out("oq", [128, 64, 32, 4])}
    if with_ctx:
        sT[256] = P.inp("sTc", [3, 64, 4, 256]); zTs[256] = P.inp("zTc", [33, 256]); tls[256] = P.inp("tlc", [1, 256]); oq[256] = P.out("oqc", [128, 64, 2, 4])
    sw = P.inp("sw", [64, 3, 3]); sbi = P.inp("sb", [64, 3]); dl = P.inp("dl", [64, 1])
    w1 = P.inp("w1", [33, 64]); w2 = P.inp("w2", [64, 64]); w3 = P.inp("w3", [64, 64]); w4 = P.inp("w4", [64, 4, 64])
    fq = P.inp("fq", [64, 4])
    hb = P.inp("hb", [2, 64])
    TAPS = {L: P.dram("TAPS%d" % L, [64, 2, 2 * L], F32, "ExternalOutput" if HY_DEBUG else "Internal") for L in Ls}

    swt = P.sb("swt", [64, 3, 3]); sbt = P.sb("sbt", [64, 3]); dlt = P.sb("dlt", [64, 1]); w1t = P.sb("w1t", [33, 64]); w2t = P.sb("w2t", [64, 64]); w3t = P.sb("w3t", [64, 64])
    w4t = P.sb("w4t", [64, 4, 64]); fqt = P.sb("fqt", [64, 4]); fbt = P.sb("fbt", [64, 3]); hbt = P.sb("hbt", [128, 2, 64]); ident = P.sb("identf", [128, 128])
    for t_, s_, k_ in ((swt, sw, "swt"), (sbt, sbi, "sbt"), (dlt, dl, "dlt"), (w1t, w1, "w1t"), (w2t, w2, "w2t"), (w3t, w3, "w3t"), (w4t, w4, "w4t"), (fqt, fq, "fqt")):
        P.dma("sp", t_[:], s_, writes=[k_])
    P.dma("sp", hbt[:].rearrange("p o c -> p (o c)"), hb.rearrange("o c -> (o c)").partition_broadcast(128), writes=["hbt"])
    P.op("dve", lambda e: e.tensor_scalar(out=fbt[:], in0=fqt[:, 1:4], scalar1=fqt[:, 0:1], scalar2=None, op0=ALU.mult), reads=["fqt"], writes=["fbt"])
    P.op("dve", lambda e: e.tensor_scalar(out=dlt[:], in0=dlt[:], scalar1=-1.0, scalar2=None, op0=ALU.mult), reads=["dlt"], writes=["dlt"])
    P.op("pool", lambda e: e.memset(ident[:], 1.0), writes=["identf"])
    P.op("pool", lambda e: e.affine_select(out=ident[:], in_=ident[:], pattern=[[-1, 128]], compare_op=ALU.is_equal, fill=0.0, base=0, channel_multiplier=1), reads=["identf"], writes=["identf"])

    zt = P.sb("zt", [33, 512]); tlt = P.sb("tlt", [64, 512]); big = P.sb("big", [128, 16384]); hA = P.sb("hA", [64, 512]); hB = P.sb("hB", [64, 512])
    Tf = big[0:64, :].rearrange("p (g l) -> p g l", g=4)
    nrm = P.sb("nrm", [64, 4]); rvt = P.sb("rvt", [64, 4096]); junk = rvt
    pA = P.ps("pA", [64, 512]); pT = P.ps("pT", [128, 64]); pC = [P.ps("pC%d" % i, [128, 128]) for i in range(2)]
    TWO_PI = float(2 * np.pi)

    def sin_layer(src_ps, dst, dkey, li, W):
        P.op("dve", lambda e: e.tensor_scalar(out=dst[:, 0:W], in0=src_ps[:, 0:W], scalar1=fqt[:, 0:1], scalar2=fbt[:, li:li + 1], op0=ALU.mult, op1=ALU.add), reads=["pA", "fqt", "fbt"], writes=[dkey])
        P.op("dve", lambda e: e.tensor_scalar(out=dst[:, 0:W], in0=dst[:, 0:W], scalar1=float(1.0 / TWO_PI), scalar2=8.5, op0=ALU.mult, op1=ALU.add), reads=[dkey], writes=[dkey])
        P.op("dve", lambda e: e.tensor_copy(out=kint[:, 0:W], in_=dst[:, 0:W]), reads=[dkey], writes=["kint"])
        P.op("dve", lambda e: e.tensor_copy(out=kflt[:, 0:W], in_=kint[:, 0:W]), reads=["kint"], writes=["kflt"])
        P.op("dve", lambda e: e.tensor_tensor(out=dst[:, 0:W], in0=dst[:, 0:W], in1=kflt[:, 0:W], op=ALU.subtract), reads=[dkey, "kflt"], writes=[dkey])
        P.op("dve", lambda e: e.tensor_scalar(out=kflt[:, 0:W], in0=dst[:, 0:W], scalar1=0.0, scalar2=None, op0=ALU.is_lt), reads=[dkey, "kflt"], writes=["kflt"])
        P.op("dve", lambda e: e.tensor_tensor(out=dst[:, 0:W], in0=dst[:, 0:W], in1=kflt[:, 0:W], op=ALU.add), reads=[dkey, "kflt"], writes=[dkey])
        P.op("act", lambda e: e.activation(out=dst[:, 0:W], in_=dst[:, 0:W], func=AF.Sin, bias=mpi[:, 0:1], scale=TWO_PI), reads=[dkey, "mpi"], writes=[dkey])

    kint = P.sb("kint", [64, 512], I32); kflt = P.sb("kflt", [64, 512])
    mpi = P.sb("mpi", [64, 1]); zero1 = P.sb("zero1", [64, 1]); hbf = P.sb("hbf", [64, 2])
    P.op("dve", lambda e: e.memset(zero1[:], 0.0), writes=["zero1"])
    P.dma("sp", hbf[:], P.inp("hbfi", [64, 2]), writes=["hbf"])
    P.op("dve", lambda e: e.memset(mpi[:], -float(np.pi)), writes=["mpi"])

    def gen_filter(L):
        for c0 in range(0, L, 512):
            def chunk(c0=c0):
                W = min(512, L - c0)
                P.dma("sp", zt[:, 0:W], zTs[L][:, c0:c0 + W], writes=["zt"]); P.dma("sp", tlt[:, 0:W], tls[L][0:1, c0:c0 + W].partition_broadcast(64), writes=["tlt"])
                P.op("act", lambda e: e.activation(out=tlt[:, 0:W], in_=tlt[:, 0:W], func=AF.Exp, scale=dlt[:, 0:1]), reads=["tlt", "dlt"], writes=["tlt"])
                P.op("pe", lambda e: e.matmul(pA[:, 0:W], w1t[:], zt[:, 0:W], start=True, stop=True), reads=["w1t", "zt"], writes=["pA"])
                sin_layer(pA, hA, "hA", 0, W)
                P.op("pe", lambda e: e.matmul(pA[:, 0:W], w2t[:], hA[:, 0:W], start=True, stop=True), reads=["w2t", "hA"], writes=["pA"])
                sin_layer(pA, hB, "hB", 1, W)
                P.op("pe", lambda e: e.matmul(pA[:, 0:W], w3t[:], hB[:, 0:W], start=True, stop=True), reads=["w3t", "hB"], writes=["pA"])
                sin_layer(pA, hA, "hA", 2, W)
                for g in range(4):
                    P.op("pe", lambda e, g=g: e.matmul(pA[:, 0:W], w4t[:, g, :], hA[:, 0:W], start=True, stop=True), reads=["w4t", "hA"], writes=["pA"])
                    P.op("dve", lambda e, g=g: e.tensor_tensor(out=Tf[:, g, c0:c0 + W], in0=pA[:, 0:W], in1=tlt[:, 0:W], op=ALU.mult), reads=["pA", "tlt"], writes=["Tf", "Tz0", "Tz1"])
            chunk()
        for g in range(4):
            lo = 0 if g < 2 else 1
            P.op("act", lambda e, g=g, lo=lo: e.activation(out=junk[:, lo:L], in_=Tf[:, g, lo:L], func=AF.Abs, accum_out=nrm[:, g:g + 1]), reads=["Tf"], writes=["rvt", "nrm"])
        P.op("dve", lambda e: e.tensor_tensor(out=nrm[:, 0:2], in0=nrm[:, 0:2], in1=nrm[:, 2:4], op=ALU.add), reads=["nrm"], writes=["nrm"])
        P.op("dve", lambda e: e.reciprocal(out=nrm[:, 0:2], in_=nrm[:, 0:2]), reads=["nrm"], writes=["nrm"])
        for g in range(4):
            o = g % 2
            P.op("dve", lambda e, g=g, o=o: e.tensor_scalar(out=Tf[:, g, 0:L], in0=Tf[:, g, 0:L], scalar1=nrm[:, o:o + 1], scalar2=None, op0=ALU.mult), reads=["Tf", "nrm"], writes=["Tf"])
        for o in range(2):
            P.op("dve", lambda e, o=o: e.tensor_tensor(out=Tf[:, o, 0:1], in0=Tf[:, o, 0:1], in1=hbf[:, o:o + 1], op=ALU.add), reads=["Tf", "hbf"], writes=["Tf"])
        P.op("pool", lambda e: e.tensor_copy(out=rvt[:, 0:L], in_=rev(Tf[:, 0, 0:L])), reads=["Tf"], writes=["rvt"])
        P.dma("sp", TAPS[L][:, 0, 0:L], rvt[:, 0:L], reads=["rvt"], writes=["TAPS"])
        P.dma("sp", TAPS[L][:, 0, L:2 * L - 1], Tf[:, 2, 1:L], reads=["Tf"], writes=["TAPS"])
        P.dma("sp", TAPS[L][:, 0, 2 * L - 1:2 * L], zero1[:, 0:1], reads=["zero1"], writes=["TAPS"], allow_slow_non_contiguous=True)
        P.dma("sp", TAPS[L][:, 1, L:2 * L], Tf[:, 1, 0:L], reads=["Tf"], writes=["TAPS"])
        P.op("pool", lambda e: e.tensor_copy(out=rvt[:, 1:L], in_=rev(Tf[:, 3, 1:L])), reads=["Tf", "rvt"], writes=["rvt"])
        P.op("pool", lambda e: e.memset(rvt[:, 0:1], 0.0), reads=["rvt"], writes=["rvt"])
        P.dma("sp", TAPS[L][:, 1, 0:L], rvt[:, 0:L], reads=["rvt"], writes=["TAPS"])

    xs = P.sb("xs", [64, 4096]); xcv = P.sb("xcv", [64, 4096])
    NBMAX = 32
    NPART = 4; CPP = 64 // NPART
    Uq = P.sb("Uq", [128, 3, CPP, NBMAX, 4])
    Zp = [P.sb("Zp%d" % i, [128, 3 * NBMAX - 2, 4]) for i in range(2)]
    Tz = [big[:, 0:8192], big[:, 8192:16384]]
    Oq = P.sb("Oq", [128, CPP, NBMAX, 4]); tmpq = P.sb("tmpq", [128, NBMAX, 4])
    cnt = [0]

    def conv_L(L):
        nb = L // 128; ncb = 2 * nb - 1
        def part(half):
            ch0 = half * CPP
            for s in range(3):
                for b in range(4):
                    def sc(s=s, b=b):
                        P.dma("sp", xs[:, 0:L], sT[L][s, :, b, :], writes=["xs"])
                        P.op("dve", lambda e: e.tensor_scalar(out=xcv[:, 0:L], in0=xs[:, 0:L], scalar1=swt[:, 1, s:s + 1], scalar2=sbt[:, s:s + 1], op0=ALU.mult, op1=ALU.add), reads=["xs", "swt", "sbt"], writes=["xcv"])
                        P.op("dve", lambda e: e.scalar_tensor_tensor(out=xcv[:, 1:L], in0=xs[:, 0:L - 1], scalar=swt[:, 0, s:s + 1], in1=xcv[:, 1:L], op0=ALU.mult, op1=ALU.add), reads=["xs", "swt", "xcv"], writes=["xcv"])
                        P.op("dve", lambda e: e.scalar_tensor_tensor(out=xcv[:, 0:L - 1], in0=xs[:, 1:L], scalar=swt[:, 2, s:s + 1], in1=xcv[:, 0:L - 1], op0=ALU.mult, op1=ALU.add), reads=["xs", "swt", "xcv"], writes=["xcv"])
                        srcx = xcv
                        if s == 1:
                            P.op("pool", lambda e: e.tensor_copy(out=xs[:, 0:L], in_=rev(xcv[:, 0:L])), reads=["xcv", "xs"], writes=["xs"])
                            srcx = xs
                        for a in range(nb):
                            ad = a if s != 1 else nb - 1 - a
                            P.op("pe", lambda e, a=a: e.transpose(pT[:, :], srcx[:, a * 128:(a + 1) * 128], ident[0:64, 0:64]), reads=["xcv", "xs", "identf"], writes=["pT"])
                            P.op("act", lambda e, ad=ad: e.activation(out=Uq[:, s, :, ad, b], in_=pT[:, ch0:ch0 + CPP], func=AF.Copy), reads=["pT"], writes=["Uq"])
                    sc()
            for cl in range(CPP):
                ch = ch0 + cl
                for o in range(2):
                    def stage(cl=cl, ch=ch, o=o):
                        i = cnt[0] % 2; cnt[0] += 1
                        tz = Tz[i]; tzk = "Tz%d" % i; zp = Zp[o]; zk = "Zp%d" % o; pc = pC[i]; pck = "pC%d" % i
                        src = bass.AP(TAPS[L].tensor, TAPS[L][ch, o, o:o + 1].offset, [[1, 128], [1, ncb * 128]])
                        P.dma("sp" if i == 0 else "act", tz[:, 0:ncb * 128], src, reads=["TAPS"], writes=[tzk])
                        if o == 0:
                            P.op("pool", lambda e: e.tensor_copy(out=zp[:, nb - 1:2 * nb - 1, :], in_=Uq[:, 0, cl, 0:nb, :]), reads=["Uq"], writes=[zk])
                        for ci in range(ncb):
                            r0 = 2 * (nb - 1) - ci
                            cb = (ncb - 1 - ci) if o == 0 else ci
                            P.op("pe", lambda e, ci=ci, r0=r0, cb=cb: e.matmul(pc[:, 0:nb * 4], tz[:, cb * 128:(cb + 1) * 128], zp[:, r0:r0 + nb, :].rearrange("p a b -> p (a b)"), start=(ci == 0), stop=(ci == ncb - 1)),
                                 reads=[tzk, zk], writes=[pck])
                        pcv = pc[:, 0:nb * 4].rearrange("p (a b) -> p a b", b=4)
                        if o == 0:
                            P.op("dve", lambda e: e.tensor_tensor(out=Zp[1][:, nb - 1:2 * nb - 1, :], in0=pcv, in1=Uq[:, 1, cl, 0:nb, :], op=ALU.mult), reads=[pck, "Uq"], writes=["Zp1"])
                        else:
                            P.op("dve", lambda e: e.tensor_tensor(out=Oq[:, cl, 0:nb, :], in0=pcv, in1=Uq[:, 2, cl, 0:nb, :], op=ALU.mult), reads=[pck, "Uq"], writes=["Oq"])
                    stage()
            P.dma("sp", oq[L][:, ch0:ch0 + CPP, :, :], Oq[:, :, 0:nb, :], reads=["Oq"], writes=["oq%d" % L])
        for half in range(NPART):
            part(half)

    for L in Ls:
        nb = L // 128
        for i in range(2):
            P.op("pool", lambda e, i=i: e.memset(Zp[i][:], 0.0), reads=[], writes=["Zp%d" % i])
        gen_filter(L)
        conv_L(L)
    return P.finish(["oq%d" % L for L in Ls])


def launch_hyena(ul, uc, p, with_ctx=True, cores=range(NCORES)):
    G = 512
    nc = build_hyena(with_ctx)
    zT, tl = hy_consts(4096); zTc, tlc = hy_consts(256)
    dl_all = hy_deltas()
    maps = []
    for c in cores:
        sl = slice(c * 64, (c + 1) * 64)
        cols = [s * G + c * 64 for s in range(3)]
        m = {"sT": np.ascontiguousarray(np.stack([ul[:, :, k:k + 64].transpose(2, 0, 1) for k in cols])), "zT": zT, "tl": tl}
        if with_ctx:
            m["sTc"] = np.ascontiguousarray(np.stack([uc[:, :, k:k + 64].transpose(2, 0, 1) for k in cols])); m["zTc"] = zTc; m["tlc"] = tlc
        swf = p["hy_short_w"].reshape(3, 3, G)[:, :, sl]
        m["sw"] = np.ascontiguousarray(swf.transpose(2, 0, 1))
        m["sb"] = np.ascontiguousarray(p["hy_short_b"].reshape(3, G)[:, sl].T)
        m["dl"] = np.ascontiguousarray(dl_all[sl][:, None])
        m["w1"] = np.ascontiguousarray(p["hy_f_w1"]); m["w2"] = np.ascontiguousarray(p["hy_f_w2"]); m["w3"] = np.ascontiguousarray(p["hy_f_w3"])
        m["w4"] = np.ascontiguousarray(p["hy_f_w4"].reshape(64, 4, G)[:, :, sl])
        m["fq"] = np.ascontiguousarray(np.stack([p["hy_f_freq"], p["hy_f_b1"], p["hy_f_b2"], p["hy_f_b3"]], -1))
        m["hb"] = np.ascontiguousarray(p["hy_bias"][:, sl]); m["hbfi"] = np.ascontiguousarray(p["hy_bias"][:, sl].T)
        maps.append(m)
    res = run(nc, maps)
    hl = np.concatenate([r["oq"].transpose(3, 2, 0, 1).reshape(4, 4096, 64) for r in res], -1)
    hc = np.concatenate([r["oqc"].transpose(3, 2, 0, 1).reshape(4, 256, 64) for r in res], -1) if with_ctx else None
    if HY_DEBUG:
        return hl, hc, res
    return hl, hc


H2T_DT = BF16
DBG_STOP = 0


def build_post():
    P = Prog()
    yT = P.inp("yT", [D, NT * 128]); xt = P.inp("xt", [NT, 128, D]); wo = P.inp("wo", [D, D])
    g1 = P.inp("g1", [2, D]); msh = P.inp("msh", [2, 2, D]); nw = P.inp("nw", [1, D]); rw = P.inp("rw", [D, 32]); rb = P.inp("rb", [1, 32])
    x1 = P.out("x1", [NT, 128, D]); h2T = P.out("h2T", [D, NT * 128], H2T_DT); gate = P.out("gate", [NT, 128, 32])
    A = P.sb("A", [128, 2, D]); S = P.sb("S", [128, 2, D]); nwb = P.sb("nwb", [128, D]); g1b = P.sb("g1b", [128, 2, D])
    wob = P.sb("wob", [128, 16, D], BF16); rwt = P.sb("rwt", [128, 16, 32]); rbt = P.sb("rbt", [1, 32]); ones = P.sb("ones", [1, 128])
    ident = P.sb("identf", [128, 128])
    xs = [P.sb("xs%d" % i, [128, D]) for i in range(2)]; ys = [P.sb("ys%d" % i, [128, 16, 128], BF16) for i in range(2)]
    tmp = P.sb("tmp", [128, D]); sq = P.sb("sq", [128, D]); ss = P.sb("ss", [128, 1]); rs = P.sb("rs", [128, 1]); h2 = P.sb("h2", [128, D])
    hTf = P.sb("hTf", [128, 16, 128]); hTb = P.sb("hTb", [128, 16, 128], BF16)
    lg = P.sb("lg", [128, 32]); mx = P.sb("mx", [128, 8]); msk = P.sb("msk", [128, 32]); ex = P.sb("ex", [128, 32]); sm = P.sb("sm", [128, 2]); gt = P.sb("gt", [128, 32])
    psm = [P.ps("psm%d" % i, [128, 512]) for i in range(3)]
    pst = [P.ps("pst%d" % i, [128, 4, 128]) for i in range(2)]
    psr = P.ps("psr", [128, 32])
    P.op("pool", lambda e: e.memset(ident[:], 1.0), writes=["identf"])
    P.op("pool", lambda e: e.affine_select(out=ident[:], in_=ident[:], pattern=[[-1, 128]], compare_op=ALU.is_equal, fill=0.0, base=0, channel_multiplier=1), reads=["identf"], writes=["identf"])
    P.op("dve", lambda e: e.memset(ones[:], 1.0), writes=["ones"])
    emit_AS(P, msh, nw, A, S, nwb)
    for wch in range(2):
        P.dma("sp", g1b[:, wch, :], g1[wch:wch + 1, :].partition_broadcast(128), writes=["g1b"])
    for kc in range(16):
        P.dma("pool", wob[:, kc, :], wo[kc * 128:(kc + 1) * 128, :], writes=["wob"])
    P.dma("sp", rwt[:], rw.rearrange("(kc p) n -> p kc n", p=128), writes=["rwt"]); P.dma("sp", rbt[:], rb[:, :], writes=["rbt"])
    kk = [0, 0]

    def tile(t):
        wch = 0 if t == 0 else 1
        x_ = xs[t % 2]; xk = "xs%d" % (t % 2); y_ = ys[t % 2]; yk = "ys%d" % (t % 2)
        P.dma("sp", x_[:], xt[t], writes=[xk])
        P.dma("pool", y_[:], yT[:, t * 128:(t + 1) * 128].rearrange("(kc p) n -> p kc n", p=128), writes=[yk])
        for n in range(4):
            i = kk[0] % 3; kk[0] += 1
            pp = psm[i]; pk = "psm%d" % i
            for kc in range(16):
                P.op("pe", lambda e, pp=pp, kc=kc, n=n: e.matmul(pp[:], y_[:, kc, :], wob[:, kc, n * 512:(n + 1) * 512], start=(kc == 0), stop=(kc == 15)), reads=[yk, "wob"], writes=[pk])
            P.op("dve", lambda e, pp=pp, n=n: e.tensor_tensor(out=tmp[:, n * 512:(n + 1) * 512], in0=pp[:], in1=g1b[:, wch, n * 512:(n + 1) * 512], op=ALU.mult), reads=[pk, "g1b"], writes=["tmp"])
        P.op("pool", lambda e: e.tensor_tensor(out=x_[:], in0=x_[:], in1=tmp[:], op=ALU.add), reads=["tmp", xk], writes=[xk])
        P.dma("sp", x1[t], x_[:], reads=[xk], writes=["x1"])
        if DBG_STOP == 1:
            return
        P.op("act", lambda e: e.activation(out=sq[:], in_=x_[:], func=AF.Square, accum_out=ss[:]), reads=[xk], writes=["sq", "ss"])
        P.op("dve", lambda e: e.tensor_scalar(out=rs[:], in0=ss[:], scalar1=1.0 / D, scalar2=EPS, op0=ALU.mult, op1=ALU.add), reads=["ss"], writes=["rs"])
        P.op("act", lambda e: e.activation(out=rs[:], in_=rs[:], func=AF.Sqrt), reads=["rs"], writes=["rs"])
        P.op("dve", lambda e: e.reciprocal(out=rs[:], in_=rs[:]), reads=["rs"], writes=["rs"])
        P.op("dve", lambda e: e.scalar_tensor_tensor(out=tmp[:], in0=x_[:], scalar=rs[:, 0:1], in1=A[:, wch, :], op0=ALU.mult, op1=ALU.mult), reads=[xk, "rs", "A", "tmp"], writes=["tmp"])
        P.op("pool", lambda e: e.tensor_tensor(out=h2[:], in0=tmp[:], in1=S[:, wch, :], op=ALU.add), reads=["tmp", "S"], writes=["h2"])
        if DBG_STOP == 2:
            return
        for g in range(4):
            i = kk[1] % 2; kk[1] += 1
            pt = pst[i]; pk = "pst%d" % i
            for j in range(4):
                kc = g * 4 + j
                P.op("pe", lambda e, pt=pt, j=j, kc=kc: e.transpose(pt[:, j, :], h2[:, kc * 128:(kc + 1) * 128], ident[:]), reads=["h2", "identf"], writes=[pk])
            P.op("act", lambda e, pt=pt, g=g: e.activation(out=hTf[:, g * 4:(g + 1) * 4, :], in_=pt[:], func=AF.Copy), reads=[pk], writes=["hTf"])
            P.op("dve", lambda e, g=g: e.tensor_copy(out=hTb[:, g * 4:(g + 1) * 4, :], in_=hTf[:, g * 4:(g + 1) * 4, :]), reads=["hTf"], writes=["hTb"])
        P.dma("sp", h2T[:, t * 128:(t + 1) * 128].rearrange("(kc p) n -> p kc n", p=128), (hTb[:] if H2T_DT == BF16 else hTf[:]), reads=["hTb", "hTf"], writes=["h2T"])
        if DBG_STOP == 3:
            return
        for kc in range(16):
            P.op("pe", lambda e, kc=kc: e.matmul(psr[:], hTf[:, kc, :], rwt[:, kc, :], start=(kc == 0), stop=False), reads=["hTf", "rwt"], writes=["psr"])
        P.op("pe", lambda e: e.matmul(psr[:], ones[:, :], rbt[:, :], start=False, stop=True), reads=["ones", "rbt"], writes=["psr"])
        P.op("dve", lambda e: e.tensor_copy(out=lg[:], in_=psr[:]), reads=["psr"], writes=["lg"])
        if DBG_STOP == 4:
            return
        P.op("dve", lambda e: e.max(out=mx[:], in_=lg[:]), reads=["lg"], writes=["mx"])
        if DBG_STOP == 5:
            return
        P.op("dve", lambda e: e.tensor_scalar(out=msk[:], in0=lg[:], scalar1=mx[:, 3:4], scalar2=None, op0=ALU.is_ge), reads=["lg", "mx"], writes=["msk"])
        P.op("dve", lambda e: e.tensor_scalar(out=sm[:, 0:1], in0=mx[:, 0:1], scalar1=-1.0, scalar2=None, op0=ALU.mult), reads=["mx"], writes=["sm"])
        P.op("act", lambda e: e.activation(out=ex[:], in_=lg[:], func=AF.Exp, bias=sm[:, 0:1]), reads=["lg", "sm"], writes=["ex"])
        P.op("dve", lambda e: e.tensor_tensor(out=ex[:], in0=ex[:], in1=msk[:], op=ALU.mult), reads=["ex", "msk"], writes=["ex"])
        P.op("dve", lambda e: e.tensor_reduce(out=sm[:, 1:2], in_=ex[:], axis=AX.X, op=ALU.add), reads=["ex", "sm"], writes=["sm"])
        P.op("dve", lambda e: e.reciprocal(out=sm[:, 1:2], in_=sm[:, 1:2]), reads=["sm"], writes=["sm"])
        P.op("dve", lambda e: e.tensor_scalar(out=gt[:], in0=ex[:], scalar1=sm[:, 1:2], scalar2=None, op0=ALU.mult), reads=["ex", "sm"], writes=["gt"])
        P.dma("sp", gate[t], gt[:], reads=["gt"], writes=["gate"])

    for t in range(NT):
        tile(t)
    return P.finish(["x1", "h2T", "gate"])


def launch_post(mix_l, mix_c, xts, mod_l, w_out_l, nw2_l, rw_l, rb_l):
    nc = build_post()
    m6 = mod_l.reshape(5, 6, D)
    mts = tile_split(mix_l, mix_c)
    maps = []
    for c in range(NCORES):
        b = c // 2
        maps.append({"yT": np.ascontiguousarray(mts[c].reshape(NT * 128, D).T), "xt": xts[c], "wo": w_out_l,
                     "g1": np.ascontiguousarray(np.stack([m6[4, 2], m6[b, 2]], 0)),
                     "msh": np.ascontiguousarray(np.stack([m6[4, 3:5], m6[b, 3:5]], 0)),
                     "nw": np.ascontiguousarray(nw2_l[None]), "rw": rw_l, "rb": np.ascontiguousarray(rb_l[None])})
    res = run(nc, maps)
    return [r["x1"] for r in res], [r["h2T"] for r in res], [r["gate"] for r in res]


NE_C = 8
NTG = 68
FE = 1024


def build_moe(nb_limit=None):
    P = Prog()
    hT = P.inp("hT", [D, NTG * 128], BF16); gtT = P.inp("gtT", [NE_C, NTG * 128]); gt = P.inp("gt", [128, NTG, NE_C])
    wgu = P.inp("wgu", [NE_C, D, 2 * FE]); bgu = P.inp("bgu", [128, NE_C, 16]); wdn = P.inp("wdn", [NE_C, FE, D]); bdn = P.inp("bdn", [NE_C, D])
    part = P.out("part", [NTG, 128, D])
    gts = P.sb("gts", [128, NTG, NE_C]); bgt = P.sb("bgt", [128, NE_C, 16]); bdt = P.sb("bdt", [NE_C, D]); gTb = [P.sb("gTb%d" % i, [NE_C, 512]) for i in range(2)]
    hb = [P.sb("hb%d" % i, [128, 16, 512], BF16) for i in range(2)]
    wgf = [P.sb("wgf%d" % i, [128, 16, 2, 128], BF16) for i in range(3)]
    wd = [P.sb("wd%d" % i, [128, 8, D], BF16) for i in range(2)]
    act = [P.sb("act%d" % i, [128, 8, 512], BF16) for i in range(2)]
    acc = P.sb("acc", [128, 4, D])
    gs = P.sb("gs", [128, 512]); ls = P.sb("ls", [128, 512]); sgs = P.sb("sgs", [128, 512])
    pg = [P.ps("pg%d" % i, [128, 512]) for i in range(2)]; pl = [P.ps("pl%d" % i, [128, 512]) for i in range(2)]; p2 = [P.ps("p2%d" % i, [128, 512]) for i in range(2)]
    P.dma("sp", gts[:], gt[:, :, :], writes=["gts"]); P.dma("sp", bgt[:], bgu[:, :, :], writes=["bgt"]); P.dma("sp", bdt[:], bdn[:, :], writes=["bdt"])
    cnt = {"wgf": 0, "wd": 0, "ps1": 0, "p2": 0, "act": 0}
    NB = NTG // 4

    def block(bi):
        h_ = hb[bi % 2]; hk = "hb%d" % (bi % 2); gT_ = gTb[bi % 2]; gk = "gTb%d" % (bi % 2)
        c0 = bi * 512
        P.dma("sp", h_[:], hT[:, c0:c0 + 512].rearrange("(kc p) n -> p kc n", p=128), writes=[hk])
        P.dma("sp", gT_[:], gtT[:, c0:c0 + 512], writes=[gk])
        for tl in range(4):
            for n in range(4):
                i = cnt["p2"] % 2; cnt["p2"] += 1
                pp = p2[i]; pk = "p2%d" % i
                P.op("pe", lambda e, pp=pp, tl=tl, n=n: e.matmul(pp[:], gT_[:, tl * 128:(tl + 1) * 128], bdt[:, n * 512:(n + 1) * 512], start=True, stop=True), reads=[gk, "bdt"], writes=[pk])
                P.op("act", lambda e, pp=pp, tl=tl, n=n: e.activation(out=acc[:, tl, n * 512:(n + 1) * 512], in_=pp[:], func=AF.Copy), reads=[pk], writes=["acc"])
        for ex in range(NE_C):
            expert(bi, ex, h_, hk)
        P.dma("sp", part[bi * 4:(bi + 1) * 4].rearrange("t p d -> p t d"), acc[:], reads=["acc"], writes=["part"])

    def expert(bi, ex, h_, hk):
        wi = cnt["wd"] % 2; cnt["wd"] += 1
        wd_ = wd[wi]; wdk = "wd%d" % wi
        P.dma("pool", wd_[:], wdn[ex].rearrange("(fc p) n -> p fc n", p=128), writes=[wdk])
        ai = cnt["act"] % 2; cnt["act"] += 1
        a_ = act[ai]; ak = "act%d" % ai
        for fc in range(8):
            def fcb(fc=fc):
                ri = cnt["wgf"] % 3; cnt["wgf"] += 1
                wf = wgf[ri]; wfk = "wgf%d" % ri
                P.dma("pool", wf[:, :, 0, :], wgu[ex, :, fc * 128:(fc + 1) * 128].rearrange("(kc p) n -> p kc n", p=128), writes=[wfk])
                P.dma("pool", wf[:, :, 1, :], wgu[ex, :, FE + fc * 128:FE + (fc + 1) * 128].rearrange("(kc p) n -> p kc n", p=128), writes=[wfk])
                pi = cnt["ps1"] % 2; cnt["ps1"] += 1
                pg_ = pg[pi]; pgk = "pg%d" % pi; pl_ = pl[pi]; plk = "pl%d" % pi
                for kc in range(16):
                    P.op("pe", lambda e, kc=kc: e.matmul(pg_[:], wf[:, kc, 0, :], h_[:, kc, :], start=(kc == 0), stop=(kc == 15)), reads=[wfk, hk], writes=[pgk])
                for kc in range(16):
                    P.op("pe", lambda e, kc=kc: e.matmul(pl_[:], wf[:, kc, 1, :], h_[:, kc, :], start=(kc == 0), stop=(kc == 15)), reads=[wfk, hk], writes=[plk])
                P.op("dve", lambda e: e.tensor_scalar(out=gs[:], in0=pg_[:], scalar1=bgt[:, ex, fc:fc + 1], scalar2=7.0, op0=ALU.add, op1=ALU.min), reads=[pgk, "bgt"], writes=["gs"])
                P.op("dve", lambda e: e.tensor_scalar(out=ls[:], in0=pl_[:], scalar1=bgt[:, ex, 8 + fc:9 + fc], scalar2=7.0, op0=ALU.add, op1=ALU.min), reads=[plk, "bgt"], writes=["ls"])
                P.op("dve", lambda e: e.tensor_scalar(out=ls[:], in0=ls[:], scalar1=-7.0, scalar2=1.0, op0=ALU.max, op1=ALU.add), reads=["ls"], writes=["ls"])
                P.op("act", lambda e: e.activation(out=sgs[:], in_=gs[:], func=AF.Sigmoid, scale=1.702), reads=["gs"], writes=["sgs"])
                P.op("dve", lambda e: e.tensor_tensor(out=gs[:], in0=gs[:], in1=sgs[:], op=ALU.mult), reads=["gs", "sgs"], writes=["gs"])
                P.op("dve", lambda e: e.tensor_tensor(out=a_[:, fc, :], in0=gs[:], in1=ls[:], op=ALU.mult), reads=["gs", "ls"], writes=[ak])
            fcb()
        for tl in range(4):
            for n in range(4):
                def st2(tl=tl, n=n):
                    i = cnt["p2"] % 2; cnt["p2"] += 1
                    pp = p2[i]; pk = "p2%d" % i
                    for fc in range(8):
                        P.op("pe", lambda e, fc=fc: e.matmul(pp[:], a_[:, fc, tl * 128:(tl + 1) * 128], wd_[:, fc, n * 512:(n + 1) * 512], start=(fc == 0), stop=(fc == 7)), reads=[ak, wdk], writes=[pk])
                    P.op("dve", lambda e: e.scalar_tensor_tensor(out=acc[:, tl, n * 512:(n + 1) * 512], in0=pp[:], scalar=gts[:, bi * 4 + tl, ex:ex + 1], in1=acc[:, tl, n * 512:(n + 1) * 512], op0=ALU.mult, op1=ALU.add),
                         reads=[pk, "gts", "acc"], writes=["acc"])
                st2()

    for bi in range(NB if nb_limit is None else nb_limit):
        block(bi)
    return P.finish(["part"])


def launch_moe(h2T_cores, gate_cores, w_gu_l, b_gu_l, w_dn_l, b_dn_l):
    nc = build_moe()
    maps = []
    for c in range(NCORES):
        g, e4 = c // 4, c % 4
        es = slice(e4 * NE_C, (e4 + 1) * NE_C)
        hT = np.ascontiguousarray(np.concatenate([h2T_cores[4 * g + k] for k in range(4)], axis=1))
        gt = np.concatenate([gate_cores[4 * g + k].reshape(17 * 128, 32) for k in range(4)], axis=0)[:, es]
        maps.append({"hT": hT, "gtT": np.ascontiguousarray(gt.T), "gt": np.ascontiguousarray(gt.reshape(NTG, 128, NE_C).transpose(1, 0, 2)),
                     "wgu": np.ascontiguousarray(w_gu_l[es]), "bgu": np.ascontiguousarray(b_gu_l[es].reshape(NE_C, 16, 128).transpose(2, 0, 1)),
                     "wdn": np.ascontiguousarray(w_dn_l[es]), "bdn": np.ascontiguousarray(b_dn_l[es])})
    res = run(nc, maps)
    parts = []
    for tc in range(NCORES):
        g, k = tc // 4, tc % 4
        parts.append([np.ascontiguousarray(res[4 * g + e4]["part"][k * 17:(k + 1) * 17]) for e4 in range(4)])
    return parts


def build_final(npart=4):
    P = Prog()
    NL = 16
    xt = P.inp("xt", [NL, 128, D]); parts = [P.inp("part%d" % k, [NL, 128, D]) for k in range(npart)]
    g5 = P.inp("g5", [1, D]); fw = P.inp("fw", [1, D]); out = P.out("out", [NL, 128, D])
    g5b = P.sb("g5b", [128, 1, D]); fwb = P.sb("fwb", [128, D]); pt_tiles = [P.sb("ptl%d" % i, [128, D]) for i in range(2)] + [P.sb("pacc", [128, D])]
    xs = [P.sb("xs%d" % i, [128, D]) for i in range(2)]; sq = P.sb("sq", [128, D]); ss = P.sb("ss", [128, 1]); rs = P.sb("rs", [128, 1])
    ob = [P.sb("ob%d" % i, [128, D]) for i in range(2)]
    P.dma("sp", g5b[:, 0, :], g5[0:1, :].partition_broadcast(128), writes=["g5b"]); P.dma("sp", fwb[:], fw[0:1, :].partition_broadcast(128), writes=["fwb"])
    ccnt = [0]

    def tile(t):
        x_ = xs[t % 2]; xk = "xs%d" % (t % 2); o_ = ob[t % 2]; ok = "ob%d" % (t % 2)
        P.dma("sp", x_[:], xt[t], writes=[xk])
        emit_combine(P, x_, xk, t, parts, g5b, 0, pt_tiles, ccnt)
        P.op("act", lambda e: e.activation(out=sq[:], in_=x_[:], func=AF.Square, accum_out=ss[:]), reads=[xk], writes=["sq", "ss"])
        P.op("dve", lambda e: e.tensor_scalar(out=rs[:], in0=ss[:], scalar1=1.0 / D, scalar2=EPS, op0=ALU.mult, op1=ALU.add), reads=["ss"], writes=["rs"])
        P.op("act", lambda e: e.activation(out=rs[:], in_=rs[:], func=AF.Sqrt), reads=["rs"], writes=["rs"])
        P.op("dve", lambda e: e.reciprocal(out=rs[:], in_=rs[:]), reads=["rs"], writes=["rs"])
        P.op("dve", lambda e: e.scalar_tensor_tensor(out=o_[:], in0=x_[:], scalar=rs[:, 0:1], in1=fwb[:], op0=ALU.mult, op1=ALU.mult), reads=[xk, "rs", "fwb"], writes=[ok])
        P.dma("sp", out[t], o_[:], reads=[ok], writes=["out"])

    for t in range(NL):
        tile(t)
    return P.finish(["out"])


def launch_final(xts, parts, mod_last, fw):
    nc = build_final(len(parts[0]))
    m6 = mod_last.reshape(5, 6, D)
    maps = []
    for c in range(NCORES):
        b = c // 2
        m = {"xt": np.ascontiguousarray(xts[c][1:]), "g5": np.ascontiguousarray(m6[b, 5][None]), "fw": np.ascontiguousarray(fw[None])}
        for k in range(len(parts[c])):
            m["part%d" % k] = np.ascontiguousarray(parts[c][k][1:])
        maps.append(m)
    res = run(nc, maps)
    out = np.empty((4, 4096, D), np.float32)
    for c in range(NCORES):
        b, h = c // 2, c % 2
        out[b, h * 2048:(h + 1) * 2048] = res[c]["out"].reshape(2048, D)
    return out


def kernel(**inp):
    inp = {k: np.asarray(v) for k, v in inp.items()}
    G = 512
    mod = launch_mod(inp["c"], inp["c_ctx"], inp["ada_w"], inp["ada_b"])
    xts = tile_split(inp["x"], inp["ctx"])
    parts = None
    vfl = vfc = None
    for l in range(2):
        last = (l == 1)
        ul, uc, xts = launch_win(xts, mod[l], np.ascontiguousarray(inp["w_in"][l]), inp["norm1_w"][l], parts, mod[l - 1] if l > 0 else None)
        hp = {k: inp[k][l] for k in inp if k.startswith("hy_")}
        hy_l, hy_c = launch_hyena(ul, uc, hp, with_ctx=not last)
        if hy_c is None:
            hy_c = np.zeros((4, 256, G), np.float32)
        rg_l, rg_c = launch_rglru(ul, uc, inp["rg_conv_w"][l], inp["rg_conv_b"][l], inp["rg_wa"][l], inp["rg_ba"][l], inp["rg_wx"][l], inp["rg_bx"][l], inp["rg_lambda"][l])
        rp = {k: inp[k][l] for k in inp if k.startswith("rw_") and k not in ("rw_v0", "rw_v1", "rw_v2")}
        if l == 0:
            vfl = np.ascontiguousarray(ul[..., 7 * G:8 * G]); vfc = np.ascontiguousarray(uc[..., 7 * G:8 * G])
            rp["rw_v0"] = np.zeros(G, np.float32); rp["rw_v1"] = np.zeros((G, 32), np.float32); rp["rw_v2"] = np.zeros((32, G), np.float32)
        else:
            rp["rw_v0"] = inp["rw_v0"][l - 1]; rp["rw_v1"] = inp["rw_v1"][l - 1]; rp["rw_v2"] = inp["rw_v2"][l - 1]
        rw_l, rw_c = launch_rwkv(ul, uc, vfl, vfc, rp, l)
        rt_l, rt_c = launch_ret(ul, uc, inp["rt_decay"][l], inp["rt_gn_w"][l])
        del ul, uc
        mix_l = np.concatenate([hy_l, rg_l, rw_l, rt_l], -1); mix_c = np.concatenate([hy_c, rg_c, rw_c, rt_c], -1)
        x1, h2T, gate = launch_post(mix_l, mix_c, xts, mod[l], np.ascontiguousarray(inp["w_out"][l]), inp["norm2_w"][l],
                                    np.ascontiguousarray(inp["moe_router_w"][l]), inp["moe_router_b"][l])
        parts = launch_moe(h2T, gate, inp["moe_w_gu"][l], inp["moe_b_gu"][l], inp["moe_w_dn"][l], inp["moe_b_dn"][l])
        xts = x1
    return launch_final(xts, parts, mod[1], inp["final_norm_w"])
```

```python
import numpy as np
import concourse.bass as bass
import concourse.mybir as mybir
from contextlib import ExitStack

F32 = mybir.dt.float32
BF16 = mybir.dt.bfloat16
I32 = mybir.dt.int32
U32 = mybir.dt.uint32
AF = mybir.ActivationFunctionType
ALU = mybir.AluOpType
AX = mybir.AxisListType

ENGS = ("pe", "act", "dve", "pool", "sp")
N_DMA_SLOTS = 8


class Prog:
    def __init__(self):
        self.nc = bass.Bass("TRN2", target_bir_lowering=False)
        self.stack = ExitStack()
        self.ops = {e: [] for e in ENGS}
        self.count = {e: 0 for e in ENGS}
        self.last_write = {}
        self.readers = {}
        self.waited = {e: {} for e in ENGS}
        self.dma_slot_uses = {}
        self.dma_rr = {e: 0 for e in ENGS}
        self.sem_names = set()
        self.psum_keys = set()
        self.relax_dve = False

    def dram(self, name, shape, dtype=F32, kind="Internal"):
        return self.nc.dram_tensor(name, list(shape), dtype, kind=kind).ap()

    def inp(self, name, shape, dtype=F32):
        return self.dram(name, shape, dtype, "ExternalInput")

    def out(self, name, shape, dtype=F32):
        return self.dram(name, shape, dtype, "ExternalOutput")

    def sb(self, name, shape, dtype=F32):
        return self.stack.enter_context(self.nc.sbuf_tensor(name, list(shape), dtype))

    def ps(self, name, shape, dtype=F32):
        self.psum_keys.add(name)
        return self.stack.enter_context(self.nc.psum_tensor(name, list(shape), dtype))

    def _deps(self, eng, reads, writes):
        toks = set()
        for k in reads:
            t = self.last_write.get(k)
            if t is not None:
                toks.add(t)
            if k in self.psum_keys:
                for t in self.readers.get(k, ()):
                    if t[2] != eng:
                        toks.add(t)
        for k in writes:
            t = self.last_write.get(k)
            if t is not None:
                toks.add(t)
            for t in self.readers.get(k, ()):
                toks.add(t)
        waits = []
        for (sem, val, src_eng) in toks:
            if src_eng == "pe" and eng == "pe":
                continue
            if self.relax_dve and src_eng == "dve" and eng == "dve" and self.count["dve"] - val >= 1:
                continue
            if self.waited[eng].get(sem, 0) >= val:
                continue
            waits.append((sem, val))
        best = {}
        for sem, val in waits:
            best[sem] = max(best.get(sem, 0), val)
        for sem, val in best.items():
            self.waited[eng][sem] = val
        return list(best.items())

    def _commit(self, tok, reads, writes):
        for k in writes:
            self.last_write[k] = tok
            self.readers[k] = []
        for k in reads:
            self.readers.setdefault(k, []).append(tok)

    def op(self, eng, fn, reads=(), writes=()):
        reads = list(reads); writes = list(writes)
        waits = self._deps(eng, reads, writes)
        self.count[eng] += 1
        sem = "c_" + eng
        self.sem_names.add(sem)
        tok = (sem, self.count[eng], eng)
        self.ops[eng].append((fn, waits, (sem, 1)))
        self._commit(tok, reads, writes)

    def dma(self, eng, out, in_, reads=(), writes=(), **kw):
        reads = list(reads); writes = list(writes)
        slot = self.dma_rr[eng] % N_DMA_SLOTS
        self.dma_rr[eng] += 1
        sem = "d_%s_%d" % (eng, slot)
        self.sem_names.add(sem)
        uses = self.dma_slot_uses.get((eng, slot), 0)
        waits = dict(self._deps(eng, reads, writes))
        if uses > 0 and self.waited[eng].get(sem, 0) < 16 * uses:
            waits[sem] = 16 * uses
            self.waited[eng][sem] = 16 * uses
        uses += 1
        self.dma_slot_uses[(eng, slot)] = uses
        tok = (sem, 16 * uses, "dma")

        def fn(e, out=out, in_=in_, kw=kw):
            return e.dma_start(out=out, in_=in_, **kw)
        self.ops[eng].append((fn, list(waits.items()), (sem, 16)))
        self._commit(tok, reads, writes)

    def finish(self, final_keys):
        waits = self._deps("sp", list(final_keys), [])
        nc = self.nc
        sems = {}
        for name in sorted(self.sem_names):
            sems[name] = self.stack.enter_context(nc.semaphore(name))
        engmap = {"pe": "tensor", "act": "scalar", "dve": "vector", "pool": "gpsimd", "sp": "sync"}
        with nc.Block() as block:
            for eng in ENGS:
                oplist = self.ops[eng]
                extra = waits if eng == "sp" else []
                if not oplist and not extra:
                    continue

                def body(e, oplist=oplist, extra=extra):
                    for fn, ws, inc in oplist:
                        for sem, val in ws:
                            e.wait_ge(sems[sem], val)
                        ins = fn(e)
                        ins.then_inc(sems[inc[0]], inc[1])
                    for sem, val in extra:
                        e.wait_ge(sems[sem], val)
                getattr(block, engmap[eng])(body)
        self.stack.close()
        return nc

    def n_instr(self):
        return {e: len(self.ops[e]) for e in ENGS}

from concourse.bass_utils import run_bass_kernel_spmd

D = 2048
NCORES = 8
EPS = 1e-6


def run(nc, in_maps):
    res = run_bass_kernel_spmd(nc, in_maps, core_ids=list(range(len(in_maps))))
    return res.results


def build_mod():
    P = Prog()
    condT = P.inp("condT", [128, 16, 5])
    aw = P.inp("aw", [2, D, 1536])
    ab = P.inp("ab", [2, 1, 1536])
    mod = P.out("mod", [2, 5, 1536])
    ct = P.sb("ct", [128, 16, 5]); sg = P.sb("sg", [128, 16, 5]); awt = P.sb("awt", [128, 16, 1536])
    abt = P.sb("abt", [1, 2, 1536]); ones = P.sb("ones", [1, 8]); ot = P.sb("ot", [5, 2, 1536])
    ps = [P.ps("ps%d" % i, [5, 512]) for i in range(2)]
    P.dma("sp", ct[:], condT[:, :, :], writes=["ct"])
    P.dma("sp", abt[:], ab.rearrange("l o n -> o l n"), writes=["abt"])
    P.op("dve", lambda e: e.memset(ones[:], 1.0), writes=["ones"])
    P.op("act", lambda e: e.activation(out=sg[:], in_=ct[:], func=AF.Sigmoid), reads=["ct"], writes=["sg"])
    P.op("dve", lambda e: e.tensor_tensor(out=ct[:], in0=ct[:], in1=sg[:], op=ALU.mult), reads=["ct", "sg"], writes=["ct"])
    k = 0
    for l in range(2):
        P.dma("sp", awt[:], aw[l].rearrange("(kc p) n -> p kc n", p=128), writes=["awt"])
        for n in range(3):
            pp = ps[k % 2]; key = "ps%d" % (k % 2); k += 1
            for kc in range(16):
                P.op("pe", lambda e, pp=pp, kc=kc, n=n: e.matmul(pp[:], ct[:, kc, :], awt[:, kc, n * 512:(n + 1) * 512], start=(kc == 0), stop=False),
                     reads=["ct", "awt"], writes=[key])
            P.op("pe", lambda e, pp=pp, l=l, n=n: e.matmul(pp[:], ones[:, 0:5], abt[:, l, n * 512:(n + 1) * 512], start=False, stop=True),
                 reads=["ones", "abt"], writes=[key])
            P.op("dve", lambda e, pp=pp, l=l, n=n: e.tensor_copy(out=ot[:, l, n * 512:(n + 1) * 512], in_=pp[:]), reads=[key], writes=["ot"])
    P.dma("sp", mod.rearrange("l r n -> r l n"), ot[:], reads=["ot"], writes=["mod"])
    return P.finish(["mod"])


def launch_mod(c, c_ctx, ada_w, ada_b):
    cond = np.concatenate([c, c_ctx[None]], 0)
    condT = np.ascontiguousarray(cond.T.reshape(16, 128, 5).transpose(1, 0, 2))
    nc = build_mod()
    maps = []
    for i in range(NCORES):
        maps.append({"condT": condT,
                     "aw": np.ascontiguousarray(ada_w[:, :, i * 1536:(i + 1) * 1536]),
                     "ab": np.ascontiguousarray(ada_b[:, None, i * 1536:(i + 1) * 1536])})
    res = run(nc, maps)
    return np.concatenate([r["mod"] for r in res], axis=2)


NT = 17
DIN = 6656


def emit_norm_mod(P, xt_ap, xkey, A, S, which, hb, hkey, tmp, sq, ss, rs, tmpk="tmp"):
    P.op("act", lambda e: e.activation(out=sq[:], in_=xt_ap, func=AF.Square, accum_out=ss[:]), reads=[xkey], writes=["sq", "ss"])
    P.op("dve", lambda e: e.tensor_scalar(out=rs[:], in0=ss[:], scalar1=1.0 / D, scalar2=EPS, op0=ALU.mult, op1=ALU.add), reads=["ss"], writes=["rs"])
    P.op("act", lambda e: e.activation(out=rs[:], in_=rs[:], func=AF.Sqrt), reads=["rs"], writes=["rs"])
    P.op("dve", lambda e: e.reciprocal(out=rs[:], in_=rs[:]), reads=["rs"], writes=["rs"])
    P.op("dve", lambda e: e.scalar_tensor_tensor(out=tmp[:], in0=xt_ap, scalar=rs[:, 0:1], in1=A[:, which, :], op0=ALU.mult, op1=ALU.mult),
         reads=[xkey, "rs", "A"], writes=[tmpk])
    P.op("pool", lambda e: e.tensor_tensor(out=hb, in0=tmp[:], in1=S[:, which, :], op=ALU.add), reads=[tmpk, "S"], writes=[hkey])


def emit_AS(P, msh, nw, A, S, nwb, nwbk="nwb"):
    P.dma("sp", nwb[:], nw[0:1, :].partition_broadcast(128), writes=[nwbk])
    for wch in range(2):
        P.dma("sp", S[:, wch, :], msh[wch, 0:1, :].partition_broadcast(128), writes=["S"])
        P.dma("sp", A[:, wch, :], msh[wch, 1:2, :].partition_broadcast(128), writes=["A"])
    P.op("dve", lambda e: e.scalar_tensor_tensor(out=A[:, 0, :], in0=A[:, 0, :], scalar=1.0, in1=nwb[:], op0=ALU.add, op1=ALU.mult), reads=["A", nwbk], writes=["A"])
    P.op("dve", lambda e: e.scalar_tensor_tensor(out=A[:, 1, :], in0=A[:, 1, :], scalar=1.0, in1=nwb[:], op0=ALU.add, op1=ALU.mult), reads=["A", nwbk], writes=["A"])


def emit_transpose_tile(P, hb, hkey, hT, t, ident, pst, pstkeys, cnt):
    for g in range(4):
        i = cnt[0] % len(pst); cnt[0] += 1
        pt = pst[i]; pk = pstkeys[i]
        for j in range(4):
            kc = g * 4 + j
            P.op("pe", lambda e, pt=pt, j=j, kc=kc: e.transpose(pt[:, j, :], hb[:, kc * 128:(kc + 1) * 128], ident[:]), reads=[hkey, "ident"], writes=[pk])
        eng = "act" if g % 2 == 0 else "dve"
        if eng == "act":
            P.op("act", lambda e, pt=pt, g=g: e.activation(out=hT[:, g * 4:(g + 1) * 4, t * 128:(t + 1) * 128], in_=pt[:], func=AF.Copy), reads=[pk], writes=[("hT", t)])
        else:
            P.op("dve", lambda e, pt=pt, g=g: e.tensor_copy(out=hT[:, g * 4:(g + 1) * 4, t * 128:(t + 1) * 128], in_=pt[:]), reads=[pk], writes=[("hT", t)])


def emit_ident(P, ident, dtype_tmp=None):
    P.op("pool", lambda e: e.memset(ident[:], 1.0), writes=["ident"])
    P.op("pool", lambda e: e.affine_select(out=ident[:], in_=ident[:], pattern=[[-1, 128]], compare_op=ALU.is_equal, fill=0.0, base=0, channel_multiplier=1),
         reads=["ident"], writes=["ident"])


def emit_combine(P, x_, xk, t, parts, g5b, which, pt_tiles, cnt):
    acc = pt_tiles[-1]
    nl = len(pt_tiles) - 1
    for k, pa in enumerate(parts):
        if k == 0:
            P.dma("act", acc[:], pa[t], writes=["pacc"])
        else:
            pt = pt_tiles[cnt[0] % nl]; pk = "ptl%d" % (cnt[0] % nl); cnt[0] += 1
            P.dma("act" if k % 2 else "sp", pt[:], pa[t], writes=[pk])
            P.op("pool", lambda e, pt=pt: e.tensor_tensor(out=acc[:], in0=acc[:], in1=pt[:], op=ALU.add), reads=["pacc", pk], writes=["pacc"])
    P.op("dve", lambda e: e.tensor_tensor(out=acc[:], in0=acc[:], in1=g5b[:, which, :], op=ALU.mult), reads=["pacc", "g5b"], writes=["pacc"])
    P.op("dve", lambda e: e.tensor_tensor(out=x_[:], in0=x_[:], in1=acc[:], op=ALU.add), reads=["pacc", xk], writes=[xk])


def build_win(npart=0):
    P = Prog()
    xt = P.inp("xt", [NT, 128, D]); msh = P.inp("msh", [2, 2, D]); nw = P.inp("nw", [1, D]); w = P.inp("w", [D, DIN])
    u = P.out("u", [NT, 128, DIN])
    if npart:
        parts = [P.inp("part%d" % k, [NT, 128, D]) for k in range(npart)]
        g5 = P.inp("g5", [2, D]); xo = P.out("xo", [NT, 128, D])
        g5b = P.sb("g5b", [128, 2, D]); pt_tiles = [P.sb("ptl%d" % i, [128, D]) for i in range(1)] + [P.sb("pacc", [128, D])]
        for wch in range(2):
            P.dma("sp", g5b[:, wch, :], g5[wch:wch + 1, :].partition_broadcast(128), writes=["g5b"])
        ccnt = [0]
    A = P.sb("A", [128, 2, D]); S = P.sb("S", [128, 2, D])
    xs = [P.sb("xs%d" % i, [128, D]) for i in range(2)]
    sq = P.sb("sq", [128, D]); ss = P.sb("ss", [128, 1]); rs = P.sb("rs", [128, 1])
    tmp = sq; nwb = sq
    hb = [P.sb("hb%d" % i, [128, D], BF16) for i in range(2)]
    hT = P.sb("hT", [128, 16, NT * 128], BF16)
    ident = P.sb("ident", [128, 128], BF16)
    wb = [P.sb("wb%d" % i, [128, 16, 512], BF16) for i in range(2)]
    ob = [P.sb("ob%d" % i, [128, 512]) for i in range(3)]
    pst = [P.ps("pst%d" % i, [128, 4, 128], BF16) for i in range(2)]
    psm = [P.ps("psm%d" % i, [128, 512]) for i in range(4)]
    emit_ident(P, ident)
    emit_AS(P, msh, nw, A, S, nwb, "sq")
    cnt = [0]
    for t in range(NT):
        x_ = xs[t % 2]; xk = "xs%d" % (t % 2); h_ = hb[t % 2]; hk = "hb%d" % (t % 2)
        P.dma("sp", x_[:], xt[t], writes=[xk])
        if npart:
            emit_combine(P, x_, xk, t, parts, g5b, 0 if t == 0 else 1, pt_tiles, ccnt)
            P.dma("sp", xo[t], x_[:], reads=[xk], writes=["xo"])
        emit_norm_mod(P, x_[:], xk, A, S, 0 if t == 0 else 1, h_[:], hk, tmp, sq, ss, rs, "sq")
        emit_transpose_tile(P, h_, hk, hT, t, ident, pst, ["pst0", "pst1"], cnt)
    k = 0
    for n in range(13):
        wt = wb[n % 2]; wk = "wb%d" % (n % 2)
        P.dma("pool", wt[:], w[:, n * 512:(n + 1) * 512].rearrange("(kc p) n -> p kc n", p=128), writes=[wk])
        for t in range(NT):
            pp = psm[k % 4]; pk = "psm%d" % (k % 4); o_ = ob[k % 3]; ok = "ob%d" % (k % 3)
            for kc in range(16):
                P.op("pe", lambda e, pp=pp, kc=kc, t=t, wt=wt: e.matmul(pp[:], hT[:, kc, t * 128:(t + 1) * 128], wt[:, kc, :], start=(kc == 0), stop=(kc == 15)),
                     reads=[("hT", t), wk], writes=[pk])
            if k % 2 == 0:
                P.op("act", lambda e, pp=pp, o_=o_: e.activation(out=o_[:], in_=pp[:], func=AF.Copy), reads=[pk], writes=[ok])
            else:
                P.op("dve", lambda e, pp=pp, o_=o_: e.tensor_copy(out=o_[:], in_=pp[:]), reads=[pk], writes=[ok])
            P.dma("sp", u[t, :, n * 512:(n + 1) * 512], o_[:], reads=[ok], writes=["u"])
            k += 1
    return P.finish(["u"] + (["xo"] if npart else []))


def tile_split(lat, ctxv):
    outs = []
    for c in range(NCORES):
        b, h = c // 2, c % 2
        ct = ctxv[b, h * 128:(h + 1) * 128][None]
        lt = lat[b, h * 2048:(h + 1) * 2048].reshape(16, 128, -1)
        outs.append(np.ascontiguousarray(np.concatenate([ct, lt], 0)))
    return outs


def tile_merge(parts):
    X = parts[0].shape[-1]
    lat = np.empty((4, 4096, X), parts[0].dtype); ctxv = np.empty((4, 256, X), parts[0].dtype)
    for c in range(NCORES):
        b, h = c // 2, c % 2
        ctxv[b, h * 128:(h + 1) * 128] = parts[c][0]
        lat[b, h * 2048:(h + 1) * 2048] = parts[c][1:].reshape(2048, X)
    return lat, ctxv


def launch_win(xts, mod_l, w_in_l, nw_l, parts=None, mod_prev=None):
    npart = 0 if parts is None else len(parts[0])
    nc = build_win(npart)
    m6 = mod_l.reshape(5, 6, D)
    maps = []
    for c in range(NCORES):
        b = c // 2
        msh = np.ascontiguousarray(np.stack([m6[4, 0:2], m6[b, 0:2]], 0))
        m = {"xt": xts[c], "msh": msh, "nw": np.ascontiguousarray(nw_l[None]), "w": w_in_l}
        if npart:
            p6 = mod_prev.reshape(5, 6, D)
            m["g5"] = np.ascontiguousarray(np.stack([p6[4, 5], p6[b, 5]], 0))
            for k in range(npart):
                m["part%d" % k] = parts[c][k]
        maps.append(m)
    res = run(nc, maps)
    ul, uc = tile_merge([r["u"] for r in res])
    xo = [r["xo"] for r in res] if npart else xts
    return ul, uc, xo


def rev(ap):
    a = [list(x) for x in ap.ap]
    assert len(a) == 2
    st, n = a[1]
    return bass.AP(ap.tensor, ap.offset + st * (n - 1), [a[0], [-st, n]])


def dview(ap, d):
    return ap if d == 0 else rev(ap)


TS = 4352
SEGS = ((0, 256), (256, 4352))


def build_rglru():
    P = Prog()
    xT = P.inp("xT", [4, 64, TS]); gT = P.inp("gT", [4, 64, TS])
    cw = P.inp("cw", [64, 2, 4]); vec = P.inp("vec", [64, 2, 4])
    wa = P.inp("wa", [2, 64, 64]); wx = P.inp("wx", [2, 64, 64])
    oT = P.out("oT", [4, 64, TS])
    cwt = P.sb("cwt", [64, 2, 4]); vt = P.sb("vt", [64, 2, 4]); wat = P.sb("wat", [64, 2, 64]); wxt = P.sb("wxt", [64, 2, 64])
    cl = P.sb("cl", [64, 2]); tl = P.sb("tl", [64, 2])
    x = P.sb("x", [64, TS]); g = P.sb("g", [64, TS]); xc = P.sb("xc", [64, TS]); r = P.sb("r", [64, TS]); ii = P.sb("ii", [64, TS])
    a = P.sb("a", [64, TS]); h0 = P.sb("h0", [64, TS]); h1 = P.sb("h1", [64, TS])
    ps = [P.ps("ps%d" % i, [64, 512]) for i in range(4)]
    P.dma("sp", cwt[:], cw[:, :, :], writes=["cwt"]); P.dma("sp", vt[:], vec[:, :, :], writes=["vt"])
    P.dma("sp", wat[:], wa.rearrange("d i j -> i d j"), writes=["wat"]); P.dma("sp", wxt[:], wx.rearrange("d i j -> i d j"), writes=["wxt"])
    P.op("act", lambda e: e.activation(out=tl[:], in_=vt[:, :, 3], func=AF.Exp, scale=-1.0), reads=["vt"], writes=["tl"])
    P.op("act", lambda e: e.activation(out=tl[:], in_=tl[:], func=AF.Ln, bias=1.0), reads=["tl"], writes=["tl"])
    P.op("dve", lambda e: e.tensor_scalar(out=cl[:], in0=tl[:], scalar1=-8.0, scalar2=None, op0=ALU.mult), reads=["tl"], writes=["cl"])
    k = 0
    for b in range(4):
        P.dma("sp", x[:], xT[b], writes=["x"]); P.dma("sp", g[:], gT[b], writes=["g"])
        for d in range(2):
            h = h0 if d == 0 else h1; hk = "h%d" % d
            P.op("dve", lambda e, d=d: e.tensor_scalar(out=xc[:], in0=x[:], scalar1=cwt[:, d, 3:4], scalar2=vt[:, d, 0:1], op0=ALU.mult, op1=ALU.add),
                 reads=["x", "cwt", "vt"], writes=["xc"])
            for (s0, s1) in SEGS:
                n = s1 - s0
                for j in range(3):
                    sh = 3 - j
                    xo = dview(xc[:, s0:s1], d); xi = dview(x[:, s0:s1], d)
                    P.op("dve", lambda e, xo=xo, xi=xi, sh=sh, n=n, d=d, j=j: e.scalar_tensor_tensor(out=xo[:, sh:n], in0=xi[:, 0:n - sh], scalar=cwt[:, d, j:j + 1], in1=xo[:, sh:n], op0=ALU.mult, op1=ALU.add),
                         reads=["x", "xc", "cwt"], writes=["xc"])
            for ci in range(0, TS, 512):
                ce = min(TS, ci + 512); w_ = ce - ci
                pa = ps[k % 4]; pak = "ps%d" % (k % 4); k += 1
                px = ps[k % 4]; pxk = "ps%d" % (k % 4); k += 1
                P.op("pe", lambda e, pa=pa, ci=ci, ce=ce, w_=w_, d=d: e.matmul(pa[:, 0:w_], wat[:, d, :], xc[:, ci:ce], start=True, stop=True), reads=["wat", "xc"], writes=[pak])
                P.op("pe", lambda e, px=px, ci=ci, ce=ce, w_=w_, d=d: e.matmul(px[:, 0:w_], wxt[:, d, :], xc[:, ci:ce], start=True, stop=True), reads=["wxt", "xc"], writes=[pxk])
                P.op("act", lambda e, pa=pa, ci=ci, ce=ce, w_=w_, d=d: e.activation(out=r[:, ci:ce], in_=pa[:, 0:w_], func=AF.Sigmoid, bias=vt[:, d, 1:2]), reads=[pak, "vt"], writes=["r"])
                P.op("act", lambda e, px=px, ci=ci, ce=ce, w_=w_, d=d: e.activation(out=ii[:, ci:ce], in_=px[:, 0:w_], func=AF.Sigmoid, bias=vt[:, d, 2:3]), reads=[pxk, "vt"], writes=["ii"])
            P.op("act", lambda e, d=d: e.activation(out=a[:], in_=r[:], func=AF.Exp, scale=cl[:, d:d + 1]), reads=["r", "cl"], writes=["a"])
            P.op("dve", lambda e: e.tensor_tensor(out=r[:], in0=a[:], in1=a[:], op=ALU.mult), reads=["a"], writes=["r"])
            P.op("act", lambda e: e.activation(out=r[:], in_=r[:], func=AF.Sqrt, scale=-1.0, bias=1.0), reads=["r"], writes=["r"])
            P.op("dve", lambda e: e.tensor_tensor(out=ii[:], in0=ii[:], in1=xc[:], op=ALU.mult), reads=["ii", "xc"], writes=["ii"])
            P.op("dve", lambda e: e.tensor_tensor(out=r[:], in0=r[:], in1=ii[:], op=ALU.mult), reads=["r", "ii"], writes=["r"])
            hv = dview(h[:, 0:256], d); av = dview(a[:, 0:256], d); bv = dview(r[:, 0:256], d)
            P.op("dve", lambda e, hv=hv, av=av, bv=bv: e.tensor_tensor_scan(out=hv, data0=av, data1=bv, initial=0.0, op0=ALU.mult, op1=ALU.add), reads=["a", "r"], writes=[hk])
            init = h[:, 255:256] if d == 0 else h[:, 0:1]
            hv = dview(h[:, 256:TS], d); av = dview(a[:, 256:TS], d); bv = dview(r[:, 256:TS], d)
            P.op("dve", lambda e, hv=hv, av=av, bv=bv, init=init: e.tensor_tensor_scan(out=hv, data0=av, data1=bv, initial=init, op0=ALU.mult, op1=ALU.add), reads=["a", "r", hk], writes=[hk])
        P.op("act", lambda e: e.activation(out=g[:], in_=g[:], func=AF.Gelu), reads=["g"], writes=["g"])
        P.op("dve", lambda e: e.tensor_tensor(out=h0[:], in0=h0[:], in1=h1[:], op=ALU.add), reads=["h0", "h1"], writes=["h0"])
        P.op("dve", lambda e: e.tensor_tensor(out=h0[:], in0=h0[:], in1=g[:], op=ALU.mult), reads=["h0", "g"], writes=["h0"])
        P.dma("sp", oT[b], h0[:], reads=["h0"], writes=["oT"])
    return P.finish(["oT"])


def scanT(ul, uc, c0, c1):
    return np.ascontiguousarray(np.concatenate([uc[:, :, c0:c1], ul[:, :, c0:c1]], axis=1).transpose(0, 2, 1))


def unscanT(oT):
    o = oT.transpose(0, 2, 1)
    return np.ascontiguousarray(o[:, 256:]), np.ascontiguousarray(o[:, :256])


def launch_rglru(ul, uc, conv_w, conv_b, wa, ba, wx, bx, lam, cores=range(NCORES)):
    G = 512
    nc = build_rglru()
    maps = []
    for c in cores:
        sl = slice(c * 64, (c + 1) * 64)
        maps.append({
            "xT": scanT(ul, uc, 3 * G + c * 64, 3 * G + (c + 1) * 64),
            "gT": scanT(ul, uc, 4 * G + c * 64, 4 * G + (c + 1) * 64),
            "cw": np.ascontiguousarray(conv_w[:, :, sl].transpose(2, 0, 1)),
            "vec": np.ascontiguousarray(np.stack([conv_b[:, sl], ba[:, sl], bx[:, sl], lam[:, sl]], -1).transpose(1, 0, 2)),
            "wa": np.ascontiguousarray(wa[:, c]), "wx": np.ascontiguousarray(wx[:, c])})
    res = run(nc, maps)
    oT = np.concatenate([r["oT"] for r in res], axis=1)
    return unscanT(oT)


def seg_chunks(W):
    out = []
    for si, (s0, s1) in enumerate(SEGS):
        c = s0
        while c < s1:
            out.append((si, c, min(c + W, s1)))
            c += W
    return out


def tau_of(col):
    return 255 - col if col < 256 else 4607 - col


RW_RELAX = True


def build_rwkv(debug=False):
    P = Prog()
    NS = TS
    rT = P.inp("rT", [4, 64, NS]); kT = P.inp("kT", [4, 64, NS]); vT = P.inp("vT", [4, 64, NS]); vfT = P.inp("vfT", [4, 64, NS])
    zT = P.inp("zT", [4, 512, NS])
    mu_h = P.inp("mu_h", [64, 2, 3]); mu_z = P.inp("mu_z", [128, 4, 2])
    w1 = P.inp("w1", [2, 512, 64]); w2 = P.inp("w2", [2, 64, 64]); a1 = P.inp("a1", [2, 512, 64]); a2 = P.inp("a2", [2, 64, 64])
    g1 = P.inp("g1", [512, 128]); g2 = P.inp("g2", [128, 64]); v1 = P.inp("v1", [512, 32]); v2 = P.inp("v2", [32, 64])
    hv = P.inp("hv", [64, 10])
    seli = P.inp("sel", [8, 4, 128])
    oT = P.out("oT", [4, 64, NS])
    kind = "ExternalOutput" if debug else "Internal"
    X = P.dram("X", [2, NS, 4, 5, 64], F32, kind); Vd = P.dram("Vd", [2, 64, 4, NS], F32, kind)
    Bd = P.dram("Bd", [2, 64, 4, NS], F32, kind); Gd = P.dram("Gd", [64, 4, NS], F32, kind); Yd = P.dram("Yd", [2, 64, 4, NS], F32, kind)
    SAd = P.dram("SAd", [2, 64, 4, NS], F32, kind); C1d = P.dram("C1d", [2, 64, 4, NS], F32, kind); C2d = P.dram("C2d", [2, 64, 4, NS], F32, kind)

    mh = P.sb("mh", [64, 2, 3]); mh1 = P.sb("mh1", [64, 2, 3]); mz = P.sb("mz", [128, 4, 2]); mz1 = P.sb("mz1", [128, 4, 2])
    w1t = P.sb("w1t", [128, 2, 4, 64]); a1t = P.sb("a1t", [128, 2, 4, 64]); w2t = P.sb("w2t", [64, 2, 64]); a2t = P.sb("a2t", [64, 2, 64])
    g1t = P.sb("g1t", [128, 4, 128]); g2t = P.sb("g2t", [128, 64]); v1t = P.sb("v1t", [128, 4, 32]); v2t = P.sb("v2t", [32, 64])
    hvt = P.sb("hvt", [128, 10]); ones64 = P.sb("ones64", [64, 64]); ident = P.sb("identf", [128, 128])
    blk = P.sb("blk", [128, 128]); F0 = P.sb("F0", [128, 64]); F1 = P.sb("F1", [128, 64])
    P.dma("sp", mh[:], mu_h[:, :, :], writes=["mh"]); P.dma("sp", mz[:], mu_z[:, :, :], writes=["mz"])
    for d in range(2):
        P.dma("sp", w1t[:, d], w1[d].rearrange("(pt p) n -> p pt n", p=128), writes=["w1t"])
        P.dma("sp", a1t[:, d], a1[d].rearrange("(pt p) n -> p pt n", p=128), writes=["a1t"])
        P.dma("sp", w2t[:, d, :], w2[d], writes=["w2t"]); P.dma("sp", a2t[:, d, :], a2[d], writes=["a2t"])
    P.dma("sp", g1t[:], g1.rearrange("(pt p) n -> p pt n", p=128), writes=["g1t"]); P.dma("sp", g2t[:], g2[:, :], writes=["g2t"])
    P.dma("sp", v1t[:], v1.rearrange("(pt p) n -> p pt n", p=128), writes=["v1t"]); P.dma("sp", v2t[:], v2[:, :], writes=["v2t"])
    P.dma("sp", hvt[0:64, :], hv[:, :], writes=["hvt"]); P.dma("sp", hvt[64:128, :], hv[:, :], writes=["hvt"])
    P.op("dve", lambda e: e.tensor_scalar(out=mh1[:], in0=mh[:], scalar1=-1.0, scalar2=1.0, op0=ALU.mult, op1=ALU.add), reads=["mh"], writes=["mh1"])
    P.op("dve", lambda e: e.tensor_scalar(out=mz1[:], in0=mz[:], scalar1=-1.0, scalar2=1.0, op0=ALU.mult, op1=ALU.add), reads=["mz"], writes=["mz1"])
    P.op("dve", lambda e: e.memset(ones64[:], 1.0), writes=["ones64"])
    P.op("pool", lambda e: e.memset(ident[:], 1.0), writes=["identf"])
    P.op("pool", lambda e: e.affine_select(out=ident[:], in_=ident[:], pattern=[[-1, 128]], compare_op=ALU.is_equal, fill=0.0, base=0, channel_multiplier=1), reads=["identf"], writes=["identf"])
    P.op("dve", lambda e: e.memset(blk[:], 0.0), writes=["blk"])
    P.op("dve", lambda e: e.memset(blk[0:64, 0:64], 1.0 / 64), reads=["blk"], writes=["blk"])
    P.op("dve", lambda e: e.memset(blk[64:128, 64:128], 1.0 / 64), reads=["blk"], writes=["blk"])
    P.op("dve", lambda e: e.memset(F0[:], 0.0), writes=["F0"]); P.op("dve", lambda e: e.memset(F1[:], 0.0), writes=["F1"])
    P.op("dve", lambda e: e.tensor_copy(out=F0[0:64, :], in_=ident[0:64, 0:64]), reads=["identf", "F0"], writes=["F0"])
    P.op("dve", lambda e: e.tensor_copy(out=F1[64:128, :], in_=ident[64:128, 64:128]), reads=["identf", "F1"], writes=["F1"])
    W0 = lambda d: hvt[0:64, d:d + 1]
    A0 = lambda d: hvt[0:64, 2 + d:3 + d]
    V0 = hvt[0:64, 4:5]; KK_ = hvt[0:64, 5:6]; KA_ = hvt[0:64, 6:7]; RK_ = hvt[0:64, 7:8]

    zc = P.sb("zc", [128, 4, 512]); rc = P.sb("rc", [64, 512]); kc = P.sb("kc", [64, 512]); vc = P.sb("vc", [64, 512]); vfc = P.sb("vfc", [64, 512])
    hid = P.sb("hid", [128, 512]); h32 = P.sb("h32", [32, 512]); sgv = P.sb("sgv", [64, 512]); gq = P.sb("gq", [64, 512])
    xz = P.sb("xz", [128, 4, 512]); xr = P.sb("xr", [64, 512]); xk = P.sb("xk", [64, 512]); xv = P.sb("xv", [64, 512])
    hw = P.sb("hw", [64, 512]); dec = P.sb("dec", [64, 512]); ha = P.sb("ha", [64, 512]); aa = P.sb("aa", [64, 512])
    kk = P.sb("kk", [64, 512]); t1 = P.sb("t1", [64, 512]); kp = P.sb("kp", [64, 512]); kka = P.sb("kka", [64, 512]); bon = P.sb("bon", [64, 512])
    stg = P.sb("stg", [128, 5, 64]); rvb4 = P.sb("rvb4", [64, 4, 512]); rv5 = P.sb("rv5", [64, 5, 512])
    wrt = P.sb("wrt", [64, 512]); c1t = P.sb("c1t", [64, 512]); c2t = P.sb("c2t", [64, 512])
    pbig = P.ps("pbig", [128, 8, 512])
    pA = pbig[:, 0, :]; pB = pbig[:, 1, :]; pT = pbig[:, 2, 0:320].rearrange("p (v j) -> p v j", v=5)
    NPX = 5
    pX = [pbig[:, 3 + i, :] for i in range(NPX)]
    P.psum_keys.update(["pA", "pB", "pT"] + ["pX%d" % i for i in range(NPX)])

    def mm_z(ps, keyp, wt, wkey, d, nout, src, skey, W):
        for pt in range(4):
            lhs = wt[:, d, pt, :] if d is not None else wt[:, pt, :]
            P.op("pe", lambda e, lhs=lhs, pt=pt: e.matmul(ps[0:nout, 0:W], lhs, src[:, pt, 0:W], start=(pt == 0), stop=(pt == 3)), reads=[wkey, skey], writes=[keyp])

    def prep_chunk(b, si, c0, c1):
        if True:
            s0, s1 = SEGS[si]; W = c1 - c0; lo = max(c0 - 1, s0); hi = min(c1 + 1, s1); q0 = lo - (c0 - 1); q1 = hi - (c0 - 1)
            WW = W + 2
            for tl, key in ((zc, "zc"), (rc, "rc"), (kc, "kc"), (vc, "vc"), (vfc, "vfc")):
                P.op("pool", lambda e, tl=tl: e.memset(tl[:], 0.0), writes=[key])
            P.dma("sp", zc[:, :, q0:q1], zT[b, :, lo:hi].rearrange("(pt p) n -> p pt n", p=128), reads=[], writes=["zc"])
            P.dma("act", rc[:, q0:q1], rT[b, :, lo:hi], writes=["rc"]); P.dma("act", kc[:, q0:q1], kT[b, :, lo:hi], writes=["kc"])
            P.dma("sp", vc[:, q0:q1], vT[b, :, lo:hi], writes=["vc"]); P.dma("act", vfc[:, q0:q1], vfT[b, :, lo:hi], writes=["vfc"])
            mm_z(pA, "pA", g1t, "g1t", None, 128, zc, "zc", WW)
            P.op("act", lambda e, WW=WW: e.activation(out=hid[:, 0:WW], in_=pA[:, 0:WW], func=AF.Sigmoid), reads=["pA"], writes=["hid"])
            P.op("pe", lambda e, WW=WW: e.matmul(pB[0:64, 0:WW], g2t[:], hid[:, 0:WW], start=True, stop=True), reads=["g2t", "hid"], writes=["pB"])
            P.op("act", lambda e, WW=WW: e.activation(out=gq[:, 0:WW], in_=pB[0:64, 0:WW], func=AF.Copy), reads=["pB"], writes=["gq"])
            P.dma("sp", Gd[:, b, c0:c1], gq[:, 1:W + 1], reads=["gq"], writes=["Gd"])
            mm_z(pA, "pA", v1t, "v1t", None, 32, zc, "zc", WW)
            P.op("act", lambda e, WW=WW: e.activation(out=h32[:, 0:WW], in_=pA[0:32, 0:WW], func=AF.Copy), reads=["pA"], writes=["h32"])
            P.op("pe", lambda e, WW=WW: e.matmul(pB[0:64, 0:WW], v2t[:], h32[:, 0:WW], start=True, stop=True), reads=["v2t", "h32"], writes=["pB"])
            P.op("act", lambda e, WW=WW: e.activation(out=sgv[:, 0:WW], in_=pB[0:64, 0:WW], func=AF.Sigmoid, bias=V0), reads=["pB", "hvt"], writes=["sgv"])
            P.op("dve", lambda e: e.tensor_tensor(out=vfc[:], in0=vfc[:], in1=vc[:], op=ALU.subtract), reads=["vfc", "vc"], writes=["vfc"])
            P.op("dve", lambda e: e.tensor_tensor(out=vfc[:], in0=vfc[:], in1=sgv[:], op=ALU.mult), reads=["vfc", "sgv"], writes=["vfc"])
            P.op("dve", lambda e: e.tensor_tensor(out=vc[:], in0=vc[:], in1=vfc[:], op=ALU.add), reads=["vfc", "vc"], writes=["vc"])
            for d in range(2):
                nb = 0 if d == 0 else 2
                for j, (src, skey, dst, dkey) in enumerate(((rc, "rc", xr, "xr"), (kc, "kc", xk, "xk"), (vc, "vc", xv, "xv"))):
                    P.op("dve", lambda e, src=src, dst=dst, d=d, j=j: e.tensor_scalar(out=dst[:, 0:W], in0=src[:, 1:W + 1], scalar1=mh1[:, d, j:j + 1], scalar2=None, op0=ALU.mult),
                         reads=[skey, "mh1"], writes=[dkey])
                    P.op("dve", lambda e, src=src, dst=dst, d=d, j=j, nb=nb: e.scalar_tensor_tensor(out=dst[:, 0:W], in0=src[:, nb:nb + W], scalar=mh[:, d, j:j + 1], in1=dst[:, 0:W], op0=ALU.mult, op1=ALU.add),
                         reads=[skey, "mh", dkey], writes=[dkey])
                for pt in range(4):
                    P.op("pool", lambda e, pt=pt, d=d: e.tensor_scalar(out=xz[:, pt, 0:W], in0=zc[:, pt, 1:W + 1], scalar1=mz1[:, pt, d:d + 1], scalar2=None, op0=ALU.mult),
                         reads=["zc", "mz1"], writes=["xz"])
                    P.op("dve", lambda e, pt=pt, d=d, nb=nb: e.scalar_tensor_tensor(out=xz[:, pt, 0:W], in0=zc[:, pt, nb:nb + W], scalar=mz[:, pt, d:d + 1], in1=xz[:, pt, 0:W], op0=ALU.mult, op1=ALU.add),
                         reads=["zc", "mz", "xz"], writes=["xz"])
                mm_z(pA, "pA", w1t, "w1t", d, 64, xz, "xz", W)
                P.op("act", lambda e: e.activation(out=hw[:, 0:W], in_=pA[0:64, 0:W], func=AF.Tanh), reads=["pA"], writes=["hw"])
                P.op("pe", lambda e, d=d: e.matmul(pB[0:64, 0:W], w2t[:, d, :], hw[:, 0:W], start=True, stop=True), reads=["w2t", "hw"], writes=["pB"])
                P.op("act", lambda e, d=d: e.activation(out=dec[:, 0:W], in_=pB[0:64, 0:W], func=AF.Sigmoid, bias=W0(d)), reads=["pB", "hvt"], writes=["dec"])
                P.op("act", lambda e: e.activation(out=dec[:, 0:W], in_=dec[:, 0:W], func=AF.Exp, scale=-float(np.exp(-0.5))), reads=["dec"], writes=["dec"])
                mm_z(pA, "pA", a1t, "a1t", d, 64, xz, "xz", W)
                P.op("act", lambda e: e.activation(out=ha[:, 0:W], in_=pA[0:64, 0:W], func=AF.Copy), reads=["pA"], writes=["ha"])
                P.op("pe", lambda e, d=d: e.matmul(pB[0:64, 0:W], a2t[:, d, :], ha[:, 0:W], start=True, stop=True), reads=["a2t", "ha"], writes=["pB"])
                P.op("act", lambda e, d=d: e.activation(out=aa[:, 0:W], in_=pB[0:64, 0:W], func=AF.Sigmoid, bias=A0(d)), reads=["pB", "hvt"], writes=["aa"])
                P.op("dve", lambda e: e.tensor_scalar(out=kk[:, 0:W], in0=xk[:, 0:W], scalar1=KK_, scalar2=None, op0=ALU.mult), reads=["xk", "hvt"], writes=["kk"])
                P.op("dve", lambda e: e.tensor_tensor(out=t1[:, 0:W], in0=kk[:, 0:W], in1=kk[:, 0:W], op=ALU.mult), reads=["kk"], writes=["t1"])
                P.op("pe", lambda e: e.matmul(pA[0:64, 0:W], ones64[:], t1[:, 0:W], start=True, stop=True), reads=["ones64", "t1"], writes=["pA"])
                P.op("act", lambda e: e.activation(out=t1[:, 0:W], in_=pA[0:64, 0:W], func=AF.Sqrt), reads=["pA"], writes=["t1"])
                P.op("dve", lambda e: e.tensor_scalar(out=t1[:, 0:W], in0=t1[:, 0:W], scalar1=1e-12, scalar2=None, op0=ALU.max), reads=["t1"], writes=["t1"])
                P.op("dve", lambda e: e.reciprocal(out=t1[:, 0:W], in_=t1[:, 0:W]), reads=["t1"], writes=["t1"])
                P.op("dve", lambda e: e.tensor_tensor(out=kk[:, 0:W], in0=kk[:, 0:W], in1=t1[:, 0:W], op=ALU.mult), reads=["kk", "t1"], writes=["kk"])
                P.op("dve", lambda e: e.tensor_scalar(out=kp[:, 0:W], in0=aa[:, 0:W], scalar1=-1.0, scalar2=KA_, op0=ALU.add, op1=ALU.mult), reads=["aa", "hvt"], writes=["kp"])
                P.op("dve", lambda e: e.scalar_tensor_tensor(out=kp[:, 0:W], in0=kp[:, 0:W], scalar=1.0, in1=xk[:, 0:W], op0=ALU.add, op1=ALU.mult), reads=["kp", "xk"], writes=["kp"])
                P.op("dve", lambda e: e.tensor_tensor(out=kka[:, 0:W], in0=kk[:, 0:W], in1=aa[:, 0:W], op=ALU.mult), reads=["kk", "aa"], writes=["kka"])
                P.op("dve", lambda e: e.scalar_tensor_tensor(out=t1[:, 0:W], in0=xr[:, 0:W], scalar=RK_, in1=kp[:, 0:W], op0=ALU.mult, op1=ALU.mult), reads=["xr", "kp", "hvt", "t1"], writes=["t1"])
                P.op("pe", lambda e: e.matmul(pB[0:64, 0:W], ones64[:], t1[:, 0:W], start=True, stop=True), reads=["ones64", "t1"], writes=["pB"])
                P.op("dve", lambda e: e.tensor_tensor(out=bon[:, 0:W], in0=pB[0:64, 0:W], in1=xv[:, 0:W], op=ALU.mult), reads=["pB", "xv"], writes=["bon"])
                P.op("dve", lambda e: e.tensor_tensor(out=wrt[:, 0:W], in0=dec[:, 0:W], in1=xr[:, 0:W], op=ALU.mult), reads=["dec", "xr"], writes=["wrt"])
                P.op("dve", lambda e: e.tensor_tensor(out=t1[:, 0:W], in0=kka[:, 0:W], in1=xr[:, 0:W], op=ALU.mult), reads=["kka", "xr", "t1"], writes=["t1"])
                P.op("pe", lambda e: e.matmul(pA[0:64, 0:W], ones64[:], t1[:, 0:W], start=True, stop=True), reads=["ones64", "t1"], writes=["pA"])
                P.op("act", lambda e: e.activation(out=c1t[:, 0:W], in_=pA[0:64, 0:W], func=AF.Copy), reads=["pA"], writes=["c1t"])
                P.op("dve", lambda e: e.tensor_tensor(out=t1[:, 0:W], in0=kp[:, 0:W], in1=xr[:, 0:W], op=ALU.mult), reads=["kp", "xr", "t1"], writes=["t1"])
                P.op("pe", lambda e: e.matmul(pB[0:64, 0:W], ones64[:], t1[:, 0:W], start=True, stop=True), reads=["ones64", "t1"], writes=["pB"])
                P.op("act", lambda e: e.activation(out=c2t[:, 0:W], in_=pB[0:64, 0:W], func=AF.Copy), reads=["pB"], writes=["c2t"])
                outs4 = ((xv, "xv", Vd, "Vd"), (bon, "bon", Bd, "Bd"), (c1t, "c1t", C1d, "C1d"), (c2t, "c2t", C2d, "C2d"))
                if d == 0:
                    t_lo = c0
                    for qi, (src_, sk_, dst_, dk_) in enumerate(outs4):
                        P.dma("sp" if qi % 2 == 0 else "act", dst_[0, :, b, t_lo:t_lo + W], src_[:, 0:W], reads=[sk_], writes=[dk_])
                else:
                    t_lo = tau_of(c1 - 1)
                    for qi, (src_, sk_, dst_, dk_) in enumerate(outs4):
                        P.op("pool", lambda e, src_=src_, qi=qi: e.tensor_copy(out=rvb4[:, qi, 0:W], in_=rev(src_[:, 0:W])), reads=[sk_], writes=[("rvb4", qi)])
                        P.dma("sp" if qi % 2 == 0 else "act", dst_[1, :, b, t_lo:t_lo + W], rvb4[:, qi, 0:W], reads=[("rvb4", qi)], writes=[dk_])
                vecs = ((kk, "kk"), (wrt, "wrt"), (kp, "kp"), (dec, "dec"), (kka, "kka"))
                if d == 1:
                    for vi, (vt_, vk_) in enumerate(vecs):
                        P.op("pool", lambda e, vt_=vt_, vi=vi: e.tensor_copy(out=rv5[:, vi, 0:W], in_=rev(vt_[:, 0:W])), reads=[vk_], writes=["rv5"])
                for bi in range(0, W, 128):
                    bw = min(128, W - bi)
                    for vi, (vt_, vk_) in enumerate(vecs):
                        if d == 0:
                            src = vt_[:, bi:bi + bw]
                        else:
                            src = rv5[:, vi, bi:bi + bw]; vk_ = "rv5"
                        P.op("pe", lambda e, src=src, vi=vi, bw=bw: e.transpose(pT[0:bw, vi, :], src, ident[0:64, 0:64]), reads=[vk_, "identf"], writes=["pT"])
                    P.op("act", lambda e, bw=bw: e.activation(out=stg[0:bw], in_=pT[0:bw], func=AF.Copy), reads=["pT"], writes=["stg"])
                    P.dma("act", X[d, t_lo + bi:t_lo + bi + bw, b], stg[0:bw], reads=["stg"], writes=["X"])

    for b in range(4):
        for (si, c0, c1) in seg_chunks(510):
            prep_chunk(b, si, c0, c1)

    TT = 8
    selt = P.sb("selt", [8, 4, 128]); P.dma("sp", selt[:], seli[:, :, :], writes=["selt"])
    Xc = [P.sb("Xc%d" % i, [8, TT, 128]) for i in range(2)]
    Bt = [P.sb("Bt%d" % i, [128, TT, 4, 3, 64]) for i in range(2)]
    Vt = [P.sb("Vt%d" % i, [128, 4, 128]) for i in range(2)]
    SQt = [P.sb("SQt%d" % i, [128, 4, 2, 128]) for i in range(2)]
    Sh = [P.sb("S%d" % h, [128, 2, 64]) for h in range(2)]; tmph = [P.sb("tmpS%d" % h, [128, 2, 64]) for h in range(2)]
    tmpw = [P.sb("tmpW%d" % h, [128, 2, 2, 64]) for h in range(2)]
    tmp2 = [P.sb("tmpK%d" % i, [128, 4, 64]) for i in range(2)]
    for h in range(2):
        P.op("dve", lambda e, h=h: e.memset(Sh[h][:], 0.0), writes=["S%d" % h])
    hs = [slice(0, 2), slice(2, 4)]
    P.relax_dve = RW_RELAX
    for ch in range(NS // TT):
        t0 = ch * TT
        bt = Bt[ch % 2]; bk = "Bt%d" % (ch % 2); xc = Xc[ch % 2]; xck = "Xc%d" % (ch % 2)
        for d in range(2):
            srcap = X[d, t0:t0 + TT, :, 0:3, :].rearrange("t b v j -> t b (v j)").partition_broadcast(64)
            P.dma("sp" if d == 0 else "act", bt[d * 64:(d + 1) * 64].rearrange("p t b v j -> p t b (v j)"), srcap, reads=["X"], writes=[bk])
            P.dma("act" if d == 0 else "sp", xc[d * 4:(d + 1) * 4], X[d, t0:t0 + TT, :, 3:5, :].rearrange("t b v j -> b t (v j)"), reads=["X"], writes=[xck])
        if t0 % 128 == 0:
            vi_ = (t0 // 128) % 2
            vt_ = Vt[vi_]; vk_ = "Vt%d" % vi_
            P.dma("sp", vt_[:], Vd[:, :, :, t0:t0 + 128].rearrange("d i b t -> (d i) b t"), reads=["Vd"], writes=[vk_])
        yi_ = (t0 // 128) % 2
        sq_ = SQt[yi_]; yk_ = "SQt%d" % yi_
        for tl in range(TT):
            t = t0 + tl; tq = t % 128
            px = pX[t % NPX]; pxk = "pX%d" % (t % NPX)
            for g in range(4):
                P.op("pe", lambda e, px=px, g=g, xc=xc, tl=tl: e.matmul(px[:, g * 128:(g + 1) * 128], selt[:, g, :], xc[:, tl, :], start=True, stop=True), reads=["selt", xck], writes=[pxk])
            pv = px.rearrange("p (g v j) -> p g v j", g=4, v=2)
            vv_ = vt_[:, :, tq:tq + 1].to_broadcast([128, 4, 64])
            tk = tmp2[t % 2]; tkk = "tmpK%d" % (t % 2)
            Kv = bt[:, tl, :, 2, :]
            P.op("pool", lambda e, tk=tk, Kv=Kv, vv_=vv_: e.tensor_tensor(out=tk[:], in0=Kv, in1=vv_, op=ALU.mult), reads=[bk, vk_], writes=[tkk])
            for h in range(2):
                P.op("dve", lambda e, h=h, a=bt[:, tl, hs[h], 0:2, :]: e.tensor_tensor(out=tmpw[h][:], in0=Sh[h][:, :, :].unsqueeze(2).to_broadcast([128, 2, 2, 64]), in1=a, op=ALU.mult),
                     reads=["S%d" % h, bk], writes=["tmpW%d" % h])
            for h in range(2):
                P.op("dve", lambda e, h=h, sq_=sq_, tq=tq: e.tensor_reduce(out=sq_[:, hs[h], :, tq], in_=tmpw[h][:], axis=AX.X, op=ALU.add), reads=["tmpW%d" % h], writes=[(yk_, h)])
            for h in range(2):
                P.op("dve", lambda e, h=h, a=pv[:, hs[h], 0, :]: e.tensor_tensor(out=Sh[h][:], in0=Sh[h][:], in1=a, op=ALU.mult), reads=["S%d" % h, pxk], writes=["S%d" % h])
            for h in range(2):
                P.op("dve", lambda e, h=h, a=pv[:, hs[h], 1, :], sq_=sq_, tq=tq: e.tensor_tensor(out=tmph[h][:], in0=a, in1=sq_[:, hs[h], 0, tq:tq + 1].to_broadcast([128, 2, 64]), op=ALU.mult),
                     reads=[(yk_, h), pxk], writes=["tmpS%d" % h])
            for h in range(2):
                P.op("dve", lambda e, h=h: e.tensor_tensor(out=Sh[h][:], in0=Sh[h][:], in1=tmph[h][:], op=ALU.subtract), reads=["S%d" % h, "tmpS%d" % h], writes=["S%d" % h])
            for h in range(2):
                P.op("dve", lambda e, h=h, tk=tk: e.tensor_tensor(out=Sh[h][:], in0=Sh[h][:], in1=tk[:, hs[h], :], op=ALU.add), reads=["S%d" % h, tkk], writes=["S%d" % h])
        if (t0 + TT) % 128 == 0:
            tb = t0 + TT - 128
            P.dma("sp", SAd[:, :, :, tb:tb + 128].rearrange("d i b t -> (d i) b t"), sq_[:, :, 0, :], reads=[(yk_, 0), (yk_, 1)], writes=["SAd"])
            P.dma("act", Yd[:, :, :, tb:tb + 128].rearrange("d i b t -> (d i) b t"), sq_[:, :, 1, :], reads=[(yk_, 0), (yk_, 1)], writes=["Yd"])

    P.relax_dve = False
    yc = P.sb("yc", [128, 512]); bc = P.sb("bc", [128, 512]); cen = P.sb("cen", [128, 512]); sq2 = P.sb("sq2", [128, 512]); zr = P.sb("zr", [128, 512])
    gc = P.sb("gc", [64, 512]); oc = P.sb("oc", [64, 512])
    sac = P.sb("sac", [128, 512]); c1c = P.sb("c1c", [128, 512]); c2c = P.sb("c2c", [128, 512]); vcc = P.sb("vcc", [128, 512])
    LNW = hvt[:, 8:9]; LNB = hvt[:, 9:10]
    def post_chunk(b, si, c0, c1):
        if True:
            W = c1 - c0; tl1 = tau_of(c1 - 1)
            P.dma("sp", yc[0:64, 0:W], Yd[0, :, b, c0:c1], reads=["Yd"], writes=["yc"]); P.dma("act", yc[64:128, 0:W], Yd[1, :, b, tl1:tl1 + W], reads=["Yd"], writes=["yc"])
            P.dma("sp", bc[0:64, 0:W], Bd[0, :, b, c0:c1], reads=["Bd"], writes=["bc"]); P.dma("act", bc[64:128, 0:W], Bd[1, :, b, tl1:tl1 + W], reads=["Bd"], writes=["bc"])
            P.dma("sp", gc[:, 0:W], Gd[:, b, c0:c1], reads=["Gd"], writes=["gc"])
            for qi, (dst_, dk_, src_, sk_) in enumerate(((sac, "sac", SAd, "SAd"), (c1c, "c1c", C1d, "C1d"), (c2c, "c2c", C2d, "C2d"), (vcc, "vcc", Vd, "Vd"))):
                P.dma("sp", dst_[0:64, 0:W], src_[0, :, b, c0:c1], reads=[sk_], writes=[dk_]); P.dma("act", dst_[64:128, 0:W], src_[1, :, b, tl1:tl1 + W], reads=[sk_], writes=[dk_])
            P.op("dve", lambda e: e.tensor_tensor(out=sac[:, 0:W], in0=sac[:, 0:W], in1=c1c[:, 0:W], op=ALU.mult), reads=["sac", "c1c"], writes=["sac"])
            P.op("dve", lambda e: e.tensor_tensor(out=yc[:, 0:W], in0=yc[:, 0:W], in1=sac[:, 0:W], op=ALU.subtract), reads=["yc", "sac"], writes=["yc"])
            P.op("pool", lambda e: e.tensor_tensor(out=vcc[:, 0:W], in0=vcc[:, 0:W], in1=c2c[:, 0:W], op=ALU.mult), reads=["vcc", "c2c"], writes=["vcc"])
            P.op("dve", lambda e: e.tensor_tensor(out=yc[:, 0:W], in0=yc[:, 0:W], in1=vcc[:, 0:W], op=ALU.add), reads=["yc", "vcc"], writes=["yc"])
            P.op("pe", lambda e: e.matmul(pA[:, 0:W], blk[:], yc[:, 0:W], start=True, stop=True), reads=["blk", "yc"], writes=["pA"])
            P.op("dve", lambda e: e.tensor_tensor(out=cen[:, 0:W], in0=yc[:, 0:W], in1=pA[:, 0:W], op=ALU.subtract), reads=["yc", "pA"], writes=["cen"])
            P.op("act", lambda e: e.activation(out=sq2[:, 0:W], in_=cen[:, 0:W], func=AF.Square), reads=["cen"], writes=["sq2"])
            P.op("pe", lambda e: e.matmul(pB[:, 0:W], blk[:], sq2[:, 0:W], start=True, stop=True), reads=["blk", "sq2"], writes=["pB"])
            P.op("act", lambda e: e.activation(out=sq2[:, 0:W], in_=pB[:, 0:W], func=AF.Sqrt, bias=64e-5), reads=["pB", "sq2"], writes=["sq2"])
            P.op("dve", lambda e: e.reciprocal(out=sq2[:, 0:W], in_=sq2[:, 0:W]), reads=["sq2"], writes=["sq2"])
            P.op("dve", lambda e: e.tensor_tensor(out=cen[:, 0:W], in0=cen[:, 0:W], in1=sq2[:, 0:W], op=ALU.mult), reads=["cen", "sq2"], writes=["cen"])
            P.op("dve", lambda e: e.tensor_scalar(out=cen[:, 0:W], in0=cen[:, 0:W], scalar1=LNW, scalar2=LNB, op0=ALU.mult, op1=ALU.add), reads=["cen", "hvt"], writes=["cen"])
            P.op("dve", lambda e: e.tensor_tensor(out=cen[:, 0:W], in0=cen[:, 0:W], in1=bc[:, 0:W], op=ALU.add), reads=["cen", "bc"], writes=["cen"])
            P.op("pool", lambda e: e.tensor_copy(out=zr[:, 0:W], in_=rev(cen[:, 0:W])), reads=["cen"], writes=["zr"])
            P.op("pe", lambda e: e.matmul(pA[0:64, 0:W], F0[:], cen[:, 0:W], start=True, stop=False), reads=["F0", "cen"], writes=["pA"])
            P.op("pe", lambda e: e.matmul(pA[0:64, 0:W], F1[:], zr[:, 0:W], start=False, stop=True), reads=["F1", "zr"], writes=["pA"])
            P.op("dve", lambda e: e.tensor_tensor(out=oc[:, 0:W], in0=pA[0:64, 0:W], in1=gc[:, 0:W], op=ALU.mult), reads=["pA", "gc"], writes=["oc"])
            P.dma("sp", oT[b, :, c0:c1], oc[:, 0:W], reads=["oc"], writes=["oT"])
    for b in range(4):
        for (si, c0, c1) in seg_chunks(512):
            post_chunk(b, si, c0, c1)
    finals = ["oT"] + (["X", "Vd", "Bd", "Gd", "Yd", "SAd", "C1d", "C2d"] if debug else [])
    return P.finish(finals)


def launch_rwkv(ul, uc, vfl, vfc_, p, layer, cores=range(NCORES), debug=False):
    G = 512
    nc = build_rwkv(debug)
    zT = scanT(ul, uc, 8 * G, 9 * G)
    maps = []
    for c in cores:
        sl = slice(c * 64, (c + 1) * 64)
        m = {"rT": scanT(ul, uc, 5 * G + c * 64, 5 * G + (c + 1) * 64), "kT": scanT(ul, uc, 6 * G + c * 64, 6 * G + (c + 1) * 64),
             "vT": scanT(ul, uc, 7 * G + c * 64, 7 * G + (c + 1) * 64), "zT": zT,
             "vfT": scanT(vfl, vfc_, c * 64, (c + 1) * 64)}
        mu = p["rw_mu"]
        m["mu_h"] = np.ascontiguousarray(mu[:, 0:3, sl].transpose(2, 0, 1))
        m["mu_z"] = np.ascontiguousarray(mu[:, 3, :].reshape(2, 4, 128).transpose(2, 1, 0))
        m["w1"] = np.ascontiguousarray(p["rw_w1"]); m["w2"] = np.ascontiguousarray(p["rw_w2"][:, :, sl])
        m["a1"] = np.ascontiguousarray(p["rw_a1"]); m["a2"] = np.ascontiguousarray(p["rw_a2"][:, :, sl])
        m["g1"] = np.ascontiguousarray(p["rw_g1"]); m["g2"] = np.ascontiguousarray(p["rw_g2"][:, sl])
        m["v1"] = np.ascontiguousarray(p["rw_v1"]); m["v2"] = np.ascontiguousarray(p["rw_v2"][:, sl])
        selc = np.zeros((8, 4, 128), np.float32)
        for dd in range(2):
            for bb in range(4):
                selc[dd * 4 + bb, bb, dd * 64:(dd + 1) * 64] = 1.0
        m["sel"] = selc
        m["hv"] = np.ascontiguousarray(np.stack([p["rw_w0"][0, sl], p["rw_w0"][1, sl], p["rw_a0"][0, sl], p["rw_a0"][1, sl], p["rw_v0"][sl],
                                                 p["rw_k_k"][sl], p["rw_k_a"][sl], p["rw_r_k"].reshape(-1)[sl], p["rw_ln_w"][sl], p["rw_ln_b"][sl]], -1))
        maps.append(m)
    res = run(nc, maps)
    oT = np.concatenate([r["oT"] for r in res], axis=1)
    if debug:
        return unscanT(oT), res
    return unscanT(oT)


def rt_consts():
    inv = np.power(10000.0, -np.arange(32, dtype=np.float32) / 32).astype(np.float32)
    t = np.arange(4096)
    rows = (t // 64).astype(np.float32); cols = (t % 64).astype(np.float32)
    ang = np.zeros((128, 4096), np.float32)
    for d in range(128):
        pos = rows if d < 64 else cols
        ang[d] = pos * inv[(d % 64) % 32]
    cosT = np.cos(ang).astype(np.float32); sinT = np.sin(ang).astype(np.float32)
    Pi = np.zeros((128, 128), np.float32)
    for m in range(128):
        if m % 64 < 32:
            Pi[m, m + 32] = -1.0
        else:
            Pi[m, m - 32] = 1.0
    j = np.arange(128, dtype=np.float32)[:, None]; i = np.arange(128, dtype=np.float32)[None, :]
    diff = i - j
    c = np.zeros((128, 6, 128), np.float32)
    c[:, 0] = np.maximum(diff, 0); c[:, 1] = (diff >= 0); c[:, 2] = np.maximum(-diff, 0); c[:, 3] = (diff < 0)
    c[:, 4] = i + 1.0 + 0 * j; c[:, 5] = 128.0 - i + 0 * j
    colv = np.zeros((128, 2), np.float32); colv[:, 0] = 127 - np.arange(128); colv[:, 1] = np.arange(128)
    return cosT, sinT, np.ascontiguousarray(Pi.T), c, colv


def build_ret():
    P = Prog()
    NCH = 34
    QT = P.inp("QT", [2, 128, TS]); KT = P.inp("KT", [2, 128, TS]); Vv = P.inp("V", [2, TS, 128]); Gg = P.inp("G", [2, TS, 128])
    dec = P.inp("dec", [2, 2]); gnw = P.inp("gnw", [2, 128])
    cosT = P.inp("cosT", [128, 4096]); sinT = P.inp("sinT", [128, 4096]); PiT = P.inp("PiT", [128, 128]); cc = P.inp("cc", [128, 6, 128]); colv = P.inp("colv", [128, 2])
    out = P.out("out", [2, TS, 128])
    ct = P.sb("ct", [128, 4096]); st = P.sb("st", [128, 4096]); pit = P.sb("pit", [128, 128]); cct = P.sb("cct", [128, 6, 128]); cvt = P.sb("cvt", [128, 2])
    ident = P.sb("identf", [128, 128])
    qt = P.sb("qt", [128, TS]); kt = P.sb("kt", [128, TS]); vt = P.sb("vt", [128, NCH, 128]); gt = P.sb("gt", [128, NCH, 128]); O = P.sb("O", [128, NCH, 128])
    lg = P.sb("lg", [128, 2]); M = P.sb("M", [128, 2, 128]); QD = P.sb("QD", [128, 2, 128]); kd = P.sb("kd", [128, 2]); cd = P.sb("cd", [128, 2])
    gwb = P.sb("gwb", [128, 128])
    S = [P.sb("S%d" % d, [128, 128]) for d in range(2)]
    A0 = P.sb("A0", [128, 128]); A1 = P.sb("A1", [128, 128]); Q0 = P.sb("Q0", [128, 128]); K0 = P.sb("K0", [128, 128]); tmpr = P.sb("tmpr", [128, 512])
    mv = P.sb("mv", [128, 4]); cen = P.sb("cen", [128, 128]); sq = P.sb("sqr", [128, 128]); sg = P.sb("sgr", [128, 128])
    pA = P.ps("pA", [128, 512]); pB = P.ps("pB", [128, 128]); pC = P.ps("pC", [128, 128]); pD = P.ps("pD", [128, 128])
    P.dma("sp", ct[:], cosT[:, :], writes=["ct"]); P.dma("act", st[:], sinT[:, :], writes=["st"]); P.dma("sp", pit[:], PiT[:, :], writes=["pit"])
    P.dma("sp", cct[:], cc[:, :, :], writes=["cct"]); P.dma("sp", cvt[:], colv[:, :], writes=["cvt"])
    P.op("pool", lambda e: e.memset(ident[:], 1.0), writes=["identf"])
    P.op("pool", lambda e: e.affine_select(out=ident[:], in_=ident[:], pattern=[[-1, 128]], compare_op=ALU.is_equal, fill=0.0, base=0, channel_multiplier=1), reads=["identf"], writes=["identf"])

    def pair(pi):
        P.dma("sp", qt[:], QT[pi], writes=["qt"]); P.dma("act", kt[:], KT[pi], writes=["kt"])
        P.dma("sp", vt[:], Vv[pi].rearrange("(n p) e -> p n e", p=128), writes=["vt"]); P.dma("act", gt[:], Gg[pi].rearrange("(n p) e -> p n e", p=128), writes=["gt"])
        P.dma("sp", lg[:], dec[pi:pi + 1, :].partition_broadcast(128), writes=["lg"])
        P.dma("sp", gwb[:], gnw[pi:pi + 1, :].partition_broadcast(128), writes=["gwb"])
        P.op("act", lambda e: e.activation(out=lg[:], in_=lg[:], func=AF.Exp), reads=["lg"], writes=["lg"])
        P.op("act", lambda e: e.activation(out=lg[:], in_=lg[:], func=AF.Ln, bias=1.0), reads=["lg"], writes=["lg"])
        P.op("dve", lambda e: e.tensor_scalar(out=lg[:], in0=lg[:], scalar1=-1.0, scalar2=None, op0=ALU.mult), reads=["lg"], writes=["lg"])
        for d in range(2):
            P.op("act", lambda e, d=d: e.activation(out=M[:, d, :], in_=cct[:, 2 * d, :], func=AF.Exp, scale=lg[:, d:d + 1]), reads=["cct", "lg"], writes=["M"])
            P.op("dve", lambda e, d=d: e.tensor_tensor(out=M[:, d, :], in0=M[:, d, :], in1=cct[:, 2 * d + 1, :], op=ALU.mult), reads=["M", "cct"], writes=["M"])
            P.op("act", lambda e, d=d: e.activation(out=QD[:, d, :], in_=cct[:, 4 + d, :], func=AF.Exp, scale=lg[:, d:d + 1]), reads=["cct", "lg"], writes=["QD"])
            P.op("act", lambda e, d=d: e.activation(out=kd[:, d:d + 1], in_=cvt[:, d:d + 1], func=AF.Exp, scale=lg[:, d:d + 1]), reads=["cvt", "lg"], writes=["kd"])
            P.op("act", lambda e, d=d: e.activation(out=cd[:, d:d + 1], in_=lg[:, d:d + 1], func=AF.Exp, scale=128.0), reads=["lg"], writes=["cd"])
            P.op("dve", lambda e, d=d: e.memset(S[d][:], 0.0), writes=["S%d" % d])
        P.op("act", lambda e: e.activation(out=kt[:], in_=kt[:], func=AF.Copy, scale=float(128 ** -0.5)), reads=["kt"], writes=["kt"])
        for (x, xk) in ((qt, "qt"), (kt, "kt")):
            for c0 in range(256, TS, 512):
                def rope(x=x, xk=xk, c0=c0):
                    P.op("pe", lambda e: e.matmul(pA[:], pit[:], x[:, c0:c0 + 512], start=True, stop=True), reads=["pit", xk], writes=["pA"])
                    P.op("dve", lambda e: e.tensor_tensor(out=tmpr[:], in0=pA[:], in1=st[:, c0 - 256:c0 + 256], op=ALU.mult), reads=["pA", "st"], writes=["tmpr"])
                    P.op("pool", lambda e: e.tensor_tensor(out=x[:, c0:c0 + 512], in0=x[:, c0:c0 + 512], in1=ct[:, c0 - 256:c0 + 256], op=ALU.mult), reads=[xk, "ct", "pA"], writes=[xk])
                    P.op("dve", lambda e: e.tensor_tensor(out=x[:, c0:c0 + 512], in0=x[:, c0:c0 + 512], in1=tmpr[:], op=ALU.add), reads=[xk, "tmpr"], writes=[xk])
                rope()

        def state_update(n, d):
            cs = slice(n * 128, (n + 1) * 128); Sk = "S%d" % d
            P.op("pe", lambda e: e.transpose(pC[:], kt[:, cs], ident[:]), reads=["kt", "identf"], writes=["pC"])
            P.op("act", lambda e: e.activation(out=K0[:], in_=pC[:], func=AF.Copy, scale=kd[:, d:d + 1]), reads=["pC", "kd"], writes=["K0"])
            P.op("pe", lambda e: e.matmul(pD[:], K0[:], vt[:, n, :], start=True, stop=True), reads=["K0", "vt"], writes=["pD"])
            P.op("dve", lambda e: e.scalar_tensor_tensor(out=S[d][:], in0=S[d][:], scalar=cd[:, d:d + 1], in1=pD[:], op0=ALU.mult, op1=ALU.add), reads=[Sk, "cd", "pD"], writes=[Sk])

        def fwd(n):
            cs = slice(n * 128, (n + 1) * 128)
            P.op("pe", lambda e: e.matmul(pB[:], kt[:, cs], qt[:, cs], start=True, stop=True), reads=["kt", "qt"], writes=["pB"])
            P.op("dve", lambda e: e.tensor_tensor(out=A0[:], in0=pB[:], in1=M[:, 0, :], op=ALU.mult), reads=["pB", "M"], writes=["A0"])
            P.op("dve", lambda e: e.tensor_tensor(out=A1[:], in0=pB[:], in1=M[:, 1, :], op=ALU.mult), reads=["pB", "M"], writes=["A1"])
            P.op("pool", lambda e: e.tensor_tensor(out=Q0[:], in0=qt[:, cs], in1=QD[:, 0, :], op=ALU.mult), reads=["qt", "QD"], writes=["Q0"])
            P.op("pe", lambda e: e.matmul(pA[:, 0:128], A0[:], vt[:, n, :], start=True, stop=False), reads=["A0", "vt"], writes=["pA"])
            P.op("pe", lambda e: e.matmul(pA[:, 0:128], A1[:], vt[:, n, :], start=False, stop=False), reads=["A1", "vt"], writes=["pA"])
            P.op("pe", lambda e: e.matmul(pA[:, 0:128], Q0[:], S[0][:], start=False, stop=True), reads=["Q0", "S0"], writes=["pA"])
            P.op("act", lambda e: e.activation(out=O[:, n, :], in_=pA[:, 0:128], func=AF.Copy), reads=["pA"], writes=[("O", n)])
            state_update(n, 0)

        def bwd(n):
            cs = slice(n * 128, (n + 1) * 128)
            P.op("pool", lambda e: e.tensor_tensor(out=Q0[:], in0=qt[:, cs], in1=QD[:, 1, :], op=ALU.mult), reads=["qt", "QD"], writes=["Q0"])
            P.op("pe", lambda e: e.matmul(pA[:, 0:128], Q0[:], S[1][:], start=True, stop=True), reads=["Q0", "S1"], writes=["pA"])
            P.op("dve", lambda e: e.tensor_tensor(out=O[:, n, :], in0=O[:, n, :], in1=pA[:, 0:128], op=ALU.add), reads=["pA", ("O", n)], writes=[("O", n)])
            state_update(n, 1)
            P.op("dve", lambda e: e.tensor_reduce(out=mv[:, 0:1], in_=O[:, n, :], axis=AX.X, op=ALU.add), reads=[("O", n)], writes=["mv"])
            P.op("dve", lambda e: e.tensor_scalar(out=mv[:, 0:1], in0=mv[:, 0:1], scalar1=-1.0 / 128, scalar2=None, op0=ALU.mult), reads=["mv"], writes=["mv"])
            P.op("dve", lambda e: e.tensor_scalar(out=cen[:], in0=O[:, n, :], scalar1=mv[:, 0:1], scalar2=None, op0=ALU.add), reads=[("O", n), "mv"], writes=["cen"])
            P.op("act", lambda e: e.activation(out=sq[:], in_=cen[:], func=AF.Square, accum_out=mv[:, 1:2]), reads=["cen", "mv"], writes=["sqr", "mv"])
            P.op("dve", lambda e: e.tensor_scalar(out=mv[:, 2:3], in0=mv[:, 1:2], scalar1=1.0 / 128, scalar2=1e-6, op0=ALU.mult, op1=ALU.add), reads=["mv"], writes=["mv"])
            P.op("act", lambda e: e.activation(out=mv[:, 2:3], in_=mv[:, 2:3], func=AF.Sqrt), reads=["mv"], writes=["mv"])
            P.op("dve", lambda e: e.reciprocal(out=mv[:, 3:4], in_=mv[:, 2:3]), reads=["mv"], writes=["mv"])
            P.op("dve", lambda e: e.scalar_tensor_tensor(out=cen[:], in0=cen[:], scalar=mv[:, 3:4], in1=gwb[:], op0=ALU.mult, op1=ALU.mult), reads=["cen", "mv", "gwb"], writes=["cen"])
            P.op("act", lambda e: e.activation(out=sg[:], in_=gt[:, n, :], func=AF.Sigmoid), reads=["gt"], writes=["sgr"])
            P.op("pool", lambda e: e.tensor_tensor(out=sg[:], in0=sg[:], in1=gt[:, n, :], op=ALU.mult), reads=["sgr", "gt"], writes=["sgr"])
            P.op("dve", lambda e: e.tensor_tensor(out=O[:, n, :], in0=cen[:], in1=sg[:], op=ALU.mult), reads=["cen", "sgr"], writes=[("O", n)])

        for n in range(NCH):
            fwd(n)
        for n in [1, 0] + list(range(NCH - 1, 1, -1)):
            bwd(n)
        P.dma("sp", out[pi].rearrange("(n p) e -> p n e", p=128), O[:], reads=[("O", n) for n in range(NCH)], writes=["out"])

    pair(0)
    pair(1)
    return P.finish(["out"])


def launch_ret(ul, uc, rt_decay_l, rt_gn_w_l, cores=range(NCORES)):
    G = 512
    nc = build_ret()
    cosT, sinT, PiT, cc, colv = rt_consts()
    u = np.concatenate([uc, ul], axis=1)
    maps = []
    for c in cores:
        qs, ks, vs, gs, ds, gw = [], [], [], [], [], []
        for pi in (2 * c, 2 * c + 1):
            b, hd = pi // 4, pi % 4
            qs.append(u[b, :, 9 * G + hd * 128:9 * G + (hd + 1) * 128].T); ks.append(u[b, :, 10 * G + hd * 128:10 * G + (hd + 1) * 128].T)
            vs.append(u[b, :, 11 * G + hd * 128:11 * G + (hd + 1) * 128]); gs.append(u[b, :, 12 * G + hd * 128:12 * G + (hd + 1) * 128])
            ds.append(rt_decay_l[:, hd]); gw.append(rt_gn_w_l[hd * 128:(hd + 1) * 128])
        maps.append({"QT": np.ascontiguousarray(np.stack(qs)), "KT": np.ascontiguousarray(np.stack(ks)), "V": np.ascontiguousarray(np.stack(vs)),
                     "G": np.ascontiguousarray(np.stack(gs)), "dec": np.ascontiguousarray(np.stack(ds)), "gnw": np.ascontiguousarray(np.stack(gw)),
                     "cosT": cosT, "sinT": sinT, "PiT": PiT, "cc": cc, "colv": colv})
    res = run(nc, maps)
    o = np.zeros((4, TS, 512), np.float32)
    for ci, c in enumerate(cores):
        for k, pi in enumerate((2 * c, 2 * c + 1)):
            b, hd = pi // 4, pi % 4
            o[b, :, hd * 128:(hd + 1) * 128] = res[ci]["out"][k]
    return np.ascontiguousarray(o[:, 256:]), np.ascontiguousarray(o[:, :256])


HY_MIN_DECAY = np.log(1e-2) / 1.5
HY_MAX_DECAY = np.log(1e-2) / 0.3


def hy_consts(L):
    t = np.linspace(0.0, 1.0, L, dtype=np.float32)
    fr = np.linspace(1e-4, 15, 16, dtype=np.float32)
    ang = (np.float32(2.0 * np.pi / L) * np.arange(L, dtype=np.float32)[:, None] * fr[None, :]).astype(np.float32)
    z = np.concatenate([t[:, None], np.cos(ang), -np.sin(ang)], axis=-1).astype(np.float32)
    return np.ascontiguousarray(z.T), np.ascontiguousarray(t[None, :])


def hy_deltas():
    return np.abs(np.linspace(HY_MIN_DECAY, HY_MAX_DECAY, 512, dtype=np.float32)).astype(np.float32)


HY_DEBUG = False


def build_hyena(with_ctx=True):
    P = Prog()
    Ls = [4096, 256] if with_ctx else [4096]
    sT = {4096: P.inp("sT", [3, 64, 4, 4096])}
    zTs = {4096: P.inp("zT", [33, 4096])}; tls = {4096: P.inp("tl", [1, 4096])}
    oq = {4096: P.out("oq", [128, 64, 32, 4])}
    if with_ctx:
        sT[256] = P.inp("sTc", [3, 64, 4, 256]); zTs[256] = P.inp("zTc", [33, 256]); tls[256] = P.inp("tlc", [1, 256]); oq[256] = P.out("oqc", [128, 64, 2, 4])
    sw = P.inp("sw", [64, 3, 3]); sbi = P.inp("sb", [64, 3]); dl = P.inp("dl", [64, 1])
    w1 = P.inp("w1", [33, 64]); w2 = P.inp("w2", [64, 64]); w3 = P.inp("w3", [64, 64]); w4 = P.inp("w4", [64, 4, 64])
    fq = P.inp("fq", [64, 4])
    hb = P.inp("hb", [2, 64])
    TAPS = {L: P.dram("TAPS%d" % L, [64, 2, 2 * L], F32, "ExternalOutput" if HY_DEBUG else "Internal") for L in Ls}

    swt = P.sb("swt", [64, 3, 3]); sbt = P.sb("sbt", [64, 3]); dlt = P.sb("dlt", [64, 1]); w1t = P.sb("w1t", [33, 64]); w2t = P.sb("w2t", [64, 64]); w3t = P.sb("w3t", [64, 64])
    w4t = P.sb("w4t", [64, 4, 64]); fqt = P.sb("fqt", [64, 4]); fbt = P.sb("fbt", [64, 3]); hbt = P.sb("hbt", [128, 2, 64]); ident = P.sb("identf", [128, 128])
    for t_, s_, k_ in ((swt, sw, "swt"), (sbt, sbi, "sbt"), (dlt, dl, "dlt"), (w1t, w1, "w1t"), (w2t, w2, "w2t"), (w3t, w3, "w3t"), (w4t, w4, "w4t"), (fqt, fq, "fqt")):
        P.dma("sp", t_[:], s_, writes=[k_])
    P.dma("sp", hbt[:].rearrange("p o c -> p (o c)"), hb.rearrange("o c -> (o c)").partition_broadcast(128), writes=["hbt"])
    P.op("dve", lambda e: e.tensor_scalar(out=fbt[:], in0=fqt[:, 1:4], scalar1=fqt[:, 0:1], scalar2=None, op0=ALU.mult), reads=["fqt"], writes=["fbt"])
    P.op("dve", lambda e: e.tensor_scalar(out=dlt[:], in0=dlt[:], scalar1=-1.0, scalar2=None, op0=ALU.mult), reads=["dlt"], writes=["dlt"])
    P.op("pool", lambda e: e.memset(ident[:], 1.0), writes=["identf"])
    P.op("pool", lambda e: e.affine_select(out=ident[:], in_=ident[:], pattern=[[-1, 128]], compare_op=ALU.is_equal, fill=0.0, base=0, channel_multiplier=1), reads=["identf"], writes=["identf"])

    zt = P.sb("zt", [33, 512]); tlt = P.sb("tlt", [64, 512]); big = P.sb("big", [128, 16384]); hA = P.sb("hA", [64, 512]); hB = P.sb("hB", [64, 512])
    Tf = big[0:64, :].rearrange("p (g l) -> p g l", g=4)
    nrm = P.sb("nrm", [64, 4]); rvt = P.sb("rvt", [64, 4096]); junk = rvt
    pA = P.ps("pA", [64, 512]); pT = P.ps("pT", [128, 64]); pC = [P.ps("pC%d" % i, [128, 128]) for i in range(2)]
    TWO_PI = float(2 * np.pi)

    def sin_layer(src_ps, dst, dkey, li, W):
        P.op("dve", lambda e: e.tensor_scalar(out=dst[:, 0:W], in0=src_ps[:, 0:W], scalar1=fqt[:, 0:1], scalar2=fbt[:, li:li + 1], op0=ALU.mult, op1=ALU.add), reads=["pA", "fqt", "fbt"], writes=[dkey])
        P.op("dve", lambda e: e.tensor_scalar(out=dst[:, 0:W], in0=dst[:, 0:W], scalar1=float(1.0 / TWO_PI), scalar2=8.5, op0=ALU.mult, op1=ALU.add), reads=[dkey], writes=[dkey])
        P.op("dve", lambda e: e.tensor_copy(out=kint[:, 0:W], in_=dst[:, 0:W]), reads=[dkey], writes=["kint"])
        P.op("dve", lambda e: e.tensor_copy(out=kflt[:, 0:W], in_=kint[:, 0:W]), reads=["kint"], writes=["kflt"])
        P.op("dve", lambda e: e.tensor_tensor(out=dst[:, 0:W], in0=dst[:, 0:W], in1=kflt[:, 0:W], op=ALU.subtract), reads=[dkey, "kflt"], writes=[dkey])
        P.op("dve", lambda e: e.tensor_scalar(out=kflt[:, 0:W], in0=dst[:, 0:W], scalar1=0.0, scalar2=None, op0=ALU.is_lt), reads=[dkey, "kflt"], writes=["kflt"])
        P.op("dve", lambda e: e.tensor_tensor(out=dst[:, 0:W], in0=dst[:, 0:W], in1=kflt[:, 0:W], op=ALU.add), reads=[dkey, "kflt"], writes=[dkey])
        P.op("act", lambda e: e.activation(out=dst[:, 0:W], in_=dst[:, 0:W], func=AF.Sin, bias=mpi[:, 0:1], scale=TWO_PI), reads=[dkey, "mpi"], writes=[dkey])

    kint = P.sb("kint", [64, 512], I32); kflt = P.sb("kflt", [64, 512])
    mpi = P.sb("mpi", [64, 1]); zero1 = P.sb("zero1", [64, 1]); hbf = P.sb("hbf", [64, 2])
    P.op("dve", lambda e: e.memset(zero1[:], 0.0), writes=["zero1"])
    P.dma("sp", hbf[:], P.inp("hbfi", [64, 2]), writes=["hbf"])
    P.op("dve", lambda e: e.memset(mpi[:], -float(np.pi)), writes=["mpi"])

    def gen_filter(L):
        for c0 in range(0, L, 512):
            def chunk(c0=c0):
                W = min(512, L - c0)
                P.dma("sp", zt[:, 0:W], zTs[L][:, c0:c0 + W], writes=["zt"]); P.dma("sp", tlt[:, 0:W], tls[L][0:1, c0:c0 + W].partition_broadcast(64), writes=["tlt"])
                P.op("act", lambda e: e.activation(out=tlt[:, 0:W], in_=tlt[:, 0:W], func=AF.Exp, scale=dlt[:, 0:1]), reads=["tlt", "dlt"], writes=["tlt"])
                P.op("pe", lambda e: e.matmul(pA[:, 0:W], w1t[:], zt[:, 0:W], start=True, stop=True), reads=["w1t", "zt"], writes=["pA"])
                sin_layer(pA, hA, "hA", 0, W)
                P.op("pe", lambda e: e.matmul(pA[:, 0:W], w2t[:], hA[:, 0:W], start=True, stop=True), reads=["w2t", "hA"], writes=["pA"])
                sin_layer(pA, hB, "hB", 1, W)
                P.op("pe", lambda e: e.matmul(pA[:, 0:W], w3t[:], hB[:, 0:W], start=True, stop=True), reads=["w3t", "hB"], writes=["pA"])
                sin_layer(pA, hA, "hA", 2, W)
                for g in range(4):
                    P.op("pe", lambda e, g=g: e.matmul(pA[:, 0:W], w4t[:, g, :], hA[:, 0:W], start=True, stop=True), reads=["w4t", "hA"], writes=["pA"])
                    P.op("dve", lambda e, g=g: e.tensor_tensor(out=Tf[:, g, c0:c0 + W], in0=pA[:, 0:W], in1=tlt[:, 0:W], op=ALU.mult), reads=["pA", "tlt"], writes=["Tf", "Tz0", "Tz1"])
            chunk()
        for g in range(4):
            lo = 0 if g < 2 else 1
            P.op("act", lambda e, g=g, lo=lo: e.activation(out=junk[:, lo:L], in_=Tf[:, g, lo:L], func=AF.Abs, accum_out=nrm[:, g:g + 1]), reads=["Tf"], writes=["rvt", "nrm"])
        P.op("dve", lambda e: e.tensor_tensor(out=nrm[:, 0:2], in0=nrm[:, 0:2], in1=nrm[:, 2:4], op=ALU.add), reads=["nrm"], writes=["nrm"])
        P.op("dve", lambda e: e.reciprocal(out=nrm[:, 0:2], in_=nrm[:, 0:2]), reads=["nrm"], writes=["nrm"])
        for g in range(4):
            o = g % 2
            P.op("dve", lambda e, g=g, o=o: e.tensor_scalar(out=Tf[:, g, 0:L], in0=Tf[:, g, 0:L], scalar1=nrm[:, o:o + 1], scalar2=None, op0=ALU.mult), reads=["Tf", "nrm"], writes=["Tf"])
        for o in range(2):
            P.op("dve", lambda e, o=o: e.tensor_tensor(out=Tf[:, o, 0:1], in0=Tf[:, o, 0:1], in1=hbf[:, o:o + 1], op=ALU.add), reads=["Tf", "hbf"], writes=["Tf"])
        P.op("pool", lambda e: e.tensor_copy(out=rvt[:, 0:L], in_=rev(Tf[:, 0, 0:L])), reads=["Tf"], writes=["rvt"])
        P.dma("sp", TAPS[L][:, 0, 0:L], rvt[:, 0:L], reads=["rvt"], writes=["TAPS"])
        P.dma("sp", TAPS[L][:, 0, L:2 * L - 1], Tf[:, 2, 1:L], reads=["Tf"], writes=["TAPS"])
        P.dma("sp", TAPS[L][:, 0, 2 * L - 1:2 * L], zero1[:, 0:1], reads=["zero1"], writes=["TAPS"], allow_slow_non_contiguous=True)
        P.dma("sp", TAPS[L][:, 1, L:2 * L], Tf[:, 1, 0:L], reads=["Tf"], writes=["TAPS"])
        P.op("pool", lambda e: e.tensor_copy(out=rvt[:, 1:L], in_=rev(Tf[:, 3, 1:L])), reads=["Tf", "rvt"], writes=["rvt"])
        P.op("pool", lambda e: e.memset(rvt[:, 0:1], 0.0), reads=["rvt"], writes=["rvt"])
        P.dma("sp", TAPS[L][:, 1, 0:L], rvt[:, 0:L], reads=["rvt"], writes=["TAPS"])

    xs = P.sb("xs", [64, 4096]); xcv = P.sb("xcv", [64, 4096])
    NBMAX = 32
    NPART = 4; CPP = 64 // NPART
    Uq = P.sb("Uq", [128, 3, CPP, NBMAX, 4])
    Zp = [P.sb("Zp%d" % i, [128, 3 * NBMAX - 2, 4]) for i in range(2)]
    Tz = [big[:, 0:8192], big[:, 8192:16384]]
    Oq = P.sb("Oq", [128, CPP, NBMAX, 4]); tmpq = P.sb("tmpq", [128, NBMAX, 4])
    cnt = [0]

    def conv_L(L):
        nb = L // 128; ncb = 2 * nb - 1
        def part(half):
            ch0 = half * CPP
            for s in range(3):
                for b in range(4):
                    def sc(s=s, b=b):
                        P.dma("sp", xs[:, 0:L], sT[L][s, :, b, :], writes=["xs"])
                        P.op("dve", lambda e: e.tensor_scalar(out=xcv[:, 0:L], in0=xs[:, 0:L], scalar1=swt[:, 1, s:s + 1], scalar2=sbt[:, s:s + 1], op0=ALU.mult, op1=ALU.add), reads=["xs", "swt", "sbt"], writes=["xcv"])
                        P.op("dve", lambda e: e.scalar_tensor_tensor(out=xcv[:, 1:L], in0=xs[:, 0:L - 1], scalar=swt[:, 0, s:s + 1], in1=xcv[:, 1:L], op0=ALU.mult, op1=ALU.add), reads=["xs", "swt", "xcv"], writes=["xcv"])
                        P.op("dve", lambda e: e.scalar_tensor_tensor(out=xcv[:, 0:L - 1], in0=xs[:, 1:L], scalar=swt[:, 2, s:s + 1], in1=xcv[:, 0:L - 1], op0=ALU.mult, op1=ALU.add), reads=["xs", "swt", "xcv"], writes=["xcv"])
                        srcx = xcv
                        if s == 1:
                            P.op("pool", lambda e: e.tensor_copy(out=xs[:, 0:L], in_=rev(xcv[:, 0:L])), reads=["xcv", "xs"], writes=["xs"])
                            srcx = xs
                        for a in range(nb):
                            ad = a if s != 1 else nb - 1 - a
                            P.op("pe", lambda e, a=a: e.transpose(pT[:, :], srcx[:, a * 128:(a + 1) * 128], ident[0:64, 0:64]), reads=["xcv", "xs", "identf"], writes=["pT"])
                            P.op("act", lambda e, ad=ad: e.activation(out=Uq[:, s, :, ad, b], in_=pT[:, ch0:ch0 + CPP], func=AF.Copy), reads=["pT"], writes=["Uq"])
                    sc()
            for cl in range(CPP):
                ch = ch0 + cl
                for o in range(2):
                    def stage(cl=cl, ch=ch, o=o):
                        i = cnt[0] % 2; cnt[0] += 1
                        tz = Tz[i]; tzk = "Tz%d" % i; zp = Zp[o]; zk = "Zp%d" % o; pc = pC[i]; pck = "pC%d" % i
                        src = bass.AP(TAPS[L].tensor, TAPS[L][ch, o, o:o + 1].offset, [[1, 128], [1, ncb * 128]])
                        P.dma("sp" if i == 0 else "act", tz[:, 0:ncb * 128], src, reads=["TAPS"], writes=[tzk])
                        if o == 0:
                            P.op("pool", lambda e: e.tensor_copy(out=zp[:, nb - 1:2 * nb - 1, :], in_=Uq[:, 0, cl, 0:nb, :]), reads=["Uq"], writes=[zk])
                        for ci in range(ncb):
                            r0 = 2 * (nb - 1) - ci
                            cb = (ncb - 1 - ci) if o == 0 else ci
                            P.op("pe", lambda e, ci=ci, r0=r0, cb=cb: e.matmul(pc[:, 0:nb * 4], tz[:, cb * 128:(cb + 1) * 128], zp[:, r0:r0 + nb, :].rearrange("p a b -> p (a b)"), start=(ci == 0), stop=(ci == ncb - 1)),
                                 reads=[tzk, zk], writes=[pck])
                        pcv = pc[:, 0:nb * 4].rearrange("p (a b) -> p a b", b=4)
                        if o == 0:
                            P.op("dve", lambda e: e.tensor_tensor(out=Zp[1][:, nb - 1:2 * nb - 1, :], in0=pcv, in1=Uq[:, 1, cl, 0:nb, :], op=ALU.mult), reads=[pck, "Uq"], writes=["Zp1"])
                        else:
                            P.op("dve", lambda e: e.tensor_tensor(out=Oq[:, cl, 0:nb, :], in0=pcv, in1=Uq[:, 2, cl, 0:nb, :], op=ALU.mult), reads=[pck, "Uq"], writes=["Oq"])
                    stage()
            P.dma("sp", oq[L][:, ch0:ch0 + CPP, :, :], Oq[:, :, 0:nb, :], reads=["Oq"], writes=["oq%d" % L])
        for half in range(NPART):
            part(half)

    for L in Ls:
        nb = L // 128
        for i in range(2):
            P.op("pool", lambda e, i=i: e.memset(Zp[i][:], 0.0), reads=[], writes=["Zp%d" % i])
        gen_filter(L)
        conv_L(L)
    return P.finish(["oq%d" % L for L in Ls])


def launch_hyena(ul, uc, p, with_ctx=True, cores=range(NCORES)):
    G = 512
    nc = build_hyena(with_ctx)
    zT, tl = hy_consts(4096); zTc, tlc = hy_consts(256)
    dl_all = hy_deltas()
    maps = []
    for c in cores:
        sl = slice(c * 64, (c + 1) * 64)
        cols = [s * G + c * 64 for s in range(3)]
        m = {"sT": np.ascontiguousarray(np.stack([ul[:, :, k:k + 64].transpose(2, 0, 1) for k in cols])), "zT": zT, "tl": tl}
        if with_ctx:
            m["sTc"] = np.ascontiguousarray(np.stack([uc[:, :, k:k + 64].transpose(2, 0, 1) for k in cols])); m["zTc"] = zTc; m["tlc"] = tlc
        swf = p["hy_short_w"].reshape(3, 3, G)[:, :, sl]
        m["sw"] = np.ascontiguousarray(swf.transpose(2, 0, 1))
        m["sb"] = np.ascontiguousarray(p["hy_short_b"].reshape(3, G)[:, sl].T)
        m["dl"] = np.ascontiguousarray(dl_all[sl][:, None])
        m["w1"] = np.ascontiguousarray(p["hy_f_w1"]); m["w2"] = np.ascontiguousarray(p["hy_f_w2"]); m["w3"] = np.ascontiguousarray(p["hy_f_w3"])
        m["w4"] = np.ascontiguousarray(p["hy_f_w4"].reshape(64, 4, G)[:, :, sl])
        m["fq"] = np.ascontiguousarray(np.stack([p["hy_f_freq"], p["hy_f_b1"], p["hy_f_b2"], p["hy_f_b3"]], -1))
        m["hb"] = np.ascontiguousarray(p["hy_bias"][:, sl]); m["hbfi"] = np.ascontiguousarray(p["hy_bias"][:, sl].T)
        maps.append(m)
    res = run(nc, maps)
    hl = np.concatenate([r["oq"].transpose(3, 2, 0, 1).reshape(4, 4096, 64) for r in res], -1)
    hc = np.concatenate([r["oqc"].transpose(3, 2, 0, 1).reshape(4, 256, 64) for r in res], -1) if with_ctx else None
    if HY_DEBUG:
        return hl, hc, res
    return hl, hc


H2T_DT = BF16
DBG_STOP = 0


def build_post():
    P = Prog()
    yT = P.inp("yT", [D, NT * 128]); xt = P.inp("xt", [NT, 128, D]); wo = P.inp("wo", [D, D])
    g1 = P.inp("g1", [2, D]); msh = P.inp("msh", [2, 2, D]); nw = P.inp("nw", [1, D]); rw = P.inp("rw", [D, 32]); rb = P.inp("rb", [1, 32])
    x1 = P.out("x1", [NT, 128, D]); h2T = P.out("h2T", [D, NT * 128], H2T_DT); gate = P.out("gate", [NT, 128, 32])
    A = P.sb("A", [128, 2, D]); S = P.sb("S", [128, 2, D]); nwb = P.sb("nwb", [128, D]); g1b = P.sb("g1b", [128, 2, D])
    wob = P.sb("wob", [128, 16, D], BF16); rwt = P.sb("rwt", [128, 16, 32]); rbt = P.sb("rbt", [1, 32]); ones = P.sb("ones", [1, 128])
    ident = P.sb("identf", [128, 128])
    xs = [P.sb("xs%d" % i, [128, D]) for i in range(2)]; ys = [P.sb("ys%d" % i, [128, 16, 128], BF16) for i in range(2)]
    tmp = P.sb("tmp", [128, D]); sq = P.sb("sq", [128, D]); ss = P.sb("ss", [128, 1]); rs = P.sb("rs", [128, 1]); h2 = P.sb("h2", [128, D])
    hTf = P.sb("hTf", [128, 16, 128]); hTb = P.sb("hTb", [128, 16, 128], BF16)
    lg = P.sb("lg", [128, 32]); mx = P.sb("mx", [128, 8]); msk = P.sb("msk", [128, 32]); ex = P.sb("ex", [128, 32]); sm = P.sb("sm", [128, 2]); gt = P.sb("gt", [128, 32])
    psm = [P.ps("psm%d" % i, [128, 512]) for i in range(3)]
    pst = [P.ps("pst%d" % i, [128, 4, 128]) for i in range(2)]
    psr = P.ps("psr", [128, 32])
    P.op("pool", lambda e: e.memset(ident[:], 1.0), writes=["identf"])
    P.op("pool", lambda e: e.affine_select(out=ident[:], in_=ident[:], pattern=[[-1, 128]], compare_op=ALU.is_equal, fill=0.0, base=0, channel_multiplier=1), reads=["identf"], writes=["identf"])
    P.op("dve", lambda e: e.memset(ones[:], 1.0), writes=["ones"])
    emit_AS(P, msh, nw, A, S, nwb)
    for wch in range(2):
        P.dma("sp", g1b[:, wch, :], g1[wch:wch + 1, :].partition_broadcast(128), writes=["g1b"])
    for kc in range(16):
        P.dma("pool", wob[:, kc, :], wo[kc * 128:(kc + 1) * 128, :], writes=["wob"])
    P.dma("sp", rwt[:], rw.rearrange("(kc p) n -> p kc n", p=128), writes=["rwt"]); P.dma("sp", rbt[:], rb[:, :], writes=["rbt"])
    kk = [0, 0]

    def tile(t):
        wch = 0 if t == 0 else 1
        x_ = xs[t % 2]; xk = "xs%d" % (t % 2); y_ = ys[t % 2]; yk = "ys%d" % (t % 2)
        P.dma("sp", x_[:], xt[t], writes=[xk])
        P.dma("pool", y_[:], yT[:, t * 128:(t + 1) * 128].rearrange("(kc p) n -> p kc n", p=128), writes=[yk])
        for n in range(4):
            i = kk[0] % 3; kk[0] += 1
            pp = psm[i]; pk = "psm%d" % i
            for kc in range(16):
                P.op("pe", lambda e, pp=pp, kc=kc, n=n: e.matmul(pp[:], y_[:, kc, :], wob[:, kc, n * 512:(n + 1) * 512], start=(kc == 0), stop=(kc == 15)), reads=[yk, "wob"], writes=[pk])
            P.op("dve", lambda e, pp=pp, n=n: e.tensor_tensor(out=tmp[:, n * 512:(n + 1) * 512], in0=pp[:], in1=g1b[:, wch, n * 512:(n + 1) * 512], op=ALU.mult), reads=[pk, "g1b"], writes=["tmp"])
        P.op("pool", lambda e: e.tensor_tensor(out=x_[:], in0=x_[:], in1=tmp[:], op=ALU.add), reads=["tmp", xk], writes=[xk])
        P.dma("sp", x1[t], x_[:], reads=[xk], writes=["x1"])
        if DBG_STOP == 1:
            return
        P.op("act", lambda e: e.activation(out=sq[:], in_=x_[:], func=AF.Square, accum_out=ss[:]), reads=[xk], writes=["sq", "ss"])
        P.op("dve", lambda e: e.tensor_scalar(out=rs[:], in0=ss[:], scalar1=1.0 / D, scalar2=EPS, op0=ALU.mult, op1=ALU.add), reads=["ss"], writes=["rs"])
        P.op("act", lambda e: e.activation(out=rs[:], in_=rs[:], func=AF.Sqrt), reads=["rs"], writes=["rs"])
        P.op("dve", lambda e: e.reciprocal(out=rs[:], in_=rs[:]), reads=["rs"], writes=["rs"])
        P.op("dve", lambda e: e.scalar_tensor_tensor(out=tmp[:], in0=x_[:], scalar=rs[:, 0:1], in1=A[:, wch, :], op0=ALU.mult, op1=ALU.mult), reads=[xk, "rs", "A", "tmp"], writes=["tmp"])
        P.op("pool", lambda e: e.tensor_tensor(out=h2[:], in0=tmp[:], in1=S[:, wch, :], op=ALU.add), reads=["tmp", "S"], writes=["h2"])
        if DBG_STOP == 2:
            return
        for g in range(4):
            i = kk[1] % 2; kk[1] += 1
            pt = pst[i]; pk = "pst%d" % i
            for j in range(4):
                kc = g * 4 + j
                P.op("pe", lambda e, pt=pt, j=j, kc=kc: e.transpose(pt[:, j, :], h2[:, kc * 128:(kc + 1) * 128], ident[:]), reads=["h2", "identf"], writes=[pk])
            P.op("act", lambda e, pt=pt, g=g: e.activation(out=hTf[:, g * 4:(g + 1) * 4, :], in_=pt[:], func=AF.Copy), reads=[pk], writes=["hTf"])
            P.op("dve", lambda e, g=g: e.tensor_copy(out=hTb[:, g * 4:(g + 1) * 4, :], in_=hTf[:, g * 4:(g + 1) * 4, :]), reads=["hTf"], writes=["hTb"])
        P.dma("sp", h2T[:, t * 128:(t + 1) * 128].rearrange("(kc p) n -> p kc n", p=128), (hTb[:] if H2T_DT == BF16 else hTf[:]), reads=["hTb", "hTf"], writes=["h2T"])
        if DBG_STOP == 3:
            return
        for kc in range(16):
            P.op("pe", lambda e, kc=kc: e.matmul(psr[:], hTf[:, kc, :], rwt[:, kc, :], start=(kc == 0), stop=False), reads=["hTf", "rwt"], writes=["psr"])
        P.op("pe", lambda e: e.matmul(psr[:], ones[:, :], rbt[:, :], start=False, stop=True), reads=["ones", "rbt"], writes=["psr"])
        P.op("dve", lambda e: e.tensor_copy(out=lg[:], in_=psr[:]), reads=["psr"], writes=["lg"])
        if DBG_STOP == 4:
            return
        P.op("dve", lambda e: e.max(out=mx[:], in_=lg[:]), reads=["lg"], writes=["mx"])
        if DBG_STOP == 5:
            return
        P.op("dve", lambda e: e.tensor_scalar(out=msk[:], in0=lg[:], scalar1=mx[:, 3:4], scalar2=None, op0=ALU.is_ge), reads=["lg", "mx"], writes=["msk"])
        P.op("dve", lambda e: e.tensor_scalar(out=sm[:, 0:1], in0=mx[:, 0:1], scalar1=-1.0, scalar2=None, op0=ALU.mult), reads=["mx"], writes=["sm"])
        P.op("act", lambda e: e.activation(out=ex[:], in_=lg[:], func=AF.Exp, bias=sm[:, 0:1]), reads=["lg", "sm"], writes=["ex"])
        P.op("dve", lambda e: e.tensor_tensor(out=ex[:], in0=ex[:], in1=msk[:], op=ALU.mult), reads=["ex", "msk"], writes=["ex"])
        P.op("dve", lambda e: e.tensor_reduce(out=sm[:, 1:2], in_=ex[:], axis=AX.X, op=ALU.add), reads=["ex", "sm"], writes=["sm"])
        P.op("dve", lambda e: e.reciprocal(out=sm[:, 1:2], in_=sm[:, 1:2]), reads=["sm"], writes=["sm"])
        P.op("dve", lambda e: e.tensor_scalar(out=gt[:], in0=ex[:], scalar1=sm[:, 1:2], scalar2=None, op0=ALU.mult), reads=["ex", "sm"], writes=["gt"])
        P.dma("sp", gate[t], gt[:], reads=["gt"], writes=["gate"])

    for t in range(NT):
        tile(t)
    return P.finish(["x1", "h2T", "gate"])


def launch_post(mix_l, mix_c, xts, mod_l, w_out_l, nw2_l, rw_l, rb_l):
    nc = build_post()
    m6 = mod_l.reshape(5, 6, D)
    mts = tile_split(mix_l, mix_c)
    maps = []
    for c in range(NCORES):
        b = c // 2
        maps.append({"yT": np.ascontiguousarray(mts[c].reshape(NT * 128, D).T), "xt": xts[c], "wo": w_out_l,
                     "g1": np.ascontiguousarray(np.stack([m6[4, 2], m6[b, 2]], 0)),
                     "msh": np.ascontiguousarray(np.stack([m6[4, 3:5], m6[b, 3:5]], 0)),
                     "nw": np.ascontiguousarray(nw2_l[None]), "rw": rw_l, "rb": np.ascontiguousarray(rb_l[None])})
    res = run(nc, maps)
    return [r["x1"] for r in res], [r["h2T"] for r in res], [r["gate"] for r in res]


NE_C = 8
NTG = 68
FE = 1024


def build_moe(nb_limit=None):
    P = Prog()
    hT = P.inp("hT", [D, NTG * 128], BF16); gtT = P.inp("gtT", [NE_C, NTG * 128]); gt = P.inp("gt", [128, NTG, NE_C])
    wgu = P.inp("wgu", [NE_C, D, 2 * FE]); bgu = P.inp("bgu", [128, NE_C, 16]); wdn = P.inp("wdn", [NE_C, FE, D]); bdn = P.inp("bdn", [NE_C, D])
    part = P.out("part", [NTG, 128, D])
    gts = P.sb("gts", [128, NTG, NE_C]); bgt = P.sb("bgt", [128, NE_C, 16]); bdt = P.sb("bdt", [NE_C, D]); gTb = [P.sb("gTb%d" % i, [NE_C, 512]) for i in range(2)]
    hb = [P.sb("hb%d" % i, [128, 16, 512], BF16) for i in range(2)]
    wgf = [P.sb("wgf%d" % i, [128, 16, 2, 128], BF16) for i in range(3)]
    wd = [P.sb("wd%d" % i, [128, 8, D], BF16) for i in range(2)]
    act = [P.sb("act%d" % i, [128, 8, 512], BF16) for i in range(2)]
    acc = P.sb("acc", [128, 4, D])
    gs = P.sb("gs", [128, 512]); ls = P.sb("ls", [128, 512]); sgs = P.sb("sgs", [128, 512])
    pg = [P.ps("pg%d" % i, [128, 512]) for i in range(2)]; pl = [P.ps("pl%d" % i, [128, 512]) for i in range(2)]; p2 = [P.ps("p2%d" % i, [128, 512]) for i in range(2)]
    P.dma("sp", gts[:], gt[:, :, :], writes=["gts"]); P.dma("sp", bgt[:], bgu[:, :, :], writes=["bgt"]); P.dma("sp", bdt[:], bdn[:, :], writes=["bdt"])
    cnt = {"wgf": 0, "wd": 0, "ps1": 0, "p2": 0, "act": 0}
    NB = NTG // 4

    def block(bi):
        h_ = hb[bi % 2]; hk = "hb%d" % (bi % 2); gT_ = gTb[bi % 2]; gk = "gTb%d" % (bi % 2)
        c0 = bi * 512
        P.dma("sp", h_[:], hT[:, c0:c0 + 512].rearrange("(kc p) n -> p kc n", p=128), writes=[hk])
        P.dma("sp", gT_[:], gtT[:, c0:c0 + 512], writes=[gk])
        for tl in range(4):
            for n in range(4):
                i = cnt["p2"] % 2; cnt["p2"] += 1
                pp = p2[i]; pk = "p2%d" % i
                P.op("pe", lambda e, pp=pp, tl=tl, n=n: e.matmul(pp[:], gT_[:, tl * 128:(tl + 1) * 128], bdt[:, n * 512:(n + 1) * 512], start=True, stop=True), reads=[gk, "bdt"], writes=[pk])
                P.op("act", lambda e, pp=pp, tl=tl, n=n: e.activation(out=acc[:, tl, n * 512:(n + 1) * 512], in_=pp[:], func=AF.Copy), reads=[pk], writes=["acc"])
        for ex in range(NE_C):
            expert(bi, ex, h_, hk)
        P.dma("sp", part[bi * 4:(bi + 1) * 4].rearrange("t p d -> p t d"), acc[:], reads=["acc"], writes=["part"])

    def expert(bi, ex, h_, hk):
        wi = cnt["wd"] % 2; cnt["wd"] += 1
        wd_ = wd[wi]; wdk = "wd%d" % wi
        P.dma("pool", wd_[:], wdn[ex].rearrange("(fc p) n -> p fc n", p=128), writes=[wdk])
        ai = cnt["act"] % 2; cnt["act"] += 1
        a_ = act[ai]; ak = "act%d" % ai
        for fc in range(8):
            def fcb(fc=fc):
                ri = cnt["wgf"] % 3; cnt["wgf"] += 1
                wf = wgf[ri]; wfk = "wgf%d" % ri
                P.dma("pool", wf[:, :, 0, :], wgu[ex, :, fc * 128:(fc + 1) * 128].rearrange("(kc p) n -> p kc n", p=128), writes=[wfk])
                P.dma("pool", wf[:, :, 1, :], wgu[ex, :, FE + fc * 128:FE + (fc + 1) * 128].rearrange("(kc p) n -> p kc n", p=128), writes=[wfk])
                pi = cnt["ps1"] % 2; cnt["ps1"] += 1
                pg_ = pg[pi]; pgk = "pg%d" % pi; pl_ = pl[pi]; plk = "pl%d" % pi
                for kc in range(16):
                    P.op("pe", lambda e, kc=kc: e.matmul(pg_[:], wf[:, kc, 0, :], h_[:, kc, :], start=(kc == 0), stop=(kc == 15)), reads=[wfk, hk], writes=[pgk])
                for kc in range(16):
                    P.op("pe", lambda e, kc=kc: e.matmul(pl_[:], wf[:, kc, 1, :], h_[:, kc, :], start=(kc == 0), stop=(kc == 15)), reads=[wfk, hk], writes=[plk])
                P.op("dve", lambda e: e.tensor_scalar(out=gs[:], in0=pg_[:], scalar1=bgt[:, ex, fc:fc + 1], scalar2=7.0, op0=ALU.add, op1=ALU.min), reads=[pgk, "bgt"], writes=["gs"])
                P.op("dve", lambda e: e.tensor_scalar(out=ls[:], in0=pl_[:], scalar1=bgt[:, ex, 8 + fc:9 + fc], scalar2=7.0, op0=ALU.add, op1=ALU.min), reads=[plk, "bgt"], writes=["ls"])
                P.op("dve", lambda e: e.tensor_scalar(out=ls[:], in0=ls[:], scalar1=-7.0, scalar2=1.0, op0=ALU.max, op1=ALU.add), reads=["ls"], writes=["ls"])
                P.op("act", lambda e: e.activation(out=sgs[:], in_=gs[:], func=AF.Sigmoid, scale=1.702), reads=["gs"], writes=["sgs"])
                P.op("dve", lambda e: e.tensor_tensor(out=gs[:], in0=gs[:], in1=sgs[:], op=ALU.mult), reads=["gs", "sgs"], writes=["gs"])
                P.op("dve", lambda e: e.tensor_tensor(out=a_[:, fc, :], in0=gs[:], in1=ls[:], op=ALU.mult), reads=["gs", "ls"], writes=[ak])
            fcb()
        for tl in range(4):
            for n in range(4):
                def st2(tl=tl, n=n):
                    i = cnt["p2"] % 2; cnt["p2"] += 1
                    pp = p2[i]; pk = "p2%d" % i
                    for fc in range(8):
                        P.op("pe", lambda e, fc=fc: e.matmul(pp[:], a_[:, fc, tl * 128:(tl + 1) * 128], wd_[:, fc, n * 512:(n + 1) * 512], start=(fc == 0), stop=(fc == 7)), reads=[ak, wdk], writes=[pk])
                    P.op("dve", lambda e: e.scalar_tensor_tensor(out=acc[:, tl, n * 512:(n + 1) * 512], in0=pp[:], scalar=gts[:, bi * 4 + tl, ex:ex + 1], in1=acc[:, tl, n * 512:(n + 1) * 512], op0=ALU.mult, op1=ALU.add),
                         reads=[pk, "gts", "acc"], writes=["acc"])
                st2()

    for bi in range(NB if nb_limit is None else nb_limit):
        block(bi)
    return P.finish(["part"])


def launch_moe(h2T_cores, gate_cores, w_gu_l, b_gu_l, w_dn_l, b_dn_l):
    nc = build_moe()
    maps = []
    for c in range(NCORES):
        g, e4 = c // 4, c % 4
        es = slice(e4 * NE_C, (e4 + 1) * NE_C)
        hT = np.ascontiguousarray(np.concatenate([h2T_cores[4 * g + k] for k in range(4)], axis=1))
        gt = np.concatenate([gate_cores[4 * g + k].reshape(17 * 128, 32) for k in range(4)], axis=0)[:, es]
        maps.append({"hT": hT, "gtT": np.ascontiguousarray(gt.T), "gt": np.ascontiguousarray(gt.reshape(NTG, 128, NE_C).transpose(1, 0, 2)),
                     "wgu": np.ascontiguousarray(w_gu_l[es]), "bgu": np.ascontiguousarray(b_gu_l[es].reshape(NE_C, 16, 128).transpose(2, 0, 1)),
                     "wdn": np.ascontiguousarray(w_dn_l[es]), "bdn": np.ascontiguousarray(b_dn_l[es])})
    res = run(nc, maps)
    parts = []
    for tc in range(NCORES):
        g, k = tc // 4, tc % 4
        parts.append([np.ascontiguousarray(res[4 * g + e4]["part"][k * 17:(k + 1) * 17]) for e4 in range(4)])
    return parts


def build_final(npart=4):
    P = Prog()
    NL = 16
    xt = P.inp("xt", [NL, 128, D]); parts = [P.inp("part%d" % k, [NL, 128, D]) for k in range(npart)]
    g5 = P.inp("g5", [1, D]); fw = P.inp("fw", [1, D]); out = P.out("out", [NL, 128, D])
    g5b = P.sb("g5b", [128, 1, D]); fwb = P.sb("fwb", [128, D]); pt_tiles = [P.sb("ptl%d" % i, [128, D]) for i in range(2)] + [P.sb("pacc", [128, D])]
    xs = [P.sb("xs%d" % i, [128, D]) for i in range(2)]; sq = P.sb("sq", [128, D]); ss = P.sb("ss", [128, 1]); rs = P.sb("rs", [128, 1])
    ob = [P.sb("ob%d" % i, [128, D]) for i in range(2)]
    P.dma("sp", g5b[:, 0, :], g5[0:1, :].partition_broadcast(128), writes=["g5b"]); P.dma("sp", fwb[:], fw[0:1, :].partition_broadcast(128), writes=["fwb"])
    ccnt = [0]

    def tile(t):
        x_ = xs[t % 2]; xk = "xs%d" % (t % 2); o_ = ob[t % 2]; ok = "ob%d" % (t % 2)
        P.dma("sp", x_[:], xt[t], writes=[xk])
        emit_combine(P, x_, xk, t, parts, g5b, 0, pt_tiles, ccnt)
        P.op("act", lambda e: e.activation(out=sq[:], in_=x_[:], func=AF.Square, accum_out=ss[:]), reads=[xk], writes=["sq", "ss"])
        P.op("dve", lambda e: e.tensor_scalar(out=rs[:], in0=ss[:], scalar1=1.0 / D, scalar2=EPS, op0=ALU.mult, op1=ALU.add), reads=["ss"], writes=["rs"])
        P.op("act", lambda e: e.activation(out=rs[:], in_=rs[:], func=AF.Sqrt), reads=["rs"], writes=["rs"])
        P.op("dve", lambda e: e.reciprocal(out=rs[:], in_=rs[:]), reads=["rs"], writes=["rs"])
        P.op("dve", lambda e: e.scalar_tensor_tensor(out=o_[:], in0=x_[:], scalar=rs[:, 0:1], in1=fwb[:], op0=ALU.mult, op1=ALU.mult), reads=[xk, "rs", "fwb"], writes=[ok])
        P.dma("sp", out[t], o_[:], reads=[ok], writes=["out"])

    for t in range(NL):
        tile(t)
    return P.finish(["out"])


def launch_final(xts, parts, mod_last, fw):
    nc = build_final(len(parts[0]))
    m6 = mod_last.reshape(5, 6, D)
    maps = []
    for c in range(NCORES):
        b = c // 2
        m = {"xt": np.ascontiguousarray(xts[c][1:]), "g5": np.ascontiguousarray(m6[b, 5][None]), "fw": np.ascontiguousarray(fw[None])}
        for k in range(len(parts[c])):
            m["part%d" % k] = np.ascontiguousarray(parts[c][k][1:])
        maps.append(m)
    res = run(nc, maps)
    out = np.empty((4, 4096, D), np.float32)
    for c in range(NCORES):
        b, h = c // 2, c % 2
        out[b, h * 2048:(h + 1) * 2048] = res[c]["out"].reshape(2048, D)
    return out


def kernel(**inp):
    inp = {k: np.asarray(v) for k, v in inp.items()}
    G = 512
    mod = launch_mod(inp["c"], inp["c_ctx"], inp["ada_w"], inp["ada_b"])
    xts = tile_split(inp["x"], inp["ctx"])
    parts = None
    vfl = vfc = None
    for l in range(2):
        last = (l == 1)
        ul, uc, xts = launch_win(xts, mod[l], np.ascontiguousarray(inp["w_in"][l]), inp["norm1_w"][l], parts, mod[l - 1] if l > 0 else None)
        hp = {k: inp[k][l] for k in inp if k.startswith("hy_")}
        hy_l, hy_c = launch_hyena(ul, uc, hp, with_ctx=not last)
        if hy_c is None:
            hy_c = np.zeros((4, 256, G), np.float32)
        rg_l, rg_c = launch_rglru(ul, uc, inp["rg_conv_w"][l], inp["rg_conv_b"][l], inp["rg_wa"][l], inp["rg_ba"][l], inp["rg_wx"][l], inp["rg_bx"][l], inp["rg_lambda"][l])
        rp = {k: inp[k][l] for k in inp if k.startswith("rw_") and k not in ("rw_v0", "rw_v1", "rw_v2")}
        if l == 0:
            vfl = np.ascontiguousarray(ul[..., 7 * G:8 * G]); vfc = np.ascontiguousarray(uc[..., 7 * G:8 * G])
            rp["rw_v0"] = np.zeros(G, np.float32); rp["rw_v1"] = np.zeros((G, 32), np.float32); rp["rw_v2"] = np.zeros((32, G), np.float32)
        else:
            rp["rw_v0"] = inp["rw_v0"][l - 1]; rp["rw_v1"] = inp["rw_v1"][l - 1]; rp["rw_v2"] = inp["rw_v2"][l - 1]
        rw_l, rw_c = launch_rwkv(ul, uc, vfl, vfc, rp, l)
        rt_l, rt_c = launch_ret(ul, uc, inp["rt_decay"][l], inp["rt_gn_w"][l])
        del ul, uc
        mix_l = np.concatenate([hy_l, rg_l, rw_l, rt_l], -1); mix_c = np.concatenate([hy_c, rg_c, rw_c, rt_c], -1)
        x1, h2T, gate = launch_post(mix_l, mix_c, xts, mod[l], np.ascontiguousarray(inp["w_out"][l]), inp["norm2_w"][l],
                                    np.ascontiguousarray(inp["moe_router_w"][l]), inp["moe_router_b"][l])
        parts = launch_moe(h2T, gate, inp["moe_w_gu"][l], inp["moe_b_gu"][l], inp["moe_w_dn"][l], inp["moe_b_dn"][l])
        xts = x1
    return launch_final(xts, parts, mod[1], inp["final_norm_w"])
```
